# Optimizing a Trainium2 kernel written in Bass

```python
import math
import jax
import jax.numpy as jnp
from jax import lax
import numpy as np

D_MODEL = 1024
BATCH = 8
SEQ = 8192
DEPTH = 2

EPS = 1e-6
ROPE_THETA = 10000.0
D_FF = 2816
GDN_HEADS = 4
GDN_DK = 128
GDN_DV = 128
GDN_CONV = 4
GDN_CHUNK = 64
RET_HEADS = 4
RET_DK = 64
RET_DV = 128
RET_CHUNK = 64
DIL_HEADS = 4
DIL_DH = 128
DIL_PATTERNS = ((128, 1), (512, 4), (2048, 16))
DIFF_HEADS = 4
DIFF_DK = 64
DIFF_DV = 128
Q_BLOCK = 128

GDN_QK_W = GDN_HEADS * GDN_DK
GDN_V_W = GDN_HEADS * GDN_DV
GDN_CONV_W = 2 * GDN_QK_W + GDN_V_W
RET_QK_W = RET_HEADS * RET_DK
RET_V_W = RET_HEADS * RET_DV
IN0_SPLITS = (GDN_CONV_W, GDN_V_W, GDN_HEADS, GDN_HEADS, RET_QK_W, RET_QK_W, RET_V_W, RET_V_W)
IN0_W = sum(IN0_SPLITS)
MIX0_W = GDN_V_W + RET_V_W
DIL_W = DIL_HEADS * DIL_DH
DIFF_QK_W = DIFF_HEADS * 2 * DIFF_DK
DIFF_V_W = DIFF_HEADS * DIFF_DV
IN1_SPLITS = (DIL_W, DIL_W, DIL_W, DIFF_QK_W, DIFF_QK_W, DIFF_V_W)
IN1_W = sum(IN1_SPLITS)
MIX1_W = DIL_W + DIFF_V_W

kernel_name = 'hybrid_delta_retention_dilated_diff'


def rmsnorm(x, w=None):
    xf = x.astype(jnp.float32)
    y = xf * lax.rsqrt(jnp.mean(xf * xf, axis=-1, keepdims=True) + EPS)
    if w is not None:
        y = y * w.astype(jnp.float32)
    return y.astype(x.dtype)


def l2norm(t):
    tf = t.astype(jnp.float32)
    return (tf * lax.rsqrt(jnp.sum(tf * tf, axis=-1, keepdims=True) + EPS)).astype(t.dtype)


def split_cols(t, sizes):
    offs = np.cumsum(sizes)[:-1].tolist()
    return jnp.split(t, offs, axis=-1)


def swiglu(h, w_up, w_down):
    gate, up = jnp.split(h @ w_up, 2, axis=-1)
    return (jax.nn.silu(gate) * up) @ w_down


def rope_tables(seq, dim):
    inv = ROPE_THETA ** (-jnp.arange(0, dim, 2, dtype=jnp.float32) / dim)
    ang = jnp.arange(seq, dtype=jnp.float32)[:, None] * inv[None, :]
    return jnp.cos(ang), jnp.sin(ang)


def apply_rope(x, cos, sin):
    x1, x2 = jnp.split(x, 2, axis=-1)
    c = cos[None, :, None, :].astype(x.dtype)
    s = sin[None, :, None, :].astype(x.dtype)
    return jnp.concatenate([x1 * c - x2 * s, x1 * s + x2 * c], axis=-1)


def causal_depthwise_conv(x, w):
    K, C = w.shape
    return lax.conv_general_dilated(x, w[:, None, :].astype(x.dtype), window_strides=(1,),
                                    padding=[(K - 1, 0)], dimension_numbers=('NWC', 'WIO', 'NWC'),
                                    feature_group_count=C)


def to_chunks(t, c):
    B, S = t.shape[:2]
    t = t.reshape(B, S // c, c, *t.shape[2:])
    return jnp.swapaxes(t, 2, 3)


def from_chunks(t):
    t = jnp.swapaxes(t, 2, 3)
    return t.reshape(t.shape[0], t.shape[1] * t.shape[2], *t.shape[3:])


def gated_delta_rule_chunked(q, k, v, g, beta):
    B, S, H, dk = q.shape
    dv = v.shape[-1]
    C = GDN_CHUNK
    f32 = jnp.float32
    qc, kc, vc = (to_chunks(t.astype(f32), C) for t in (q, k, v))
    gc, bc = (to_chunks(t.astype(f32), C) for t in (g, beta))
    gcum = jnp.cumsum(gc, axis=-1)
    idx = jnp.arange(C)
    causal = idx[:, None] >= idx[None, :]
    strict = idx[:, None] > idx[None, :]
    gdiff = gcum[..., :, None] - gcum[..., None, :]
    decay = jnp.where(causal, jnp.exp(jnp.where(causal, gdiff, 0.0)), 0.0)
    a_strict = jnp.where(strict, jnp.einsum('bnhid,bnhjd->bnhij', kc, kc) * decay * bc[..., :, None], 0.0)
    rhs = jnp.concatenate([vc * bc[..., None], kc * (bc * jnp.exp(gcum))[..., None]], axis=-1)
    sol = lax.linalg.triangular_solve(a_strict + jnp.eye(C, dtype=f32), rhs, left_side=True,
                                      lower=True, unit_diagonal=True)
    u, w = sol[..., :dv], sol[..., dv:]
    qk = jnp.einsum('bnhid,bnhjd->bnhij', qc, kc) * decay
    q_dec = qc * jnp.exp(gcum)[..., None]
    g_last = gcum[..., -1]
    k_dec = kc * jnp.exp(g_last[..., None] - gcum)[..., None]

    def step(state, xs):
        qk_n, q_n, w_n, u_n, k_n, gl_n = xs
        v_new = u_n - jnp.einsum('bhcd,bhde->bhce', w_n, state)
        o_n = jnp.einsum('bhcd,bhde->bhce', q_n, state) + jnp.einsum('bhij,bhje->bhie', qk_n, v_new)
        state = state * jnp.exp(gl_n)[..., None, None] + jnp.einsum('bhcd,bhce->bhde', k_n, v_new)
        return state, o_n

    xs = tuple(jnp.moveaxis(t, 1, 0) for t in (qk, q_dec, w, u, k_dec, g_last))
    _, o = lax.scan(step, jnp.zeros((B, H, dk, dv), f32), xs)
    return from_chunks(jnp.moveaxis(o, 0, 1)).astype(v.dtype)


def retention_chunked(q, k, v):
    B, S, H, dk = q.shape
    dv = v.shape[-1]
    C = RET_CHUNK
    f32 = jnp.float32
    log_gamma = jnp.log(1.0 - 2.0 ** (-5.0 - jnp.arange(H, dtype=f32)))
    qc, kc, vc = (to_chunks(t.astype(f32), C) for t in (q, k, v))
    idx = jnp.arange(C, dtype=f32)
    rel = idx[:, None] - idx[None, :]
    causal = rel >= 0
    dmask = jnp.where(causal, jnp.exp(jnp.where(causal, rel, 0.0)[None] * log_gamma[:, None, None]), 0.0)
    intra = jnp.einsum('bnhij,bnhje->bnhie', jnp.einsum('bnhid,bnhjd->bnhij', qc, kc) * dmask, vc)
    k_scale = jnp.exp((C - 1 - idx)[None, :] * log_gamma[:, None])
    q_scale = jnp.exp((idx + 1)[None, :] * log_gamma[:, None])
    chunk_decay = jnp.exp(C * log_gamma)
    kv = jnp.einsum('bnhcd,bnhce->bnhde', kc * k_scale[..., None], vc)

    def step(state, kv_n):
        return state * chunk_decay[:, None, None] + kv_n, state

    _, prev = lax.scan(step, jnp.zeros((B, H, dk, dv), f32), jnp.moveaxis(kv, 1, 0))
    prev = jnp.moveaxis(prev, 0, 1)
    inter = jnp.einsum('bnhcd,bnhde->bnhce', qc, prev) * q_scale[..., None]
    return from_chunks(intra + inter).astype(v.dtype)


def dilated_branch(q, k, v, window, dilation):
    B, S, H, d = q.shape
    L = S // dilation
    n_keys = window // dilation
    blk = min(n_keys, L)
    nb = -(-L // blk)
    Lp = nb * blk

    def strided(t):
        t = t.reshape(B, L, dilation, H, d).swapaxes(1, 2)
        t = jnp.pad(t, ((0, 0), (0, 0), (0, Lp - L), (0, 0), (0, 0)))
        return t.reshape(B, dilation, nb, blk, H, d)

    def with_prev(t):
        prev = jnp.pad(t, ((0, 0), (0, 0), (1, 0), (0, 0), (0, 0), (0, 0)))[:, :, :-1]
        return jnp.concatenate([prev, t], axis=3)

    qs = strided(q)
    kb, vb = with_prev(strided(k)), with_prev(strided(v))
    s = jnp.einsum('brnqhd,brnkhd->brnhqk', qs, kb).astype(jnp.float32)
    qi = jnp.arange(blk)[:, None] + blk
    kj = jnp.arange(2 * blk)[None, :]
    dist = qi - kj
    band = (dist >= 0) & (dist <= n_keys)
    has_prev = (jnp.arange(nb) > 0)[:, None, None] | (kj >= blk)[None]
    valid = band[None] & has_prev
    s = jnp.where(valid[:, None], s, -jnp.inf)
    m = jnp.max(s, axis=-1)
    p = jnp.exp(s - m[..., None])
    l = jnp.sum(p, axis=-1)
    o = jnp.einsum('brnhqk,brnkhd->brnqhd', p.astype(v.dtype), vb).astype(jnp.float32)

    def unstrided(t):
        t = t.reshape(B, dilation, Lp, *t.shape[4:])[:, :, :L]
        return t.swapaxes(1, 2).reshape(B, S, *t.shape[3:])

    return unstrided(o), unstrided(m.swapaxes(3, 4)), unstrided(l.swapaxes(3, 4))


def dilated_attention(q, k, v):
    outs = [dilated_branch(q, k, v, w, r) for (w, r) in DIL_PATTERNS]
    m_all = jnp.max(jnp.stack([o[1] for o in outs]), axis=0)
    num = jnp.zeros_like(outs[0][0])
    den = jnp.zeros_like(outs[0][2])
    for o_g, m_g, l_g in outs:
        scale = jnp.exp(m_g - m_all)
        num = num + o_g * scale[..., None]
        den = den + l_g * scale
    return (num / den[..., None]).astype(v.dtype)


def differential_attention(q1, q2, k1, k2, v, lam):
    B, S, H, dk = q1.shape
    nq = S // Q_BLOCK

    def blocks(t):
        return jnp.moveaxis(t.reshape(B, nq, Q_BLOCK, H, t.shape[-1]), 1, 0)

    key_pos = jnp.arange(S)

    def one_block(args):
        i, qa, qb = args
        q_pos = i * Q_BLOCK + jnp.arange(Q_BLOCK)
        causal = (q_pos[:, None] >= key_pos[None, :])[None, None]
        s1 = jnp.where(causal, jnp.einsum('bqhd,bkhd->bhqk', qa, k1).astype(jnp.float32), -jnp.inf)
        s2 = jnp.where(causal, jnp.einsum('bqhd,bkhd->bhqk', qb, k2).astype(jnp.float32), -jnp.inf)
        attn = jax.nn.softmax(s1, axis=-1) - lam * jax.nn.softmax(s2, axis=-1)
        return jnp.einsum('bhqk,bkhe->bqhe', attn.astype(v.dtype), v)

    o = lax.map(one_block, (jnp.arange(nq), blocks(q1), blocks(q2)))
    return jnp.moveaxis(o, 0, 1).reshape(B, S, H, v.shape[-1])


def mixer_delta_retention(h, w_in, conv_w, a_log, dt_bias, gdn_norm, w_out, cos, sin):
    B, S, _ = h.shape
    qkv, z, b, a, rq, rk, rv, rg = split_cols(h @ w_in, IN0_SPLITS)
    qkv = jax.nn.silu(causal_depthwise_conv(qkv, conv_w))
    q, k, v = split_cols(qkv, (GDN_QK_W, GDN_QK_W, GDN_V_W))
    q = l2norm(q.reshape(B, S, GDN_HEADS, GDN_DK)) * GDN_DK ** -0.5
    k = l2norm(k.reshape(B, S, GDN_HEADS, GDN_DK))
    v = v.reshape(B, S, GDN_HEADS, GDN_DV)
    beta = jax.nn.sigmoid(b)
    g = -jnp.exp(a_log) * jax.nn.softplus(a + dt_bias)
    o_a = gated_delta_rule_chunked(q, k, v, g, beta)
    o_a = rmsnorm(o_a, gdn_norm) * jax.nn.silu(z.reshape(B, S, GDN_HEADS, GDN_DV))
    rq = apply_rope(rq.reshape(B, S, RET_HEADS, RET_DK), cos, sin) * RET_DK ** -0.5
    rk = apply_rope(rk.reshape(B, S, RET_HEADS, RET_DK), cos, sin)
    o_b = retention_chunked(rq, rk, rv.reshape(B, S, RET_HEADS, RET_DV))
    o_b = rmsnorm(o_b) * jax.nn.silu(rg.reshape(B, S, RET_HEADS, RET_DV))
    o = jnp.concatenate([o_a.reshape(B, S, GDN_V_W), o_b.reshape(B, S, RET_V_W)], axis=-1)
    return o @ w_out


def mixer_dilated_differential(h, w_in, lambda_q1, lambda_k1, lambda_q2, lambda_k2, diff_norm, w_out,
                               cos128, sin128, cos64, sin64, lambda_init):
    B, S, _ = h.shape
    cq, ck, cv, dq, dk, dv = split_cols(h @ w_in, IN1_SPLITS)
    cq = apply_rope(cq.reshape(B, S, DIL_HEADS, DIL_DH), cos128, sin128) * DIL_DH ** -0.5
    ck = apply_rope(ck.reshape(B, S, DIL_HEADS, DIL_DH), cos128, sin128)
    o_c = dilated_attention(cq, ck, cv.reshape(B, S, DIL_HEADS, DIL_DH))
    dq = dq.reshape(B, S, DIFF_HEADS, 2, DIFF_DK)
    dk = dk.reshape(B, S, DIFF_HEADS, 2, DIFF_DK)
    q1 = apply_rope(dq[..., 0, :], cos64, sin64) * DIFF_DK ** -0.5
    q2 = apply_rope(dq[..., 1, :], cos64, sin64) * DIFF_DK ** -0.5
    k1 = apply_rope(dk[..., 0, :], cos64, sin64)
    k2 = apply_rope(dk[..., 1, :], cos64, sin64)
    lam = (jnp.exp(jnp.sum(lambda_q1 * lambda_k1).astype(jnp.float32))
           - jnp.exp(jnp.sum(lambda_q2 * lambda_k2).astype(jnp.float32)) + lambda_init)
    o_d = differential_attention(q1, q2, k1, k2, dv.reshape(B, S, DIFF_HEADS, DIFF_DV), lam)
    o_d = rmsnorm(o_d, diff_norm) * (1.0 - lambda_init)
    o = jnp.concatenate([o_c.reshape(B, S, DIL_W), o_d.reshape(B, S, DIFF_V_W)], axis=-1)
    return o @ w_out


def setup_inputs(seed: int = 0) -> dict:
    key = jax.random.key(seed)
    ks = iter(jax.random.split(key, 64))

    def nrm(shape, scale):
        return jax.random.normal(next(ks), shape, jnp.float32) * scale

    def gain(n):
        return 1.0 + 0.02 * jax.random.normal(next(ks), (n,), jnp.float32)

    inp = {}
    inp['x'] = nrm((BATCH, SEQ, D_MODEL), 1.0)

    def ffn(prefix):
        inp[prefix + '_norm'] = gain(D_MODEL)
        inp[prefix + '_w_up'] = nrm((D_MODEL, 2 * D_FF), D_MODEL ** -0.5)
        inp[prefix + '_w_down'] = nrm((D_FF, D_MODEL), D_FF ** -0.5)

    ffn('l0_ffn1')
    inp['l0_mix_norm'] = gain(D_MODEL)
    inp['l0_w_in'] = nrm((D_MODEL, IN0_W), D_MODEL ** -0.5)
    inp['l0_conv_w'] = nrm((GDN_CONV, GDN_CONV_W), GDN_CONV ** -0.5)
    inp['l0_a_log'] = jnp.log(jax.random.uniform(next(ks), (GDN_HEADS,), jnp.float32, 1.0, 16.0))
    dt = jnp.exp(jax.random.uniform(next(ks), (GDN_HEADS,), jnp.float32, math.log(1e-3), math.log(1e-1)))
    inp['l0_dt_bias'] = dt + jnp.log(-jnp.expm1(-dt))
    inp['l0_gdn_norm'] = gain(GDN_DV)
    inp['l0_w_out'] = nrm((MIX0_W, D_MODEL), MIX0_W ** -0.5)
    ffn('l0_ffn2')
    ffn('l1_ffn1')
    inp['l1_mix_norm'] = gain(D_MODEL)
    inp['l1_w_in'] = nrm((D_MODEL, IN1_W), D_MODEL ** -0.5)
    inp['l1_lambda_q1'] = nrm((DIFF_DK,), 0.1)
    inp['l1_lambda_k1'] = nrm((DIFF_DK,), 0.1)
    inp['l1_lambda_q2'] = nrm((DIFF_DK,), 0.1)
    inp['l1_lambda_k2'] = nrm((DIFF_DK,), 0.1)
    inp['l1_diff_norm'] = gain(DIFF_DV)
    inp['l1_w_out'] = nrm((MIX1_W, D_MODEL), MIX1_W ** -0.5)
    ffn('l1_ffn2')
    inp['final_norm'] = gain(D_MODEL)
    return inp


def reference(x, l0_ffn1_norm, l0_ffn1_w_up, l0_ffn1_w_down, l0_mix_norm, l0_w_in, l0_conv_w, l0_a_log,
              l0_dt_bias, l0_gdn_norm, l0_w_out, l0_ffn2_norm, l0_ffn2_w_up, l0_ffn2_w_down,
              l1_ffn1_norm, l1_ffn1_w_up, l1_ffn1_w_down, l1_mix_norm, l1_w_in, l1_lambda_q1, l1_lambda_k1,
              l1_lambda_q2, l1_lambda_k2, l1_diff_norm, l1_w_out, l1_ffn2_norm, l1_ffn2_w_up, l1_ffn2_w_down,
              final_norm):
    S = x.shape[1]
    cos64, sin64 = rope_tables(S, 64)
    cos128, sin128 = rope_tables(S, 128)
    ffn1 = ((l0_ffn1_norm, l0_ffn1_w_up, l0_ffn1_w_down), (l1_ffn1_norm, l1_ffn1_w_up, l1_ffn1_w_down))
    ffn2 = ((l0_ffn2_norm, l0_ffn2_w_up, l0_ffn2_w_down), (l1_ffn2_norm, l1_ffn2_w_up, l1_ffn2_w_down))
    mix_norm = (l0_mix_norm, l1_mix_norm)
    for i in range(DEPTH):
        n1, wu1, wd1 = ffn1[i]
        x = x + 0.5 * swiglu(rmsnorm(x, n1), wu1, wd1)
        h = rmsnorm(x, mix_norm[i])
        if i % 2 == 0:
            mix = mixer_delta_retention(h, l0_w_in, l0_conv_w, l0_a_log, l0_dt_bias, l0_gdn_norm, l0_w_out,
                                        cos64, sin64)
        else:
            lambda_init = 0.8 - 0.6 * math.exp(-0.3 * i)
            mix = mixer_dilated_differential(h, l1_w_in, l1_lambda_q1, l1_lambda_k1, l1_lambda_q2, l1_lambda_k2,
                                             l1_diff_norm, l1_w_out, cos128, sin128, cos64, sin64, lambda_init)
        x = x + mix
        n2, wu2, wd2 = ffn2[i]
        x = x + 0.5 * swiglu(rmsnorm(x, n2), wu2, wd2)
    return rmsnorm(x, final_norm)
```

```python
from contextlib import ExitStack
import math
import numpy as np
import ml_dtypes
import concourse.bass as bass
import concourse.mybir as mybir
from concourse.bass_utils import run_bass_kernel_spmd

F32 = mybir.dt.float32
BF16 = mybir.dt.bfloat16
AF = mybir.ActivationFunctionType
ALU = mybir.AluOpType
AX = mybir.AxisListType

D = 1024
DFF = 2816
EPS = 1e-6
SBUF_BYTES = 196608 - 2048
EPOCH = 16000
ENGS = ("pe", "act", "dve", "pool", "sp")


class _Rec:
    def __init__(self):
        self.call = None

    def __getattr__(self, name):
        def f(*a, **k):
            self.call = (name, a, k)
            return self
        return f


class Prog:
    def __init__(self, nc, stack):
        self.nc = nc
        self.stack = stack
        self.streams = {e: [] for e in ENGS}
        self.cnt = {e: 0 for e in ENGS}
        self.esem = {e: None for e in ENGS}
        self.ebase = {e: 0 for e in ENGS}
        self.lastw = {}
        self.readers = {}
        self.known = {e: {} for e in ENGS}
        self.dsems = {}
        self.nsem = 0
        self.pending = {e: [] for e in ENGS}
        self.sb = None
        self.sb_off = 0
        self.sb_persist = 0
        self.latest = {}

    def newsem(self, name):
        self.nsem += 1
        return self.stack.enter_context(self.nc.semaphore("s%d_%s" % (self.nsem, name)))

    def init_sbuf(self):
        self.sb = self.stack.enter_context(self.nc.sbuf_tensor("sbuf_all", [128, SBUF_BYTES // 4], F32))

    def alloc(self, cols, dtype, parts=128):
        nbytes = cols * (4 if dtype == F32 else 2)
        nbytes = (nbytes + 63) // 64 * 64
        assert self.sb_off + nbytes <= SBUF_BYTES, ("SBUF overflow", self.sb_off, nbytes)
        a = self.sb[0:parts, self.sb_off // 4:(self.sb_off + nbytes) // 4]
        self.sb_off += nbytes
        if dtype != F32:
            a = a.bitcast(dtype)
        return a[:, 0:cols]

    def mark_persistent(self):
        self.sb_persist = self.sb_off

    def reset_phase(self):
        self.sb_off = self.sb_persist

    def _event(self, eng):
        if self.esem[eng] is None or self.cnt[eng] - self.ebase[eng] >= EPOCH:
            self.esem[eng] = self.newsem(eng)
            self.ebase[eng] = self.cnt[eng]
        self.cnt[eng] += 1
        return (self.esem[eng], self.cnt[eng] - self.ebase[eng], eng)

    def op(self, eng, fn, reads=(), writes=(), dsem=None):
        deps = list(self.pending[eng])
        self.pending[eng] = []
        for k in reads:
            ev = self.lastw.get(k)
            if ev is not None:
                deps.append(ev)
        for k in writes:
            ev = self.lastw.get(k)
            if ev is not None:
                deps.append(ev)
            deps.extend(self.readers.get(k, {}).values())
        waits = {}
        kn = self.known[eng]
        for (sem, val, peng) in deps:
            if peng == eng and eng == "pe":
                continue
            if kn.get(id(sem), 0) >= val:
                continue
            if waits.get(id(sem), (None, 0))[1] < val:
                waits[id(sem)] = (sem, val)
        for sid, (sem, val) in waits.items():
            kn[sid] = val
        if fn is None:
            self.streams[eng].append((list(waits.values()), None, None))
            return None
        rec = _Rec()
        fn(rec)
        fn = rec.call
        if dsem is None:
            ev = self._event(eng)
            inc = (ev[0], 1)
        else:
            if dsem not in self.dsems:
                self.dsems[dsem] = [self.newsem("d"), 0]
            d = self.dsems[dsem]
            d[1] += 16
            ev = (d[0], d[1], "dma")
            inc = (d[0], 16)
        self.streams[eng].append((list(waits.values()), fn, inc))
        self.latest[id(ev[0])] = ev
        for k in writes:
            self.lastw[k] = ev
            self.readers[k] = {}
        for k in reads:
            if k in writes:
                continue
            self.readers.setdefault(k, {})[id(ev[0])] = ev
        return ev

    def barrier(self):
        evs = list(self.latest.values())
        for e in ENGS:
            self.pending[e] = list(evs)

    def emit(self):
        with self.nc.Block() as block:
            decos = {"pe": block.tensor, "act": block.scalar, "dve": block.vector,
                     "pool": block.gpsimd, "sp": block.sync}
            for eng in ENGS:
                stream = self.streams[eng]

                def body(e, stream=stream):
                    for waits, fn, inc in stream:
                        for sem, val in waits:
                            e.wait_ge(sem, val)
                        if fn is not None:
                            getattr(e, fn[0])(*fn[1], **fn[2]).then_inc(inc[0], inc[1])

                decos[eng](body)


def v3(ap, a):
    return ap.rearrange("p (a b) -> p a b", a=a)


def bcast_rows(vec_ap, n, parts=128):
    return vec_ap.rearrange("(o n) -> o n", o=1).broadcast_to([parts, n])


class Ctx:
    pass


def setup_common(pr, c):
    nc = pr.nc
    c.ps = [pr.stack.enter_context(nc.psum_tensor("ps%d" % i, [128, 512], F32)) for i in range(8)]
    c.ident_f = pr.alloc(128, F32)
    c.ident_b = pr.alloc(128, BF16)
    c.mhalf = pr.alloc(1, F32)
    c.ones_b = pr.alloc(128, BF16)
    c.ones_f = pr.alloc(128, F32)
    pr.op("sp", lambda e: e.dma_start(out=c.ident_f, in_=c.dram["ident"][:, :]), writes=["ident_f"], dsem="const")
    pr.op("dve", lambda e: e.tensor_copy(out=c.ident_b, in_=c.ident_f), reads=["ident_f"], writes=["ident_b"])
    pr.op("pool", lambda e: e.memset(c.mhalf, -0.5), writes=["mhalf"])
    pr.op("pool", lambda e: e.memset(c.ones_b, 1.0), writes=["ones_b"])
    pr.op("pool", lambda e: e.memset(c.ones_f, 1.0), writes=["ones_f"])
    pr.mark_persistent()


def rms_rows(pr, c, x_ap, xkey, out_ap, outkey, wn, wnkey, junk, ss, tag):
    pr.op("dve", lambda e: e.scalar_tensor_tensor(out=junk, in0=x_ap, scalar=1.0, in1=x_ap,
                                                  op0=ALU.mult, op1=ALU.mult, accum_out=ss[:, 0:1]),
          reads=[xkey], writes=["junk" + tag, "ss" + tag])
    pr.op("dve", lambda e: e.tensor_scalar(out=ss[:, 1:2], in0=ss[:, 0:1], scalar1=1.0 / x_ap.shape[-1], scalar2=EPS,
                                           op0=ALU.mult, op1=ALU.add),
          reads=["ss" + tag], writes=["ms" + tag])
    pr.op("pool", lambda e: e.tensor_tensor(out=ss[:, 2:3], in0=ss[:, 1:2], in1=c.mhalf, op=ALU.pow),
          reads=["ms" + tag, "mhalf"], writes=["rstd" + tag])
    if wn is not None:
        pr.op("dve", lambda e: e.scalar_tensor_tensor(out=out_ap, in0=x_ap, scalar=ss[:, 2:3], in1=wn,
                                                      op0=ALU.mult, op1=ALU.mult),
              reads=[xkey, "rstd" + tag, wnkey], writes=[outkey])
    else:
        pr.op("dve", lambda e: e.tensor_scalar(out=out_ap, in0=x_ap, scalar1=ss[:, 2:3], scalar2=None,
                                               op0=ALU.mult),
              reads=[xkey, "rstd" + tag], writes=[outkey])


def load_weight_bf16(pr, dst3, src2, nk, ncols, key, dsem, colchunk=1024):
    for k in range(nk):
        c0 = 0
        while c0 < ncols:
            w = min(colchunk, ncols - c0)
            pr.op("pool", lambda e, k=k, c0=c0, w=w: e.dma_start(out=dst3[:, k, c0:c0 + w],
                                                                  in_=src2[k * 128:(k + 1) * 128, c0:c0 + w]),
                  writes=[key], dsem=dsem)
            c0 += w


def load_weight_chunked(pr, dst3, src2, nk, ncols, key, dsem, cw, order=None):
    ncc = (ncols + cw - 1) // cw
    for cc in (order if order is not None else range(ncc)):
        c0 = cc * cw
        w = min(cw, ncols - c0)
        for k in range(nk):
            pr.op("pool", lambda e, k=k, c0=c0, w=w: e.dma_start(out=dst3[:, k, c0:c0 + w],
                                                                  in_=src2[k * 128:(k + 1) * 128, c0:c0 + w]),
                  writes=[(key, cc)], dsem=(dsem, cc))


def wkeys(key, c0, w, cw):
    return [(key, cc) for cc in range(c0 // cw, (c0 + w - 1) // cw + 1)]


def ffn_phase(pr, c, tag, src, dst, wup, wdn, nrm, S, fin=None, out_final=None):
    pr.barrier()
    pr.reset_phase()
    G = 256
    NG = S // G
    NF = DFF // 128
    wup_sb = v3(pr.alloc(8 * 2 * DFF, BF16), 8)
    wdn_sb = v3(pr.alloc(NF * D, BF16), NF)
    xs = [v3(pr.alloc(2 * D, F32), 2) for _ in range(2)]
    hb = [pr.alloc(D, BF16) for _ in range(2)]
    hT = [v3(pr.alloc(8 * G, BF16), 8) for _ in range(2)]
    actT = v3(pr.alloc(NF * G, BF16), NF)
    sg = [pr.alloc(G, F32) for _ in range(2)]
    junk = pr.alloc(D, BF16)
    wn = pr.alloc(D, F32)
    ss = pr.alloc(4, F32)
    if fin is not None:
        wfin = pr.alloc(D, F32)
        pr.op("sp", lambda e: e.dma_start(out=wfin, in_=bcast_rows(fin, D)), writes=["wfin"], dsem="wn2")
    pr.op("sp", lambda e: e.dma_start(out=wn, in_=bcast_rows(nrm, D)), writes=["wn"], dsem="wn")
    load_weight_chunked(pr, wup_sb, wup, 8, 2 * DFF, "wup", "wup", 704, order=[0, 4, 1, 5, 2, 6, 3, 7])
    load_weight_bf16(pr, wdn_sb, wdn, NF, D, "wdn", "wdn")
    pst = c.ps[0][:, :].bitcast(BF16)
    psg = [c.ps[1], c.ps[2]]
    psu = [c.ps[3], c.ps[4]]
    psd = [c.ps[5], c.ps[6]]

    def load(g):
        s = g % 2
        pr.op("sp", lambda e: e.dma_start(out=xs[s], in_=src[g * G:(g + 1) * G, :].rearrange("(n p) d -> p n d", p=128)),
              reads=[("xd", g)], writes=[("x", s, 0), ("x", s, 1)], dsem=("xl", s))

    def prep_dve(g):
        s = g % 2
        for t in range(2):
            rms_rows(pr, c, xs[s][:, t, :], ("x", s, t), hb[t], ("hb", t), wn, "wn", junk, ss, "f")

    def prep_pe(g):
        s = g % 2
        for t in range(2):
            for k in range(8):
                pr.op("pe", lambda e, t=t, k=k: e.transpose(out=pst[:, k * 128:(k + 1) * 128],
                                                            in_=hb[t][:, k * 128:(k + 1) * 128], identity=c.ident_b),
                      reads=[("hb", t), "ident_b"], writes=["pst"])
            pr.op("act", lambda e, t=t, s=s: e.activation(out=hT[s][:, :, t * 128:(t + 1) * 128], in_=v3(pst, 8), func=AF.Copy),
                  reads=["pst"], writes=[("hT", s)])

    def up(g):
        s = g % 2
        for f in range(NF):
            b = f % 2
            for k in range(8):
                pr.op("pe", lambda e, f=f, k=k, b=b: e.matmul(psg[b][:, 0:G], lhsT=wup_sb[:, k, f * 128:(f + 1) * 128],
                                                              rhs=hT[s][:, k, :], start=(k == 0), stop=(k == 7)),
                      reads=wkeys("wup", f * 128, 128, 704) + [("hT", s)], writes=[("psg", b)])
            for k in range(8):
                pr.op("pe", lambda e, f=f, k=k, b=b: e.matmul(psu[b][:, 0:G], lhsT=wup_sb[:, k, DFF + f * 128:DFF + (f + 1) * 128],
                                                              rhs=hT[s][:, k, :], start=(k == 0), stop=(k == 7)),
                      reads=wkeys("wup", DFF + f * 128, 128, 704) + [("hT", s)], writes=[("psu", b)])
            pr.op("act", lambda e, b=b: e.activation(out=sg[b], in_=psg[b][:, 0:G], func=AF.Silu),
                  reads=[("psg", b)], writes=[("sg", b)])
            pr.op("dve", lambda e, f=f, b=b: e.tensor_tensor(out=actT[:, f, :], in0=sg[b], in1=psu[b][:, 0:G], op=ALU.mult),
                  reads=[("sg", b), ("psu", b)], writes=["actT"])

    def down(g):
        s = g % 2
        for t in range(2):
            for hf in range(2):
                b = hf
                for f in range(NF):
                    pr.op("pe", lambda e, f=f, t=t, hf=hf, b=b: e.matmul(psd[b][:, :], lhsT=actT[:, f, t * 128:(t + 1) * 128],
                                                                          rhs=wdn_sb[:, f, hf * 512:(hf + 1) * 512],
                                                                          start=(f == 0), stop=(f == NF - 1)),
                          reads=["actT", "wdn"], writes=[("psd", b)])
                pr.op("dve", lambda e, t=t, hf=hf, b=b, s=s: e.scalar_tensor_tensor(
                    out=xs[s][:, t, hf * 512:(hf + 1) * 512], in0=psd[b][:, :], scalar=0.5,
                    in1=xs[s][:, t, hf * 512:(hf + 1) * 512], op0=ALU.mult, op1=ALU.add),
                      reads=[("psd", b), ("x", s, t)], writes=[("x", s, t)])
            if fin is not None:
                rms_rows(pr, c, xs[s][:, t, :], ("x", s, t), xs[s][:, t, :], ("x", s, t), wfin, "wfin", junk, ss, "f")
        tgt = dst if fin is None else out_final
        pr.op("sp", lambda e: e.dma_start(out=tgt[g * G:(g + 1) * G, :].rearrange("(n p) d -> p n d", p=128), in_=xs[s]),
              reads=[("x", s, 0), ("x", s, 1)], writes=[("xd", g)], dsem=("xst", s))

    load(0)
    prep_dve(0)
    prep_pe(0)
    for g in range(NG):
        if g + 1 < NG:
            load(g + 1)
            prep_dve(g + 1)
        up(g)
        if g + 1 < NG:
            prep_pe(g + 1)
        down(g)


def proj_phase(pr, c, S, src, w_dram, ncols, nrm, fm_jobs, tm_jobs, fmT, tm_out, tabs, tm32_job=None, tm32_out=None):
    pr.barrier()
    pr.reset_phase()
    G = 256
    NG = S // G
    nch = len(fm_jobs)
    ntab = 0 if tabs is None else tabs.shape[0]
    w_sb = v3(pr.alloc(8 * ncols, BF16), 8)
    xs = [v3(pr.alloc(2 * D, F32), 2) for _ in range(2)]
    hb = [pr.alloc(D, BF16) for _ in range(2)]
    hT = [v3(pr.alloc(8 * G, BF16), 8) for _ in range(2)]
    junk = pr.alloc(D, BF16)
    wn = pr.alloc(D, F32)
    ss = pr.alloc(4, F32)
    stage = [v3(pr.alloc(nch * G, BF16), nch) for _ in range(2)]
    ntm = len(tm_jobs)
    tstage = [v3(pr.alloc(2 * max(ntm, 1) * 512, BF16), 2) for _ in range(2)]
    t32 = [v3(pr.alloc(2 * 8, F32), 2) for _ in range(2)]
    tab = [v3(pr.alloc(max(ntab, 1) * 2 * G, F32), max(ntab, 1) * 2) for _ in range(2)]
    t1 = [pr.alloc(G, F32) for _ in range(2)]
    t2 = [pr.alloc(G, F32) for _ in range(2)]
    pr.op("sp", lambda e: e.dma_start(out=wn, in_=bcast_rows(nrm, D)), writes=["wn"], dsem="wn")
    load_weight_chunked(pr, w_sb, w_dram, 8, ncols, "w_in", "w_in", 512)
    pst = c.ps[0][:, :].bitcast(BF16)
    pA = [c.ps[1], c.ps[2]]
    pB = [c.ps[3], c.ps[4]]
    pT = [c.ps[5], c.ps[6]]

    def load(g):
        s = g % 2
        pr.op("sp", lambda e: e.dma_start(out=xs[s], in_=src[g * G:(g + 1) * G, :].rearrange("(n p) d -> p n d", p=128)),
              reads=[("xd", g)], writes=[("x", s, 0), ("x", s, 1)], dsem=("xl", s))
        if ntab:
            pr.op("sp", lambda e: e.dma_start(out=tab[s], in_=tabs.rearrange("t two p s -> p (t two) s")[:, :, g * G:(g + 1) * G]),
                  writes=[("tab", s)], dsem=("tabl", s))

    def prep(g):
        s = g % 2
        for t in range(2):
            rms_rows(pr, c, xs[s][:, t, :], ("x", s, t), hb[t], ("hb", t), wn, "wn", junk, ss, "f")
        for t in range(2):
            for k in range(8):
                pr.op("pe", lambda e, t=t, k=k: e.transpose(out=pst[:, k * 128:(k + 1) * 128],
                                                            in_=hb[t][:, k * 128:(k + 1) * 128], identity=c.ident_b),
                      reads=[("hb", t), "ident_b"], writes=["pst"])
            pr.op("act", lambda e, t=t, s=s: e.activation(out=hT[s][:, :, t * 128:(t + 1) * 128], in_=v3(pst, 8), func=AF.Copy),
                  reads=["pst"], writes=[("hT", s)])

    def mm_fm(ps, col0, s, key):
        for k in range(8):
            pr.op("pe", lambda e, k=k: e.matmul(ps[:, 0:G], lhsT=w_sb[:, k, col0:col0 + 128], rhs=hT[s][:, k, :],
                                                start=(k == 0), stop=(k == 7)),
                  reads=wkeys("w_in", col0, 128, 512) + [("hT", s)], writes=[key])

    def compute(g):
        s = g % 2
        for j, (col0, pcol0, ti) in enumerate(fm_jobs):
            b = j % 2
            mm_fm(pA[b], col0, s, ("pA", b))
            if pcol0 is None:
                pr.op("act", lambda e, j=j, b=b: e.activation(out=stage[s][:, j, :], in_=pA[b][:, 0:G], func=AF.Copy),
                      reads=[("pA", b)], writes=[("stage", s)])
            else:
                mm_fm(pB[b], pcol0, s, ("pB", b))
                pr.op("dve", lambda e, b=b, ti=ti: e.tensor_tensor(out=t1[b], in0=pA[b][:, 0:G], in1=tab[s][:, 2 * ti, :], op=ALU.mult),
                      reads=[("pA", b), ("tab", s)], writes=[("t1", b)])
                pr.op("dve", lambda e, b=b, ti=ti: e.tensor_tensor(out=t2[b], in0=pB[b][:, 0:G], in1=tab[s][:, 2 * ti + 1, :], op=ALU.mult),
                      reads=[("pB", b), ("tab", s)], writes=[("t2", b)])
                pr.op("pool", lambda e, b=b, j=j: e.tensor_tensor(out=stage[s][:, j, :], in0=t1[b], in1=t2[b], op=ALU.add),
                      reads=[("t1", b), ("t2", b)], writes=[("stage", s)])
        pr.op("sp", lambda e: e.dma_start(out=fmT.rearrange("c p s -> p c s")[:, :, g * G:(g + 1) * G], in_=stage[s]),
              reads=[("stage", s)], writes=[("fmT", g)], dsem=("fst", s))
        for t in range(2):
            for i, col0 in enumerate(tm_jobs):
                b = i % 2
                for k in range(8):
                    pr.op("pe", lambda e, k=k, t=t, col0=col0, b=b: e.matmul(pT[b][:, :], lhsT=hT[s][:, k, t * 128:(t + 1) * 128],
                                                                              rhs=w_sb[:, k, col0:col0 + 512],
                                                                              start=(k == 0), stop=(k == 7)),
                          reads=wkeys("w_in", col0, 512, 512) + [("hT", s)], writes=[("pT", b)])
                pr.op("act", lambda e, t=t, i=i, b=b: e.activation(out=tstage[s][:, t, i * 512:(i + 1) * 512], in_=pT[b][:, :], func=AF.Copy),
                      reads=[("pT", b)], writes=[("tstage", s)])
            if tm32_job is not None:
                col0, n = tm32_job
                for k in range(8):
                    pr.op("pe", lambda e, k=k, t=t: e.matmul(pT[0][:, 0:n], lhsT=hT[s][:, k, t * 128:(t + 1) * 128],
                                                              rhs=w_sb[:, k, col0:col0 + n], start=(k == 0), stop=(k == 7)),
                          reads=wkeys("w_in", col0, n, 512) + [("hT", s)], writes=[("pT", 0)])
                pr.op("act", lambda e, t=t: e.activation(out=t32[s][:, t, 0:n], in_=pT[0][:, 0:n], func=AF.Copy),
                      reads=[("pT", 0)], writes=[("t32", s)])
        if ntm:
            pr.op("sp", lambda e: e.dma_start(out=tm_out[g * G:(g + 1) * G, :].rearrange("(n p) d -> p n d", p=128), in_=tstage[s]),
                  reads=[("tstage", s)], writes=[("tm", g)], dsem=("tst", s))
        if tm32_job is not None:
            n = tm32_job[1]
            pr.op("sp", lambda e: e.dma_start(out=tm32_out[g * G:(g + 1) * G, :].rearrange("(n p) d -> p n d", p=128),
                                              in_=t32[s][:, :, 0:n]),
                  reads=[("t32", s)], writes=[("tm32", g)], dsem=("t32st", s))

    load(0)
    prep(0)
    for g in range(NG):
        if g + 1 < NG:
            load(g + 1)
        compute(g)
        if g + 1 < NG:
            prep(g + 1)


def outproj_phase(pr, c, S, oT, w_out, src, dst):
    pr.barrier()
    pr.reset_phase()
    G = 256
    NG = S // G
    w_sb = v3(pr.alloc(8 * D, BF16), 8)
    load_weight_bf16(pr, w_sb, w_out, 8, D, "w_out", "w_out")
    xs = [v3(pr.alloc(2 * D, F32), 2) for _ in range(2)]
    ot = [v3(pr.alloc(8 * G, BF16), 8) for _ in range(2)]
    pO = [c.ps[1], c.ps[2]]

    def load(g):
        s = g % 2
        pr.op("sp", lambda e: e.dma_start(out=xs[s], in_=src[g * G:(g + 1) * G, :].rearrange("(n p) d -> p n d", p=128)),
              reads=[("xd", g)], writes=[("x", s)], dsem=("xl", s))
        pr.op("sp", lambda e: e.dma_start(out=ot[s], in_=oT.rearrange("c p s -> p c s")[:, :, g * G:(g + 1) * G]),
              reads=["oT"], writes=[("ot", s)], dsem=("otl", s))

    load(0)
    for g in range(NG):
        s = g % 2
        if g + 1 < NG:
            load(g + 1)
        for t in range(2):
            for hf in range(2):
                for k in range(8):
                    pr.op("pe", lambda e, k=k, t=t, hf=hf: e.matmul(pO[hf][:, :], lhsT=ot[s][:, k, t * 128:(t + 1) * 128],
                                                                    rhs=w_sb[:, k, hf * 512:(hf + 1) * 512],
                                                                    start=(k == 0), stop=(k == 7)),
                          reads=["w_out", ("ot", s)], writes=[("pO", hf)])
                pr.op("dve", lambda e, t=t, hf=hf: e.tensor_tensor(out=xs[s][:, t, hf * 512:(hf + 1) * 512], in0=pO[hf][:, :],
                                                                   in1=xs[s][:, t, hf * 512:(hf + 1) * 512], op=ALU.add),
                      reads=[("pO", hf), ("x", s)], writes=[("x", s)])
        pr.op("sp", lambda e: e.dma_start(out=dst[g * G:(g + 1) * G, :].rearrange("(n p) d -> p n d", p=128), in_=xs[s]),
              reads=[("x", s)], writes=[("xd", g)], dsem=("xst", s))


def pipeline(fronts, backs, look):
    n = len(fronts)
    for i in range(n + look):
        if i < n:
            fronts[i]()
        if i >= look:
            backs[i - look]()


def col_maxnorm(pr, c, T, key, rows, S, sq, psb, out_max, tagk):
    r0, r1 = rows
    first = True
    for g in range(S // 512):
        pr.op("pool", lambda e, g=g: e.tensor_tensor(out=sq[r0:r1, :], in0=T[r0:r1, g * 512:(g + 1) * 512],
                                                     in1=T[r0:r1, g * 512:(g + 1) * 512], op=ALU.mult),
              reads=[key], writes=["sq"])
        pr.op("pe", lambda e: e.matmul(psb[:, :], lhsT=c.ones_b[r0:r1, :], rhs=sq[r0:r1, :], start=True, stop=True),
              reads=["sq", "ones_b"], writes=["psb"])
        if first:
            pr.op("dve", lambda e: e.tensor_reduce(out=out_max, in_=psb[:, :], axis=AX.X, op=ALU.max),
                  reads=["psb"], writes=[tagk])
            first = False
        else:
            pr.op("dve", lambda e: e.tensor_reduce(out=c.tmpmax, in_=psb[:, :], axis=AX.X, op=ALU.max),
                  reads=["psb"], writes=["tmpmax"])
            pr.op("dve", lambda e: e.tensor_tensor(out=out_max, in0=out_max, in1=c.tmpmax, op=ALU.max),
                  reads=["tmpmax", tagk], writes=[tagk])


def neg_bound(pr, c, mq, mk, out_negM, keys, outkey):
    pr.op("dve", lambda e: e.tensor_tensor(out=c.tmpmax, in0=mq, in1=mk, op=ALU.mult), reads=keys, writes=["tmpmax"])
    pr.op("pool", lambda e: e.tensor_tensor(out=c.tmpmax, in0=c.tmpmax, in1=c.phalf, op=ALU.pow), reads=["tmpmax", "phalf"], writes=["tmpmax"])
    pr.op("dve", lambda e: e.tensor_scalar(out=out_negM, in0=c.tmpmax, scalar1=-1.0, scalar2=None, op0=ALU.mult),
          reads=["tmpmax"], writes=[outkey])


def fm_epilogue(pr, c, o_ap, okey, N, gate_ap, gatekey, wcol, wkey, sqb, psb, rs, out_ap, outkey, const_scale=1.0, psbkey="psb"):
    pr.op("pool", lambda e: e.tensor_tensor(out=sqb[:, 0:N], in0=o_ap, in1=o_ap, op=ALU.mult), reads=[okey], writes=["sqb"])
    pr.op("pe", lambda e: e.matmul(psb[:, 0:N], lhsT=c.ones_b, rhs=sqb[:, 0:N], start=True, stop=True),
          reads=["sqb", "ones_b"], writes=[psbkey])
    pr.op("dve", lambda e: e.tensor_scalar(out=rs[:, 0:N], in0=psb[:, 0:N], scalar1=1.0 / 128.0, scalar2=EPS, op0=ALU.mult, op1=ALU.add),
          reads=[psbkey], writes=["rs"])
    pr.op("act", lambda e: e.activation(out=rs[:, 0:N], in_=rs[:, 0:N], func=AF.Ln), reads=["rs"], writes=["rs"])
    pr.op("act", lambda e: e.activation(out=rs[:, 0:N], in_=rs[:, 0:N], func=AF.Exp, scale=-0.5), reads=["rs"], writes=["rs"])
    if gate_ap is None:
        if wcol is None:
            pr.op("dve", lambda e: e.tensor_tensor(out=out_ap, in0=o_ap, in1=rs[:, 0:N], op=ALU.mult),
                  reads=[okey, "rs"], writes=[outkey])
        else:
            pr.op("dve", lambda e: e.scalar_tensor_tensor(out=out_ap, in0=o_ap, scalar=wcol, in1=rs[:, 0:N], op0=ALU.mult, op1=ALU.mult),
                  reads=[okey, "rs", wkey], writes=[outkey])
    else:
        if wcol is None:
            pr.op("dve", lambda e: e.tensor_tensor(out=rs[:, 0:N], in0=o_ap, in1=rs[:, 0:N], op=ALU.mult),
                  reads=[okey, "rs"], writes=["rs"])
        else:
            pr.op("dve", lambda e: e.scalar_tensor_tensor(out=rs[:, 0:N], in0=o_ap, scalar=wcol, in1=rs[:, 0:N], op0=ALU.mult, op1=ALU.mult),
                  reads=[okey, "rs", wkey], writes=["rs"])
        pr.op("dve", lambda e: e.tensor_tensor(out=out_ap, in0=rs[:, 0:N], in1=gate_ap, op=ALU.mult),
              reads=["rs", gatekey], writes=[outkey])


def l1_attn_phase(pr, c, S, fmT, tm, oT, lam_dram, dnorm_dram, lambda_init):
    pr.barrier()
    pr.reset_phase()
    NB = S // 128
    QT = pr.alloc(S, BF16)
    KT = pr.alloc(S, BF16)
    Vp = v3(pr.alloc(NB * 128, BF16), NB)
    Vp3 = [Vp, v3(pr.alloc(NB * 128, BF16), NB), v3(pr.alloc(NB * 128, BF16), NB)]
    num = pr.alloc(S, F32)
    den = pr.alloc(S, F32)
    pTs = [pr.alloc(512, BF16) for _ in range(3)]
    sq = pr.alloc(512, BF16)
    sqb = pr.alloc(512, BF16)
    rs = pr.alloc(512, F32)
    o1s = [pr.alloc(512, F32) for _ in range(2)]
    o2 = pr.alloc(512, F32)
    rs2 = pr.alloc(512, F32)
    ost = [pr.alloc(512, BF16) for _ in range(2)]
    mask2 = pr.alloc(256, BF16)
    maskd = v3(pr.alloc(4 * 512, BF16), 4)
    small = pr.alloc(16, F32)
    c.tmpmax = small[:, 0:1]
    c.phalf = small[:, 1:2]
    mq = small[:, 2:3]
    mk = small[:, 3:4]
    negM = small[:, 4:5]
    lamneg = small[:, 5:6]
    dnorm = small[:, 6:7]
    lamt = pr.alloc(256, F32)
    pr.op("pool", lambda e: e.memset(c.phalf, 0.5), writes=["phalf"])
    pr.op("pool", lambda e: e.memset(mask2, 1.0), writes=["mask2"])
    pr.op("pool", lambda e: e.affine_select(out=mask2[:, 0:128], in_=mask2[:, 0:128], pattern=[[-1, 128]], compare_op=ALU.is_ge,
                                            fill=0.0, base=0, channel_multiplier=1), reads=["mask2"], writes=["mask2"])
    pr.op("pool", lambda e: e.affine_select(out=mask2[:, 128:256], in_=mask2[:, 128:256], pattern=[[1, 128]], compare_op=ALU.is_ge,
                                            fill=0.0, base=0, channel_multiplier=-1), reads=["mask2"], writes=["mask2"])
    pr.op("pool", lambda e: e.memset(maskd, 1.0), writes=["maskd"])
    for dd in range(4):
        pr.op("pool", lambda e, dd=dd: e.affine_select(out=maskd[:, dd, :], in_=maskd[:, dd, :], pattern=[[1, 512]], compare_op=ALU.is_ge,
                                                       fill=0.0, base=-128 * dd, channel_multiplier=-1), reads=["maskd"], writes=["maskd"])
    pr.op("sp", lambda e: e.dma_start(out=lamt, in_=bcast_rows(lam_dram, 256)), writes=["lamt"], dsem="lamt")
    pr.op("sp", lambda e: e.dma_start(out=dnorm, in_=dnorm_dram.rearrange("(p o) -> p o", o=1)), writes=["dnorm"], dsem="dnorm")
    pr.op("dve", lambda e: e.tensor_tensor(out=lamt[:, 0:64], in0=lamt[:, 0:64], in1=lamt[:, 64:128], op=ALU.mult), reads=["lamt"], writes=["lamt"])
    pr.op("dve", lambda e: e.tensor_tensor(out=lamt[:, 128:192], in0=lamt[:, 128:192], in1=lamt[:, 192:256], op=ALU.mult), reads=["lamt"], writes=["lamt"])
    pr.op("dve", lambda e: e.tensor_reduce(out=small[:, 8:9], in_=lamt[:, 0:64], axis=AX.X, op=ALU.add), reads=["lamt"], writes=["lam_a"])
    pr.op("dve", lambda e: e.tensor_reduce(out=small[:, 9:10], in_=lamt[:, 128:192], axis=AX.X, op=ALU.add), reads=["lamt"], writes=["lam_b"])
    pr.op("act", lambda e: e.activation(out=small[:, 8:10], in_=small[:, 8:10], func=AF.Exp), reads=["lam_a", "lam_b"], writes=["lam_a", "lam_b"])
    pr.op("dve", lambda e: e.tensor_tensor(out=small[:, 10:11], in0=small[:, 9:10], in1=small[:, 8:9], op=ALU.subtract), reads=["lam_a", "lam_b"], writes=["lam_c"])
    pr.op("dve", lambda e: e.tensor_scalar(out=lamneg, in0=small[:, 10:11], scalar1=-float(lambda_init), scalar2=None, op0=ALU.add),
          reads=["lam_c"], writes=["lamneg"])
    pr.op("dve", lambda e: e.tensor_scalar(out=dnorm, in0=dnorm, scalar1=float(1.0 - lambda_init), scalar2=None, op0=ALU.mult),
          reads=["dnorm"], writes=["dnorm"])
    psc = [c.ps[0], c.ps[1], c.ps[2]]
    pO = [c.ps[3], c.ps[4]]
    pL = [c.ps[5], c.ps[6]]
    psb = c.ps[7]
    oTv = oT.rearrange("c p s -> p c s")
    fmv = fmT.rearrange("c p s -> p c s")

    for h in range(4):
        pr.op("sp", lambda e, h=h: e.dma_start(out=QT, in_=fmv[:, h, :]), reads=["fmT"], writes=["QT"], dsem="ql")
        pr.op("sp", lambda e, h=h: e.dma_start(out=KT, in_=fmv[:, 4 + h, :]), reads=["fmT"], writes=["KT"], dsem="kl")
        col_maxnorm(pr, c, QT, "QT", (0, 128), S, sq, psb, mq, "mq")
        col_maxnorm(pr, c, KT, "KT", (0, 128), S, sq, psb, mk, "mk")
        neg_bound(pr, c, mq, mk, negM, ["mq", "mk"], "negM")
        fronts, backs = [], []
        cnt = 0
        gcnt = 0
        for bi, d in enumerate((1, 4, 16)):
            L = S // d
            nb = L // 128
            for r in range(d):
                for n0 in range(0, nb, 16):
                    n1 = min(nb, n0 + 16)
                    pr.op("sp", lambda e, r=r, d=d, nb=nb, h=h, n0=n0, n1=n1: e.dma_start(
                        out=Vp3[bi][:, r * nb + n0:r * nb + n1, :],
                        in_=tm.rearrange("(n m r) c -> r m n c", r=d, m=128)[r, :, n0:n1, h * 128:(h + 1) * 128]),
                          reads=["tm"], writes=[("Vp", bi)], dsem=("vl", bi))
            Qv = QT.rearrange("p (m r) -> p r m", r=d)
            Kv = KT.rearrange("p (m r) -> p r m", r=d)
            numv = num.rearrange("p (m r) -> p r m", r=d)
            denv = den.rearrange("p (m r) -> p r m", r=d)
            for r in range(d):
                for n0 in range(0, nb, 4):
                    nn = min(4, nb - n0)
                    gp = gcnt % 2
                    gcnt += 1
                    for i in range(nn):
                        n = n0 + i
                        sb_ = cnt % 3
                        cnt += 1

                        def front(n=n, r=r, sb_=sb_, Kv=Kv, Qv=Qv):
                            sc = psc[sb_]
                            pT = pTs[sb_]
                            lo = 0 if n > 0 else 128
                            if n > 0:
                                pr.op("pe", lambda e: e.matmul(sc[:, 0:128], lhsT=Kv[:, r, (n - 1) * 128:n * 128],
                                                               rhs=Qv[:, r, n * 128:(n + 1) * 128], start=True, stop=True),
                                      reads=["QT", "KT"], writes=[("sc", sb_)])
                            pr.op("pe", lambda e: e.matmul(sc[:, 128:256], lhsT=Kv[:, r, n * 128:(n + 1) * 128],
                                                           rhs=Qv[:, r, n * 128:(n + 1) * 128], start=True, stop=True),
                                  reads=["QT", "KT"], writes=[("sc", sb_)])
                            pr.op("act", lambda e: e.activation(out=pT[:, lo:256], in_=sc[:, lo:256], func=AF.Exp, bias=negM),
                                  reads=[("sc", sb_), "negM"], writes=[("pT", sb_)])
                            pr.op("dve", lambda e: e.tensor_tensor(out=pT[:, lo:256], in0=pT[:, lo:256], in1=mask2[:, lo:256], op=ALU.mult),
                                  reads=[("pT", sb_), "mask2"], writes=[("pT", sb_)])

                        def back(n=n, r=r, i=i, sb_=sb_, nb=nb, bi=bi, gp=gp, nn=nn, n0=n0, numv=numv, denv=denv):
                            pT = pTs[sb_]
                            Vb = Vp3[bi]
                            cs = slice(i * 128, (i + 1) * 128)
                            kO, kL = ("pO", gp), ("pL", gp)
                            if n > 0:
                                pr.op("pe", lambda e: e.matmul(pO[gp][:, cs], lhsT=Vb[:, r * nb + n - 1, :], rhs=pT[:, 0:128], start=True, stop=False),
                                      reads=[("Vp", bi), ("pT", sb_)], writes=[kO])
                            pr.op("pe", lambda e: e.matmul(pO[gp][:, cs], lhsT=Vb[:, r * nb + n, :], rhs=pT[:, 128:256], start=(n == 0), stop=True),
                                  reads=[("Vp", bi), ("pT", sb_)], writes=[kO])
                            if n > 0:
                                pr.op("pe", lambda e: e.matmul(pL[gp][:, cs], lhsT=c.ones_b, rhs=pT[:, 0:128], start=True, stop=False),
                                      reads=["ones_b", ("pT", sb_)], writes=[kL])
                            pr.op("pe", lambda e: e.matmul(pL[gp][:, cs], lhsT=c.ones_b, rhs=pT[:, 128:256], start=(n == 0), stop=True),
                                  reads=["ones_b", ("pT", sb_)], writes=[kL])
                            if i == nn - 1:
                                W = nn * 128
                                dstn = numv[:, r, n0 * 128:n0 * 128 + W]
                                dstd = denv[:, r, n0 * 128:n0 * 128 + W]
                                if bi == 0:
                                    pr.op("act", lambda e: e.activation(out=dstn, in_=pO[gp][:, 0:W], func=AF.Copy), reads=[kO], writes=["num"])
                                    pr.op("dve", lambda e: e.tensor_copy(out=dstd, in_=pL[gp][:, 0:W]), reads=[kL], writes=["den"])
                                else:
                                    pr.op("dve", lambda e: e.tensor_tensor(out=dstn, in0=pO[gp][:, 0:W], in1=dstn, op=ALU.add), reads=[kO, "num"], writes=["num"])
                                    pr.op("dve", lambda e: e.tensor_tensor(out=dstd, in0=pL[gp][:, 0:W], in1=dstd, op=ALU.add), reads=[kL, "den"], writes=["den"])

                        fronts.append(front)
                        backs.append(back)
        pipeline(fronts, backs, 2)
        for g in range(S // 512):
            s = g % 2
            cs = slice(g * 512, (g + 1) * 512)
            pr.op("dve", lambda e, cs=cs: e.reciprocal(out=den[:, cs], in_=den[:, cs]), reads=["den"], writes=["den"])
            pr.op("dve", lambda e, cs=cs, s=s: e.tensor_tensor(out=ost[s], in0=num[:, cs], in1=den[:, cs], op=ALU.mult),
                  reads=["num", "den"], writes=[("ost", s)])
            pr.op("sp", lambda e, cs=cs, s=s, h=h: e.dma_start(out=oTv[:, h, cs], in_=ost[s]), reads=[("ost", s)], writes=["oT"], dsem=("ostd", s))

    for h in range(4):
        pr.op("sp", lambda e, h=h: e.dma_start(out=QT, in_=fmv[:, 8 + h, :]), reads=["fmT"], writes=["QT"], dsem="ql")
        pr.op("sp", lambda e, h=h: e.dma_start(out=KT, in_=fmv[:, 12 + h, :]), reads=["fmT"], writes=["KT"], dsem="kl")
        for n0 in range(0, NB, 16):
            n1 = min(NB, n0 + 16)
            pr.op("sp", lambda e, h=h, n0=n0, n1=n1: e.dma_start(
                out=Vp[:, n0:n1, :], in_=tm.rearrange("(n p) c -> p n c", p=128)[:, n0:n1, 512 + h * 128:512 + (h + 1) * 128]),
                  reads=["tm"], writes=[("Vp", 0)], dsem="vl")
        negMs = [small[:, 11:12], small[:, 12:13]]
        for cp in range(2):
            col_maxnorm(pr, c, QT, "QT", (64 * cp, 64 * cp + 64), S, sq, psb, mq, "mq")
            col_maxnorm(pr, c, KT, "KT", (64 * cp, 64 * cp + 64), S, sq, psb, mk, "mk")
            neg_bound(pr, c, mq, mk, negMs[cp], ["mq", "mk"], ("negMd", cp))
        fronts, backs = [], []
        cnt = 0
        for qg in range(S // 512):
            qs = slice(qg * 512, (qg + 1) * 512)
            nkb = 4 * (qg + 1)
            for kb in range(nkb):
                for cp in range(2):
                    rows = slice(64 * cp, 64 * cp + 64)
                    sb_ = cnt % 3
                    cnt += 1

                    def front(kb=kb, sb_=sb_, rows=rows, qs=qs, cp=cp, dd=kb - 4 * qg):
                        sc = psc[sb_]
                        pT = pTs[sb_]
                        pr.op("pe", lambda e: e.matmul(sc[:, :], lhsT=KT[rows, kb * 128:(kb + 1) * 128], rhs=QT[rows, qs], start=True, stop=True),
                              reads=["QT", "KT"], writes=[("sc", sb_)])
                        pr.op("act", lambda e: e.activation(out=pT, in_=sc[:, :], func=AF.Exp, bias=negMs[cp]),
                              reads=[("sc", sb_), ("negMd", cp)], writes=[("pT", sb_)])
                        if dd >= 0:
                            pr.op("pool", lambda e: e.tensor_tensor(out=pT, in0=pT, in1=maskd[:, dd, :], op=ALU.mult),
                                  reads=[("pT", sb_), "maskd"], writes=[("pT", sb_)])

                    def back(kb=kb, sb_=sb_, cp=cp, nkb=nkb, qg=qg, qs=qs, h=h):
                        pT = pTs[sb_]
                        pr.op("pe", lambda e: e.matmul(pO[cp][:, :], lhsT=Vp[:, kb, :], rhs=pT, start=(kb == 0), stop=(kb == nkb - 1)),
                              reads=[("Vp", 0), ("pT", sb_)], writes=[("pO", cp)])
                        pr.op("pe", lambda e: e.matmul(pL[cp][:, :], lhsT=c.ones_b, rhs=pT, start=(kb == 0), stop=(kb == nkb - 1)),
                              reads=["ones_b", ("pT", sb_)], writes=[("pL", cp)])
                        if kb != nkb - 1:
                            return
                        oa = o1s[qg % 2]
                        ka = ("o1", qg % 2)
                        if cp == 0:
                            pr.op("dve", lambda e: e.reciprocal(out=rs2, in_=pL[0][:, :]), reads=[("pL", 0)], writes=["rs2"])
                            pr.op("dve", lambda e: e.tensor_tensor(out=oa, in0=pO[0][:, :], in1=rs2, op=ALU.mult), reads=[("pO", 0), "rs2"], writes=[ka])
                        else:
                            pr.op("dve", lambda e: e.reciprocal(out=rs2, in_=pL[1][:, :]), reads=[("pL", 1)], writes=["rs2"])
                            pr.op("dve", lambda e: e.tensor_tensor(out=o2, in0=pO[1][:, :], in1=rs2, op=ALU.mult), reads=[("pO", 1), "rs2"], writes=["o2"])
                            pr.op("dve", lambda e: e.scalar_tensor_tensor(out=oa, in0=o2, scalar=lamneg, in1=oa, op0=ALU.mult, op1=ALU.add),
                                  reads=[ka, "o2", "lamneg"], writes=[ka])
                            s = qg % 2
                            fm_epilogue(pr, c, oa, ka, 512, None, None, dnorm, "dnorm", sqb, psb, rs, ost[s], ("ost", s))
                            pr.op("sp", lambda e: e.dma_start(out=oTv[:, 4 + h, qs], in_=ost[s]), reads=[("ost", s)], writes=["oT"], dsem=("ostd", s))

                    fronts.append(front)
                    backs.append(back)
        pipeline(fronts, backs, 2)


def l0_attn_phase(pr, c, S, fmT, tm, tm32, oT, convT, alog_d, dtb_d, gnorm_d, rmask_d, rqsc_d, rksc_d):
    pr.barrier()
    pr.reset_phase()
    NB = S // 128
    NG4 = NB // 4
    raw = pr.alloc(S, BF16)
    acc = pr.alloc(S, F32)
    qh = pr.alloc(S, BF16)
    kh = pr.alloc(S, BF16)
    vtok = v3(pr.alloc(S, BF16), NB)
    kdec = v3(pr.alloc(S, BF16), NB)
    zt = pr.alloc(S, BF16)
    ba = v3(pr.alloc(NB * 8, F32), NB)
    gcol = pr.alloc(NB, F32)
    negb = pr.alloc(NB, F32)
    egc = pr.alloc(NB, F32)
    edec = pr.alloc(NB, F32)
    egl = pr.alloc(NB, F32)
    tmpn = pr.alloc(NB, F32)
    cw = pr.alloc(4, F32)
    small = pr.alloc(16, F32)
    alog = small[:, 0:4]
    dtb = small[:, 4:8]
    gnorm = small[:, 8:9]
    rksc = small[:, 12:16]
    U4i = pr.alloc(512, F32)
    U4s = pr.alloc(512, F32)
    I4 = pr.alloc(512, F32)
    Lst = pr.alloc(128, F32)
    Gt = pr.alloc(512, F32)
    Dx = pr.alloc(512, F32)
    Dm = pr.alloc(512, F32)
    DmS = pr.alloc(512, F32)
    EGs = pr.alloc(512, F32)
    XY = [[pr.alloc(512, F32), pr.alloc(512, F32)] for _ in range(2)]
    Pm = pr.alloc(512, F32)
    PTb = pr.alloc(512, BF16)
    MTb = pr.alloc(512, BF16)
    qtl = pr.alloc(512, BF16)
    PTb2 = [PTb, pr.alloc(512, BF16)]
    MTb2 = [MTb, pr.alloc(512, BF16)]
    qtl2 = [qtl, pr.alloc(512, BF16)]
    Rm = pr.alloc(128, BF16)
    vnew = pr.alloc(128, BF16)
    sq = pr.alloc(512, BF16)
    sqb = pr.alloc(512, BF16)
    rs = pr.alloc(512, F32)
    S_f = pr.alloc(128, F32)
    S_b = pr.alloc(128, BF16)
    o_sb = pr.alloc(512, F32)
    gate = pr.alloc(512, F32)
    ost = [pr.alloc(512, BF16) for _ in range(2)]
    rmask = pr.alloc(512, F32)
    rqsc = pr.alloc(512, F32)
    ps = c.ps
    oTv = oT.rearrange("c p s -> p c s")
    fmv = fmT.rearrange("c p s -> p c s")
    K0, K1, K2, K3, K4, K5, K6 = "ps0", "ps1", "ps2", "ps3", "ps4", "ps5", "ps6"

    pr.op("pool", lambda e: e.memset(U4i, 1.0), writes=["U4i"])
    pr.op("pool", lambda e: e.memset(U4s, 1.0), writes=["U4s"])
    pr.op("pool", lambda e: e.memset(Lst, 1.0), writes=["Lst"])
    for i in range(4):
        cs = slice(i * 128, (i + 1) * 128)
        pr.op("pool", lambda e, cs=cs: e.affine_select(out=U4i[:, cs], in_=U4i[:, cs], pattern=[[1, 128]], compare_op=ALU.is_ge,
                                                       fill=0.0, base=0, channel_multiplier=-1), reads=["U4i"], writes=["U4i"])
        pr.op("pool", lambda e, cs=cs: e.affine_select(out=U4s[:, cs], in_=U4s[:, cs], pattern=[[1, 128]], compare_op=ALU.is_ge,
                                                       fill=0.0, base=-1, channel_multiplier=-1), reads=["U4s"], writes=["U4s"])
        pr.op("dve", lambda e, cs=cs: e.tensor_copy(out=I4[:, cs], in_=c.ident_f), reads=["ident_f"], writes=["I4"])
    pr.op("pool", lambda e: e.affine_select(out=Lst, in_=Lst, pattern=[[-1, 128]], compare_op=ALU.is_ge,
                                            fill=0.0, base=-1, channel_multiplier=1), reads=["Lst"], writes=["Lst"])
    pr.op("sp", lambda e: e.dma_start(out=alog, in_=bcast_rows(alog_d, 4)), writes=["alog"], dsem="c1")
    pr.op("sp", lambda e: e.dma_start(out=dtb, in_=bcast_rows(dtb_d, 4)), writes=["dtb"], dsem="c2")
    pr.op("sp", lambda e: e.dma_start(out=gnorm, in_=gnorm_d.rearrange("(p o) -> p o", o=1)), writes=["gnorm"], dsem="c3")
    pr.op("sp", lambda e: e.dma_start(out=rksc, in_=rksc_d[:, :]), writes=["rksc"], dsem="c4")
    pr.op("sp", lambda e: e.dma_start(out=ba, in_=tm32.rearrange("(n p) c -> p n c", p=128)), reads=["tm32"], writes=["ba"], dsem="c5")
    pr.op("act", lambda e: e.activation(out=alog, in_=alog, func=AF.Exp), reads=["alog"], writes=["alog"])
    pr.op("dve", lambda e: e.tensor_scalar(out=alog, in0=alog, scalar1=-1.0, scalar2=None, op0=ALU.mult), reads=["alog"], writes=["alog"])

    def conv(ch):
        pr.op("sp", lambda e: e.dma_start(out=raw, in_=fmv[:, ch, :]), reads=["fmT"], writes=["raw"], dsem="rawl")
        pr.op("sp", lambda e: e.dma_start(out=cw, in_=convT[ch * 128:(ch + 1) * 128, :]), writes=["cw"], dsem="cwl")
        pr.op("dve", lambda e: e.tensor_scalar(out=acc, in0=raw, scalar1=cw[:, 3:4], scalar2=None, op0=ALU.mult),
              reads=["raw", "cw"], writes=["acc"])
        for sh in (1, 2, 3):
            pr.op("dve", lambda e, sh=sh: e.scalar_tensor_tensor(out=acc[:, sh:S], in0=raw[:, 0:S - sh], scalar=cw[:, 3 - sh:4 - sh],
                                                                 in1=acc[:, sh:S], op0=ALU.mult, op1=ALU.add),
                  reads=["raw", "cw", "acc"], writes=["acc"])
        pr.op("act", lambda e: e.activation(out=acc, in_=acc, func=AF.Silu), reads=["acc"], writes=["acc"])

    def l2norm(outb, outkey, scale):
        for g in range(S // 512):
            cs = slice(g * 512, (g + 1) * 512)
            pr.op("pool", lambda e, cs=cs: e.tensor_tensor(out=sq, in0=acc[:, cs], in1=acc[:, cs], op=ALU.mult), reads=["acc"], writes=["sq"])
            pr.op("pe", lambda e: e.matmul(ps[3][:, :], lhsT=c.ones_b, rhs=sq, start=True, stop=True), reads=["sq", "ones_b"], writes=[K3])
            pr.op("dve", lambda e: e.tensor_scalar(out=rs, in0=ps[3][:, :], scalar1=EPS, scalar2=None, op0=ALU.add), reads=[K3], writes=["rs"])
            pr.op("act", lambda e: e.activation(out=rs, in_=rs, func=AF.Ln), reads=["rs"], writes=["rs"])
            pr.op("act", lambda e: e.activation(out=rs, in_=rs, func=AF.Exp, scale=-0.5), reads=["rs"], writes=["rs"])
            pr.op("dve", lambda e, cs=cs: e.scalar_tensor_tensor(out=outb[:, cs], in0=acc[:, cs], scalar=float(scale), in1=rs, op0=ALU.mult, op1=ALU.mult),
                  reads=["acc", "rs"], writes=[outkey])

    def epilogue(gq, chunk, wcol, wkey, src_ps=None, src_key=None):
        cs = slice(gq * 512, (gq + 1) * 512)
        s_ = gq % 2
        if src_ps is None:
            src_ps, src_key = ps[0], K0
        pr.op("act", lambda e: e.activation(out=o_sb, in_=src_ps[:, :], func=AF.Copy), reads=[src_key], writes=["o_sb"])
        pr.op("act", lambda e: e.activation(out=gate, in_=zt[:, cs], func=AF.Silu), reads=["zt"], writes=["gate"])
        fm_epilogue(pr, c, o_sb, "o_sb", 512, gate, "gate", wcol, wkey, sqb, ps[3], rs, ost[s_], ("ost", s_), psbkey=K3)
        pr.op("sp", lambda e: e.dma_start(out=oTv[:, chunk, cs], in_=ost[s_]), reads=[("ost", s_)], writes=["oT"], dsem=("ostd", s_))

    psT1 = ps[1][:, :].bitcast(BF16)

    for h in range(4):
        pr.op("act", lambda e, h=h: e.activation(out=tmpn, in_=ba[:, :, h], func=AF.Exp, scale=-1.0), reads=["ba"], writes=["tmpn"])
        pr.op("dve", lambda e: e.tensor_scalar(out=tmpn, in0=tmpn, scalar1=1.0, scalar2=None, op0=ALU.add), reads=["tmpn"], writes=["tmpn"])
        pr.op("dve", lambda e: e.reciprocal(out=tmpn, in_=tmpn), reads=["tmpn"], writes=["tmpn"])
        pr.op("dve", lambda e: e.tensor_scalar(out=negb, in0=tmpn, scalar1=-1.0, scalar2=None, op0=ALU.mult), reads=["tmpn"], writes=["negb"])
        pr.op("act", lambda e, h=h: e.activation(out=tmpn, in_=ba[:, :, 4 + h], func=AF.Exp, bias=dtb[:, h:h + 1]), reads=["ba", "dtb"], writes=["tmpn"])
        pr.op("dve", lambda e: e.tensor_scalar(out=tmpn, in0=tmpn, scalar1=1.0, scalar2=None, op0=ALU.add), reads=["tmpn"], writes=["tmpn"])
        pr.op("act", lambda e: e.activation(out=tmpn, in_=tmpn, func=AF.Ln), reads=["tmpn"], writes=["tmpn"])
        pr.op("dve", lambda e, h=h: e.tensor_scalar(out=gcol, in0=tmpn, scalar1=alog[:, h:h + 1], scalar2=None, op0=ALU.mult),
              reads=["tmpn", "alog"], writes=["gcol"])
        pr.op("pe", lambda e: e.matmul(ps[4][:, 0:NB], lhsT=U4i[:, 0:128], rhs=gcol, start=True, stop=True), reads=["U4i", "gcol"], writes=[K4])
        pr.op("pe", lambda e: e.matmul(ps[5][:, 0:NB], lhsT=c.ones_f, rhs=gcol, start=True, stop=True), reads=["ones_f", "gcol"], writes=[K5])
        pr.op("act", lambda e: e.activation(out=egc, in_=ps[4][:, 0:NB], func=AF.Exp), reads=[K4], writes=["egc"])
        pr.op("act", lambda e: e.activation(out=egl, in_=ps[5][:, 0:NB], func=AF.Exp), reads=[K5], writes=["egl"])
        pr.op("dve", lambda e: e.tensor_copy(out=tmpn, in_=ps[4][:, 0:NB]), reads=[K4], writes=["tmpn"])
        pr.op("dve", lambda e: e.tensor_tensor(out=tmpn, in0=ps[5][:, 0:NB], in1=tmpn, op=ALU.subtract), reads=[K5, "tmpn"], writes=["tmpn"])
        pr.op("act", lambda e: e.activation(out=edec, in_=tmpn, func=AF.Exp), reads=["tmpn"], writes=["edec"])
        conv(h)
        l2norm(qh, "qh", 128 ** -0.5)
        conv(4 + h)
        l2norm(kh, "kh", 1.0)
        for g4 in range(NG4):
            for i in range(4):
                n = 4 * g4 + i
                pr.op("pe", lambda e, n=n, i=i: e.transpose(out=psT1[:, i * 128:(i + 1) * 128], in_=kh[:, n * 128:(n + 1) * 128], identity=c.ident_b),
                      reads=["kh", "ident_b"], writes=[K1])
            for i in range(4):
                n = 4 * g4 + i
                pr.op("dve", lambda e, n=n, i=i: e.tensor_scalar(out=kdec[:, n, :], in0=psT1[:, i * 128:(i + 1) * 128], scalar1=edec[:, n:n + 1],
                                                                 scalar2=None, op0=ALU.mult), reads=[K1, "edec"], writes=["kdec"])
        conv(8 + h)
        for g4 in range(NG4):
            for i in range(4):
                n = 4 * g4 + i
                pr.op("pe", lambda e, n=n, i=i: e.transpose(out=ps[2][:, i * 128:(i + 1) * 128], in_=acc[:, n * 128:(n + 1) * 128], identity=c.ident_f),
                      reads=["acc", "ident_f"], writes=[K2])
            pr.op("act", lambda e, g4=g4: e.activation(out=vtok[:, 4 * g4:4 * g4 + 4, :], in_=v3(ps[2][:, :], 4), func=AF.Copy), reads=[K2], writes=["vtok"])
        pr.op("sp", lambda e, h=h: e.dma_start(out=zt, in_=fmv[:, 12 + h, :]), reads=["fmT"], writes=["zt"], dsem="ztl")
        pr.op("pool", lambda e: e.memset(S_f, 0.0), writes=["S_f"])
        pr.op("pool", lambda e: e.memset(S_b, 0.0), writes=["S_b"])
        def pre_gen(g4, b):
            gs = slice(g4 * 512, (g4 + 1) * 512)
            kP, kM, kQ = ("PTb", b), ("MTb", b), ("qtl", b)
            for i in range(4):
                n = 4 * g4 + i
                pr.op("dve", lambda e, n=n, i=i: e.tensor_scalar(out=Gt[:, i * 128:(i + 1) * 128], in0=U4i[:, 0:128], scalar1=gcol[:, n:n + 1],
                                                                 scalar2=None, op0=ALU.mult), reads=["U4i", "gcol"], writes=["Gt"])
            yield
            for i in range(4):
                cs = slice(i * 128, (i + 1) * 128)
                pr.op("pe", lambda e, cs=cs: e.matmul(ps[0][:, cs], lhsT=Lst, rhs=Gt[:, cs], start=True, stop=True), reads=["Lst", "Gt"], writes=[K0])
                pr.op("pe", lambda e, cs=cs: e.matmul(ps[1][:, cs], lhsT=c.ones_f, rhs=Gt[:, cs], start=True, stop=True), reads=["ones_f", "Gt"], writes=[K1])
            yield
            pr.op("act", lambda e: e.activation(out=Dx, in_=ps[0][:, :], func=AF.Exp), reads=[K0], writes=["Dx"])
            pr.op("act", lambda e: e.activation(out=EGs, in_=ps[1][:, :], func=AF.Exp), reads=[K1], writes=["EGs"])
            yield
            pr.op("dve", lambda e: e.tensor_tensor(out=Dm, in0=Dx, in1=U4i, op=ALU.mult), reads=["Dx", "U4i"], writes=["Dm"])
            pr.op("pool", lambda e: e.tensor_tensor(out=DmS, in0=Dx, in1=U4s, op=ALU.mult), reads=["Dx", "U4s"], writes=["DmS"])
            pr.op("dve", lambda e: e.tensor_tensor(out=qtl2[b], in0=qh[:, gs], in1=EGs, op=ALU.mult), reads=["qh", "EGs"], writes=[kQ])
            for i in range(4):
                n = 4 * g4 + i
                cs = slice(i * 128, (i + 1) * 128)
                ns = slice(n * 128, (n + 1) * 128)
                pr.op("pe", lambda e, cs=cs, ns=ns: e.matmul(ps[0][:, cs], lhsT=kh[:, ns], rhs=kh[:, ns], start=True, stop=True), reads=["kh"], writes=[K0])
                pr.op("pe", lambda e, cs=cs, ns=ns: e.matmul(ps[1][:, cs], lhsT=kh[:, ns], rhs=qh[:, ns], start=True, stop=True), reads=["kh", "qh"], writes=[K1])
            yield
            pr.op("dve", lambda e: e.tensor_tensor(out=MTb2[b], in0=ps[1][:, :], in1=Dm, op=ALU.mult), reads=[K1, "Dm"], writes=[kM])
            X, Y = XY[0]
            for i in range(4):
                n = 4 * g4 + i
                cs = slice(i * 128, (i + 1) * 128)
                pr.op("dve", lambda e, cs=cs, n=n: e.scalar_tensor_tensor(out=X[:, cs], in0=ps[0][:, cs], scalar=negb[:, n:n + 1], in1=DmS[:, cs],
                                                                          op0=ALU.mult, op1=ALU.mult), reads=[K0, "negb", "DmS"], writes=["X0"])
            yield
            for i in range(4):
                cs = slice(i * 128, (i + 1) * 128)
                pr.op("pe", lambda e, cs=cs: e.transpose(out=ps[5][:, cs], in_=X[:, cs], identity=c.ident_f), reads=["X0", "ident_f"], writes=[K5])
            pr.op("dve", lambda e: e.tensor_tensor(out=Pm, in0=X, in1=I4, op=ALU.add), reads=["X0", "I4"], writes=["Pm"])
            yield
            pr.op("act", lambda e: e.activation(out=Y, in_=ps[5][:, :], func=AF.Copy), reads=[K5], writes=["Y0"])
            yield
            cur = 0
            for m in range(6):
                Xc, Yc = XY[cur]
                Xn, Yn = XY[1 - cur]
                kc = ("X%d" % cur, "Y%d" % cur)
                kn_ = ("X%d" % (1 - cur), "Y%d" % (1 - cur))
                for i in range(4):
                    cs = slice(i * 128, (i + 1) * 128)
                    pr.op("pe", lambda e, cs=cs: e.matmul(ps[5][:, cs], lhsT=Xc[:, cs], rhs=Yc[:, cs], start=True, stop=True),
                          reads=[kc[0], kc[1]], writes=[K5])
                if m < 5:
                    for i in range(4):
                        cs = slice(i * 128, (i + 1) * 128)
                        pr.op("pe", lambda e, cs=cs: e.matmul(ps[4][:, cs], lhsT=Yc[:, cs], rhs=Xc[:, cs], start=True, stop=True),
                              reads=[kc[0], kc[1]], writes=[K4])
                yield
                pr.op("act", lambda e: e.activation(out=Yn, in_=ps[5][:, :], func=AF.Copy), reads=[K5], writes=[kn_[1]])
                if m < 5:
                    pr.op("dve", lambda e: e.tensor_copy(out=Xn, in_=ps[4][:, :]), reads=[K4], writes=[kn_[0]])
                yield
                for i in range(4):
                    cs = slice(i * 128, (i + 1) * 128)
                    pr.op("pe", lambda e, cs=cs: e.matmul(ps[6][:, cs], lhsT=Yn[:, cs], rhs=Pm[:, cs], start=True, stop=True),
                          reads=[kn_[1], "Pm"], writes=[K6])
                yield
                pr.op("dve", lambda e: e.tensor_tensor(out=Pm, in0=ps[6][:, :], in1=Pm, op=ALU.add), reads=[K6, "Pm"], writes=["Pm"])
                yield
                cur = 1 - cur
            pr.op("act", lambda e: e.activation(out=PTb2[b], in_=Pm, func=AF.Copy), reads=["Pm"], writes=[kP])
            yield

        def scan_gen(g4, b):
            kP, kM, kQ = ("PTb", b), ("MTb", b), ("qtl", b)
            for i in range(4):
                n = 4 * g4 + i
                cs = slice(i * 128, (i + 1) * 128)
                ns = slice(n * 128, (n + 1) * 128)
                pr.op("pe", lambda e: e.matmul(ps[7][:, 0:128], lhsT=kh[:, ns], rhs=S_b, start=True, stop=True), reads=["kh", "S_b"], writes=[("ps7", 0)])
                yield
                pr.op("dve", lambda e: e.scalar_tensor_tensor(out=Rm, in0=ps[7][:, 0:128], scalar=egc[:, n:n + 1], in1=vtok[:, n, :],
                                                              op0=ALU.mult, op1=ALU.subtract), reads=[("ps7", 0), "egc", "vtok"], writes=["Rm"])
                yield
                pr.op("pe", lambda e: e.matmul(ps[7][:, 128:256], lhsT=PTb2[b][:, cs], rhs=Rm, start=True, stop=True), reads=[kP, "Rm"], writes=[("ps7", 1)])
                yield
                pr.op("dve", lambda e: e.tensor_scalar(out=vnew, in0=ps[7][:, 128:256], scalar1=negb[:, n:n + 1], scalar2=None, op0=ALU.mult),
                      reads=[("ps7", 1), "negb"], writes=["vnew"])
                yield
                pr.op("pe", lambda e: e.matmul(ps[2][:, cs], lhsT=S_b, rhs=qtl2[b][:, cs], start=True, stop=False), reads=["S_b", kQ], writes=[K2])
                pr.op("pe", lambda e: e.matmul(ps[2][:, cs], lhsT=vnew, rhs=MTb2[b][:, cs], start=False, stop=True), reads=["vnew", kM], writes=[K2])
                pr.op("pe", lambda e: e.matmul(ps[7][:, 256:384], lhsT=kdec[:, n, :], rhs=vnew, start=True, stop=True), reads=["kdec", "vnew"], writes=[("ps7", 2)])
                yield
                pr.op("dve", lambda e: e.scalar_tensor_tensor(out=S_f, in0=S_f, scalar=egl[:, n:n + 1], in1=ps[7][:, 256:384],
                                                              op0=ALU.mult, op1=ALU.add), reads=["S_f", "egl", ("ps7", 2)], writes=["S_f"])
                pr.op("act", lambda e: e.activation(out=S_b, in_=S_f, func=AF.Copy), reads=["S_f"], writes=["S_b"])
                yield
            epilogue(g4, h, gnorm, "gnorm", src_ps=ps[2], src_key=K2)
            yield

        for g4 in range(NG4 + 1):
            gens = []
            if g4 < NG4:
                gens.append(pre_gen(g4, g4 % 2))
            if g4 >= 1:
                gens.append(scan_gen(g4 - 1, (g4 - 1) % 2))
            while gens:
                for gg in list(gens):
                    try:
                        next(gg)
                    except StopIteration:
                        gens.remove(gg)

    for h in range(4):
        rows = slice(64 * (h % 2), 64 * (h % 2) + 64)
        gam = 1.0 - 2.0 ** (-5.0 - h)
        pr.op("sp", lambda e, h=h: e.dma_start(out=qh, in_=fmv[:, 16 + h // 2, :]), reads=["fmT"], writes=["qh"], dsem="ql")
        pr.op("sp", lambda e, h=h: e.dma_start(out=kh, in_=fmv[:, 18 + h // 2, :]), reads=["fmT"], writes=["kh"], dsem="kl")
        pr.op("sp", lambda e, h=h: e.dma_start(out=zt, in_=fmv[:, 20 + h, :]), reads=["fmT"], writes=["zt"], dsem="ztl")
        pr.op("sp", lambda e, h=h: e.dma_start(out=rmask, in_=rmask_d[h, :, :]), writes=["rmask"], dsem="rml")
        pr.op("sp", lambda e, h=h: e.dma_start(out=rqsc, in_=rqsc_d[h, :, :]), writes=["rqsc"], dsem="rql")
        for n0 in range(0, NB, 16):
            n1 = min(NB, n0 + 16)
            pr.op("sp", lambda e, h=h, n0=n0, n1=n1: e.dma_start(
                out=vtok[:, n0:n1, :], in_=tm.rearrange("(n p) c -> p n c", p=128)[:, n0:n1, h * 128:(h + 1) * 128]),
                  reads=["tm"], writes=["vtok"], dsem="vl")
        for g4 in range(NG4):
            for i in range(4):
                n = 4 * g4 + i
                pr.op("pe", lambda e, n=n, i=i: e.transpose(out=psT1[:, i * 64:(i + 1) * 64], in_=kh[rows, n * 128:(n + 1) * 128],
                                                            identity=c.ident_b[rows, rows]), reads=["kh", "ident_b"], writes=[K1])
            pr.op("dve", lambda e, g4=g4, h=h: e.tensor_scalar(out=kdec[:, 4 * g4:4 * g4 + 4, 0:64], in0=v3(psT1[:, 0:256], 4), scalar1=rksc[:, h:h + 1],
                                                               scalar2=None, op0=ALU.mult), reads=[K1, "rksc"], writes=["kdec"])
        pr.op("pool", lambda e: e.memset(S_f, 0.0), writes=["S_f"])
        pr.op("pool", lambda e: e.memset(S_b, 0.0), writes=["S_b"])
        for g4 in range(NG4):
            gs = slice(g4 * 512, (g4 + 1) * 512)
            for i in range(4):
                n = 4 * g4 + i
                cs = slice(i * 128, (i + 1) * 128)
                ns = slice(n * 128, (n + 1) * 128)
                pr.op("pe", lambda e, cs=cs, ns=ns: e.matmul(ps[2][:, cs], lhsT=kh[rows, ns], rhs=qh[rows, ns], start=True, stop=True), reads=["kh", "qh"], writes=[K2])
            pr.op("dve", lambda e: e.tensor_tensor(out=PTb, in0=ps[2][:, :], in1=rmask, op=ALU.mult), reads=[K2, "rmask"], writes=[("PTb", 0)])
            pr.op("dve", lambda e, gs=gs: e.tensor_tensor(out=qtl[rows, :], in0=qh[rows, gs], in1=rqsc[rows, :], op=ALU.mult), reads=["qh", "rqsc"], writes=[("qtl", 0)])
            for i in range(4):
                n = 4 * g4 + i
                cs = slice(i * 128, (i + 1) * 128)
                pr.op("pe", lambda e, cs=cs: e.matmul(ps[0][:, cs], lhsT=S_b[rows, :], rhs=qtl[rows, cs], start=True, stop=False), reads=["S_b", ("qtl", 0)], writes=[K0])
                pr.op("pe", lambda e, cs=cs, n=n: e.matmul(ps[0][:, cs], lhsT=vtok[:, n, :], rhs=PTb[:, cs], start=False, stop=True), reads=["vtok", ("PTb", 0)], writes=[K0])
                pr.op("pe", lambda e, n=n: e.matmul(ps[7][rows, 0:128], lhsT=kdec[:, n, 0:64], rhs=vtok[:, n, :], start=True, stop=True), reads=["kdec", "vtok"], writes=[("ps7", 0)])
                pr.op("dve", lambda e: e.scalar_tensor_tensor(out=S_f[rows, :], in0=S_f[rows, :], scalar=float(gam ** 128), in1=ps[7][rows, 0:128],
                                                              op0=ALU.mult, op1=ALU.add), reads=["S_f", ("ps7", 0)], writes=["S_f"])
                pr.op("act", lambda e: e.activation(out=S_b[rows, :], in_=S_f[rows, :], func=AF.Copy), reads=["S_f"], writes=["S_b"])
            epilogue(g4, 4 + h, None, None)


ROPE_THETA = 10000.0


def rope_np(S, dim):
    inv = (ROPE_THETA ** (-np.arange(0, dim, 2, dtype=np.float32) / np.float32(dim))).astype(np.float32)
    ang = np.arange(S, dtype=np.float32)[:, None] * inv[None, :]
    return np.cos(ang).astype(np.float32), np.sin(ang).astype(np.float32)


def rope_table(S, dim, reps, scale):
    cos, sin = rope_np(S, dim)
    half = dim // 2
    cl = np.concatenate([cos.T, cos.T], axis=0)
    sl = np.concatenate([-sin.T, sin.T], axis=0)
    cl = np.tile(cl, (reps, 1)) * np.float32(scale)
    sl = np.tile(sl, (reps, 1)) * np.float32(scale)
    return np.stack([cl, sl], 0).astype(np.float32)


def perm_cols(col0, nheads, dim):
    idx = []
    half = dim // 2
    for h in range(nheads):
        for f in range(dim):
            idx.append(col0 + h * dim + (f + half) % dim)
    return np.array(idx)


def host_consts(S, weights):
    c = {"ident": np.eye(128, dtype=np.float32)}
    w1 = weights["l1_w_in"]
    c["l1_w_ext"] = np.ascontiguousarray(np.concatenate(
        [w1, w1[:, perm_cols(0, 4, 128)], w1[:, perm_cols(512, 4, 128)],
         w1[:, perm_cols(1536, 8, 64)], w1[:, perm_cols(2048, 8, 64)]], axis=1))
    c["l1_tabs"] = np.ascontiguousarray(np.stack([
        rope_table(S, 128, 1, 128 ** -0.5), rope_table(S, 128, 1, 1.0),
        rope_table(S, 64, 2, 64 ** -0.5), rope_table(S, 64, 2, 1.0)], 0))
    w0 = weights["l0_w_in"]
    c["l0_w_ext"] = np.ascontiguousarray(np.concatenate([w0, w0[:, perm_cols(2056, 4, 64)], w0[:, perm_cols(2312, 4, 64)]], axis=1))
    c["l0_tabs"] = np.ascontiguousarray(np.stack([rope_table(S, 64, 2, 64 ** -0.5), rope_table(S, 64, 2, 1.0)], 0))
    c["l0_convT"] = np.ascontiguousarray(weights["l0_conv_w"].T)
    idx = np.arange(128, dtype=np.float64)
    rm = np.zeros((4, 128, 512), np.float32)
    rq = np.zeros((4, 128, 512), np.float32)
    rk = np.zeros((128, 4), np.float32)
    for h in range(4):
        lg = np.log(1.0 - 2.0 ** (-5.0 - h))
        rel = idx[None, :] - idx[:, None]
        m = np.where(rel >= 0, np.exp(np.where(rel >= 0, rel, 0.0) * lg), 0.0)
        rm[h] = np.tile(m, (1, 4))
        rq[h] = np.tile(np.exp((idx + 1.0) * lg)[None, :], (128, 4))
        rk[:, h] = np.exp((127.0 - idx) * lg)
    c["l0_rmask"] = rm
    c["l0_rqsc"] = rq
    c["l0_rksc"] = rk
    c["l1_lam"] = np.ascontiguousarray(np.concatenate(
        [weights["l1_lambda_q1"], weights["l1_lambda_k1"], weights["l1_lambda_q2"], weights["l1_lambda_k2"]]))
    return c


W_NAMES = ["l0_ffn1_norm", "l0_ffn1_w_up", "l0_ffn1_w_down", "l0_mix_norm", "l0_w_in", "l0_conv_w", "l0_a_log",
           "l0_dt_bias", "l0_gdn_norm", "l0_w_out", "l0_ffn2_norm", "l0_ffn2_w_up", "l0_ffn2_w_down",
           "l1_ffn1_norm", "l1_ffn1_w_up", "l1_ffn1_w_down", "l1_mix_norm", "l1_w_in", "l1_lambda_q1", "l1_lambda_k1",
           "l1_lambda_q2", "l1_lambda_k2", "l1_diff_norm", "l1_w_out", "l1_ffn2_norm", "l1_ffn2_w_up", "l1_ffn2_w_down",
           "final_norm"]


def build_program(S, phases, shapes):
    nc = bass.Bass("TRN2", target_bir_lowering=False)
    stack = ExitStack()
    pr = Prog(nc, stack)
    c = Ctx()
    c.dram = {}
    for name, shp in shapes.items():
        c.dram[name] = nc.dram_tensor(name, list(shp), F32, kind="ExternalInput").ap()
    x_in = nc.dram_tensor("x", [S, D], F32, kind="ExternalInput").ap()
    out = nc.dram_tensor("out", [S, D], F32, kind="ExternalOutput").ap()
    xs = nc.dram_tensor("xstream", [S, D], F32, kind="Internal").ap()
    with stack:
        pr.init_sbuf()
        setup_common(pr, c)
        src = x_in
        w = c.dram
        for i, ph in enumerate(phases):
            last = (i == len(phases) - 1)
            if ph in ("l0_ffn1", "l0_ffn2", "l1_ffn1", "l1_ffn2"):
                fin = w["final_norm"] if ph == "l1_ffn2" else None
                if fin is None and last:
                    ffn_phase(pr, c, ph, src, out, w[ph + "_w_up"], w[ph + "_w_down"], w[ph + "_norm"], S)
                else:
                    ffn_phase(pr, c, ph, src, xs, w[ph + "_w_up"], w[ph + "_w_down"], w[ph + "_norm"], S,
                              fin=fin, out_final=out)
                src = xs
            elif ph == "l0_mix":
                fmT = nc.dram_tensor("fmT0", [24, 128, S], BF16, kind="Internal").ap()
                tm = nc.dram_tensor("tm0", [S, 512], BF16, kind="Internal").ap()
                tm32 = nc.dram_tensor("tm32_0", [S, 8], F32, kind="Internal").ap()
                oT = nc.dram_tensor("oT0", [8, 128, S], BF16, kind="Internal").ap()
                fm_jobs = ([(j * 128, None, None) for j in range(16)] + [(2056 + j * 128, 3592 + j * 128, 0) for j in range(2)]
                           + [(2312 + j * 128, 3848 + j * 128, 1) for j in range(2)] + [(3080 + j * 128, None, None) for j in range(4)])
                proj_phase(pr, c, S, src, w["l0_w_ext"], 4104, w["l0_mix_norm"], fm_jobs, [2568], fmT, tm, w["l0_tabs"],
                           tm32_job=(2048, 8), tm32_out=tm32)
                l0_attn_phase(pr, c, S, fmT, tm, tm32, oT, w["l0_convT"], w["l0_a_log"], w["l0_dt_bias"], w["l0_gdn_norm"],
                              w["l0_rmask"], w["l0_rqsc"], w["l0_rksc"])
                dstp = out if last else xs
                outproj_phase(pr, c, S, oT, w["l0_w_out"], src, dstp)
                src = xs
            elif ph == "l1_mix":
                fmT = nc.dram_tensor("fmT1", [16, 128, S], BF16, kind="Internal").ap()
                tm = nc.dram_tensor("tm1", [S, 1024], BF16, kind="Internal").ap()
                oT = nc.dram_tensor("oT1", [8, 128, S], BF16, kind="Internal").ap()
                fm_jobs = ([(h * 128, 3072 + h * 128, 0) for h in range(4)] + [(512 + h * 128, 3584 + h * 128, 1) for h in range(4)]
                           + [(1536 + h * 128, 4096 + h * 128, 2) for h in range(4)] + [(2048 + h * 128, 4608 + h * 128, 3) for h in range(4)])
                proj_phase(pr, c, S, src, w["l1_w_ext"], 5120, w["l1_mix_norm"], fm_jobs, [1024, 2560], fmT, tm, w["l1_tabs"])
                lambda_init = 0.8 - 0.6 * math.exp(-0.3 * 1)
                l1_attn_phase(pr, c, S, fmT, tm, oT, w["l1_lam"], w["l1_diff_norm"], lambda_init)
                dstp = out if last else xs
                outproj_phase(pr, c, S, oT, w["l1_w_out"], src, dstp)
                src = xs
            else:
                raise ValueError(ph)
        pr.barrier()
        pr.op("sp", None)
        pr.emit()
    return nc


ALL_PHASES = ["l0_ffn1", "l0_mix", "l0_ffn2", "l1_ffn1", "l1_mix", "l1_ffn2"]


def run(S, phases, x_list, weights):
    consts = host_consts(S, weights)
    shapes = {k: v.shape for k, v in weights.items()}
    shapes.update({k: v.shape for k, v in consts.items()})
    nc = build_program(S, phases, shapes)
    in_maps = []
    for xb in x_list:
        m = {"x": np.ascontiguousarray(xb)}
        m.update(weights)
        m.update(consts)
        in_maps.append(m)
    res = run_bass_kernel_spmd(nc, in_maps, core_ids=list(range(len(x_list))))
    return [r["out"] for r in res.results]


def kernel(**inputs):
    x = np.asarray(inputs["x"])
    B, S, _ = x.shape
    weights = {k: np.ascontiguousarray(np.asarray(inputs[k], dtype=np.float32)) for k in W_NAMES}
    outs = run(S, ALL_PHASES, [x[b] for b in range(B)], weights)
    return np.stack(outs, axis=0).astype(np.float32)
```

```python
from contextlib import ExitStack
import math
import numpy as np
import ml_dtypes
import concourse.bass as bass
import concourse.mybir as mybir
from concourse.bass_utils import run_bass_kernel_spmd

F32 = mybir.dt.float32
BF16 = mybir.dt.bfloat16
AF = mybir.ActivationFunctionType
ALU = mybir.AluOpType
AX = mybir.AxisListType

D = 1024
DFF = 2816
EPS = 1e-6
SBUF_BYTES = 196608 - 2048
EPOCH = 16000
ENGS = ("pe", "act", "dve", "pool", "sp")


class _Rec:
    def __init__(self):
        self.call = None

    def __getattr__(self, name):
        def f(*a, **k):
            self.call = (name, a, k)
            return self
        return f


class Prog:
    def __init__(self, nc, stack):
        self.nc = nc
        self.stack = stack
        self.streams = {e: [] for e in ENGS}
        self.cnt = {e: 0 for e in ENGS}
        self.esem = {e: None for e in ENGS}
        self.ebase = {e: 0 for e in ENGS}
        self.lastw = {}
        self.readers = {}
        self.known = {e: {} for e in ENGS}
        self.dsems = {}
        self.nsem = 0
        self.pending = {e: [] for e in ENGS}
        self.sb = None
        self.sb_off = 0
        self.sb_persist = 0
        self.latest = {}

    def newsem(self, name):
        self.nsem += 1
        return self.stack.enter_context(self.nc.semaphore("s%d_%s" % (self.nsem, name)))

    def init_sbuf(self):
        self.sb = self.stack.enter_context(self.nc.sbuf_tensor("sbuf_all", [128, SBUF_BYTES // 4], F32))

    def alloc(self, cols, dtype, parts=128):
        nbytes = cols * (4 if dtype == F32 else 2)
        nbytes = (nbytes + 63) // 64 * 64
        assert self.sb_off + nbytes <= SBUF_BYTES, ("SBUF overflow", self.sb_off, nbytes)
        a = self.sb[0:parts, self.sb_off // 4:(self.sb_off + nbytes) // 4]
        self.sb_off += nbytes
        if dtype != F32:
            a = a.bitcast(dtype)
        return a[:, 0:cols]

    def mark_persistent(self):
        self.sb_persist = self.sb_off

    def reset_phase(self):
        self.sb_off = self.sb_persist

    def _event(self, eng):
        if self.esem[eng] is None or self.cnt[eng] - self.ebase[eng] >= EPOCH:
            self.esem[eng] = self.newsem(eng)
            self.ebase[eng] = self.cnt[eng]
        self.cnt[eng] += 1
        return (self.esem[eng], self.cnt[eng] - self.ebase[eng], eng)

    def op(self, eng, fn, reads=(), writes=(), dsem=None):
        deps = list(self.pending[eng])
        self.pending[eng] = []
        for k in reads:
            ev = self.lastw.get(k)
            if ev is not None:
                deps.append(ev)
        for k in writes:
            ev = self.lastw.get(k)
            if ev is not None:
                deps.append(ev)
            deps.extend(self.readers.get(k, {}).values())
        waits = {}
        kn = self.known[eng]
        for (sem, val, peng) in deps:
            if peng == eng and eng == "pe":
                continue
            if kn.get(id(sem), 0) >= val:
                continue
            if waits.get(id(sem), (None, 0))[1] < val:
                waits[id(sem)] = (sem, val)
        for sid, (sem, val) in waits.items():
            kn[sid] = val
        if fn is None:
            self.streams[eng].append((list(waits.values()), None, None))
            return None
        rec = _Rec()
        fn(rec)
        fn = rec.call
        if dsem is None:
            ev = self._event(eng)
            inc = (ev[0], 1)
        else:
            if dsem not in self.dsems:
                self.dsems[dsem] = [self.newsem("d"), 0]
            d = self.dsems[dsem]
            d[1] += 16
            ev = (d[0], d[1], "dma")
            inc = (d[0], 16)
        self.streams[eng].append((list(waits.values()), fn, inc))
        self.latest[id(ev[0])] = ev
        for k in writes:
            self.lastw[k] = ev
            self.readers[k] = {}
        for k in reads:
            if k in writes:
                continue
            self.readers.setdefault(k, {})[id(ev[0])] = ev
        return ev

    def barrier(self):
        evs = list(self.latest.values())
        for e in ENGS:
            self.pending[e] = list(evs)

    def emit(self):
        with self.nc.Block() as block:
            decos = {"pe": block.tensor, "act": block.scalar, "dve": block.vector,
                     "pool": block.gpsimd, "sp": block.sync}
            for eng in ENGS:
                stream = self.streams[eng]

                def body(e, stream=stream):
                    for waits, fn, inc in stream:
                        for sem, val in waits:
                            e.wait_ge(sem, val)
                        if fn is not None:
                            getattr(e, fn[0])(*fn[1], **fn[2]).then_inc(inc[0], inc[1])

                decos[eng](body)


def v3(ap, a):
    return ap.rearrange("p (a b) -> p a b", a=a)


def bcast_rows(vec_ap, n, parts=128):
    return vec_ap.rearrange("(o n) -> o n", o=1).broadcast_to([parts, n])


class Ctx:
    pass


def setup_common(pr, c):
    nc = pr.nc
    c.ps = [pr.stack.enter_context(nc.psum_tensor("ps%d" % i, [128, 512], F32)) for i in range(8)]
    c.ident_f = pr.alloc(128, F32)
    c.ident_b = pr.alloc(128, BF16)
    c.mhalf = pr.alloc(1, F32)
    c.ones_b = pr.alloc(128, BF16)
    c.ones_f = pr.alloc(128, F32)
    pr.op("sp", lambda e: e.dma_start(out=c.ident_f, in_=c.dram["ident"][:, :]), writes=["ident_f"], dsem="const")
    pr.op("dve", lambda e: e.tensor_copy(out=c.ident_b, in_=c.ident_f), reads=["ident_f"], writes=["ident_b"])
    pr.op("pool", lambda e: e.memset(c.mhalf, -0.5), writes=["mhalf"])
    pr.op("pool", lambda e: e.memset(c.ones_b, 1.0), writes=["ones_b"])
    pr.op("pool", lambda e: e.memset(c.ones_f, 1.0), writes=["ones_f"])
    pr.mark_persistent()


def rms_rows(pr, c, x_ap, xkey, out_ap, outkey, wn, wnkey, junk, ss, tag):
    pr.op("dve", lambda e: e.scalar_tensor_tensor(out=junk, in0=x_ap, scalar=1.0, in1=x_ap,
                                                  op0=ALU.mult, op1=ALU.mult, accum_out=ss[:, 0:1]),
          reads=[xkey], writes=["junk" + tag, "ss" + tag])
    pr.op("dve", lambda e: e.tensor_scalar(out=ss[:, 1:2], in0=ss[:, 0:1], scalar1=1.0 / x_ap.shape[-1], scalar2=EPS,
                                           op0=ALU.mult, op1=ALU.add),
          reads=["ss" + tag], writes=["ms" + tag])
    pr.op("pool", lambda e: e.tensor_tensor(out=ss[:, 2:3], in0=ss[:, 1:2], in1=c.mhalf, op=ALU.pow),
          reads=["ms" + tag, "mhalf"], writes=["rstd" + tag])
    if wn is not None:
        pr.op("dve", lambda e: e.scalar_tensor_tensor(out=out_ap, in0=x_ap, scalar=ss[:, 2:3], in1=wn,
                                                      op0=ALU.mult, op1=ALU.mult),
              reads=[xkey, "rstd" + tag, wnkey], writes=[outkey])
    else:
        pr.op("dve", lambda e: e.tensor_scalar(out=out_ap, in0=x_ap, scalar1=ss[:, 2:3], scalar2=None,
                                               op0=ALU.mult),
              reads=[xkey, "rstd" + tag], writes=[outkey])


def load_weight_bf16(pr, dst3, src2, nk, ncols, key, dsem, colchunk=1024):
    for k in range(nk):
        c0 = 0
        while c0 < ncols:
            w = min(colchunk, ncols - c0)
            pr.op("pool", lambda e, k=k, c0=c0, w=w: e.dma_start(out=dst3[:, k, c0:c0 + w],
                                                                  in_=src2[k * 128:(k + 1) * 128, c0:c0 + w]),
                  writes=[key], dsem=dsem)
            c0 += w


def load_weight_chunked(pr, dst3, src2, nk, ncols, key, dsem, cw, order=None):
    ncc = (ncols + cw - 1) // cw
    for cc in (order if order is not None else range(ncc)):
        c0 = cc * cw
        w = min(cw, ncols - c0)
        for k in range(nk):
            pr.op("pool", lambda e, k=k, c0=c0, w=w: e.dma_start(out=dst3[:, k, c0:c0 + w],
                                                                  in_=src2[k * 128:(k + 1) * 128, c0:c0 + w]),
                  writes=[(key, cc)], dsem=(dsem, cc))


def wkeys(key, c0, w, cw):
    return [(key, cc) for cc in range(c0 // cw, (c0 + w - 1) // cw + 1)]


def ffn_phase(pr, c, tag, src, dst, wup, wdn, nrm, S, fin=None, out_final=None):
    pr.barrier()
    pr.reset_phase()
    G = 256
    NG = S // G
    NF = DFF // 128
    wup_sb = v3(pr.alloc(8 * 2 * DFF, BF16), 8)
    wdn_sb = v3(pr.alloc(NF * D, BF16), NF)
    xs = [v3(pr.alloc(2 * D, F32), 2) for _ in range(2)]
    hb = [pr.alloc(D, BF16) for _ in range(2)]
    hT = [v3(pr.alloc(8 * G, BF16), 8) for _ in range(2)]
    actT = v3(pr.alloc(NF * G, BF16), NF)
    sg = [pr.alloc(G, F32) for _ in range(2)]
    junk = pr.alloc(D, BF16)
    wn = pr.alloc(D, F32)
    ss = pr.alloc(4, F32)
    if fin is not None:
        wfin = pr.alloc(D, F32)
        pr.op("sp", lambda e: e.dma_start(out=wfin, in_=bcast_rows(fin, D)), writes=["wfin"], dsem="wn2")
    pr.op("sp", lambda e: e.dma_start(out=wn, in_=bcast_rows(nrm, D)), writes=["wn"], dsem="wn")
    load_weight_chunked(pr, wup_sb, wup, 8, 2 * DFF, "wup", "wup", 704, order=[0, 4, 1, 5, 2, 6, 3, 7])
    load_weight_bf16(pr, wdn_sb, wdn, NF, D, "wdn", "wdn")
    pst = c.ps[0][:, :].bitcast(BF16)
    psg = [c.ps[1], c.ps[2]]
    psu = [c.ps[3], c.ps[4]]
    psd = [c.ps[5], c.ps[6]]

    def load(g):
        s = g % 2
        pr.op("sp", lambda e: e.dma_start(out=xs[s], in_=src[g * G:(g + 1) * G, :].rearrange("(n p) d -> p n d", p=128)),
              reads=[("xd", g)], writes=[("x", s, 0), ("x", s, 1)], dsem=("xl", s))

    def prep_dve(g):
        s = g % 2
        for t in range(2):
            rms_rows(pr, c, xs[s][:, t, :], ("x", s, t), hb[t], ("hb", t), wn, "wn", junk, ss, "f")

    def prep_pe(g):
        s = g % 2
        for t in range(2):
            for k in range(8):
                pr.op("pe", lambda e, t=t, k=k: e.transpose(out=pst[:, k * 128:(k + 1) * 128],
                                                            in_=hb[t][:, k * 128:(k + 1) * 128], identity=c.ident_b),
                      reads=[("hb", t), "ident_b"], writes=["pst"])
            pr.op("act", lambda e, t=t, s=s: e.activation(out=hT[s][:, :, t * 128:(t + 1) * 128], in_=v3(pst, 8), func=AF.Copy),
                  reads=["pst"], writes=[("hT", s)])

    def up(g):
        s = g % 2
        for f in range(NF):
            b = f % 2
            for k in range(8):
                pr.op("pe", lambda e, f=f, k=k, b=b: e.matmul(psg[b][:, 0:G], lhsT=wup_sb[:, k, f * 128:(f + 1) * 128],
                                                              rhs=hT[s][:, k, :], start=(k == 0), stop=(k == 7)),
                      reads=wkeys("wup", f * 128, 128, 704) + [("hT", s)], writes=[("psg", b)])
            for k in range(8):
                pr.op("pe", lambda e, f=f, k=k, b=b: e.matmul(psu[b][:, 0:G], lhsT=wup_sb[:, k, DFF + f * 128:DFF + (f + 1) * 128],
                                                              rhs=hT[s][:, k, :], start=(k == 0), stop=(k == 7)),
                      reads=wkeys("wup", DFF + f * 128, 128, 704) + [("hT", s)], writes=[("psu", b)])
            pr.op("act", lambda e, b=b: e.activation(out=sg[b], in_=psg[b][:, 0:G], func=AF.Silu),
                  reads=[("psg", b)], writes=[("sg", b)])
            pr.op("dve", lambda e, f=f, b=b: e.tensor_tensor(out=actT[:, f, :], in0=sg[b], in1=psu[b][:, 0:G], op=ALU.mult),
                  reads=[("sg", b), ("psu", b)], writes=["actT"])

    def down(g):
        s = g % 2
        for t in range(2):
            for hf in range(2):
                b = hf
                for f in range(NF):
                    pr.op("pe", lambda e, f=f, t=t, hf=hf, b=b: e.matmul(psd[b][:, :], lhsT=actT[:, f, t * 128:(t + 1) * 128],
                                                                          rhs=wdn_sb[:, f, hf * 512:(hf + 1) * 512],
                                                                          start=(f == 0), stop=(f == NF - 1)),
                          reads=["actT", "wdn"], writes=[("psd", b)])
                pr.op("dve", lambda e, t=t, hf=hf, b=b, s=s: e.scalar_tensor_tensor(
                    out=xs[s][:, t, hf * 512:(hf + 1) * 512], in0=psd[b][:, :], scalar=0.5,
                    in1=xs[s][:, t, hf * 512:(hf + 1) * 512], op0=ALU.mult, op1=ALU.add),
                      reads=[("psd", b), ("x", s, t)], writes=[("x", s, t)])
            if fin is not None:
                rms_rows(pr, c, xs[s][:, t, :], ("x", s, t), xs[s][:, t, :], ("x", s, t), wfin, "wfin", junk, ss, "f")
        tgt = dst if fin is None else out_final
        pr.op("sp", lambda e: e.dma_start(out=tgt[g * G:(g + 1) * G, :].rearrange("(n p) d -> p n d", p=128), in_=xs[s]),
              reads=[("x", s, 0), ("x", s, 1)], writes=[("xd", g)], dsem=("xst", s))

    load(0)
    prep_dve(0)
    prep_pe(0)
    for g in range(NG):
        if g + 1 < NG:
            load(g + 1)
            prep_dve(g + 1)
        up(g)
        if g + 1 < NG:
            prep_pe(g + 1)
        down(g)


def proj_phase(pr, c, S, src, w_dram, ncols, nrm, fm_jobs, tm_jobs, fmT, tm_out, tabs, tm32_job=None, tm32_out=None):
    pr.barrier()
    pr.reset_phase()
    G = 256
    NG = S // G
    nch = len(fm_jobs)
    ntab = 0 if tabs is None else tabs.shape[0]
    w_sb = v3(pr.alloc(8 * ncols, BF16), 8)
    xs = [v3(pr.alloc(2 * D, F32), 2) for _ in range(2)]
    hb = [pr.alloc(D, BF16) for _ in range(2)]
    hT = [v3(pr.alloc(8 * G, BF16), 8) for _ in range(2)]
    junk = pr.alloc(D, BF16)
    wn = pr.alloc(D, F32)
    ss = pr.alloc(4, F32)
    stage = [v3(pr.alloc(nch * G, BF16), nch) for _ in range(2)]
    ntm = len(tm_jobs)
    tstage = [v3(pr.alloc(2 * max(ntm, 1) * 512, BF16), 2) for _ in range(2)]
    t32 = [v3(pr.alloc(2 * 8, F32), 2) for _ in range(2)]
    tab = [v3(pr.alloc(max(ntab, 1) * 2 * G, F32), max(ntab, 1) * 2) for _ in range(2)]
    t1 = [pr.alloc(G, F32) for _ in range(2)]
    t2 = [pr.alloc(G, F32) for _ in range(2)]
    pr.op("sp", lambda e: e.dma_start(out=wn, in_=bcast_rows(nrm, D)), writes=["wn"], dsem="wn")
    load_weight_chunked(pr, w_sb, w_dram, 8, ncols, "w_in", "w_in", 512)
    pst = c.ps[0][:, :].bitcast(BF16)
    pA = [c.ps[1], c.ps[2]]
    pB = [c.ps[3], c.ps[4]]
    pT = [c.ps[5], c.ps[6]]

    def load(g):
        s = g % 2
        pr.op("sp", lambda e: e.dma_start(out=xs[s], in_=src[g * G:(g + 1) * G, :].rearrange("(n p) d -> p n d", p=128)),
              reads=[("xd", g)], writes=[("x", s, 0), ("x", s, 1)], dsem=("xl", s))
        if ntab:
            pr.op("sp", lambda e: e.dma_start(out=tab[s], in_=tabs.rearrange("t two p s -> p (t two) s")[:, :, g * G:(g + 1) * G]),
                  writes=[("tab", s)], dsem=("tabl", s))

    def prep(g):
        s = g % 2
        for t in range(2):
            rms_rows(pr, c, xs[s][:, t, :], ("x", s, t), hb[t], ("hb", t), wn, "wn", junk, ss, "f")
        for t in range(2):
            for k in range(8):
                pr.op("pe", lambda e, t=t, k=k: e.transpose(out=pst[:, k * 128:(k + 1) * 128],
                                                            in_=hb[t][:, k * 128:(k + 1) * 128], identity=c.ident_b),
                      reads=[("hb", t), "ident_b"], writes=["pst"])
            pr.op("act", lambda e, t=t, s=s: e.activation(out=hT[s][:, :, t * 128:(t + 1) * 128], in_=v3(pst, 8), func=AF.Copy),
                  reads=["pst"], writes=[("hT", s)])

    def mm_fm(ps, col0, s, key):
        for k in range(8):
            pr.op("pe", lambda e, k=k: e.matmul(ps[:, 0:G], lhsT=w_sb[:, k, col0:col0 + 128], rhs=hT[s][:, k, :],
                                                start=(k == 0), stop=(k == 7)),
                  reads=wkeys("w_in", col0, 128, 512) + [("hT", s)], writes=[key])

    def compute(g):
        s = g % 2
        for j, (col0, pcol0, ti) in enumerate(fm_jobs):
            b = j % 2
            mm_fm(pA[b], col0, s, ("pA", b))
            if pcol0 is None:
                pr.op("act", lambda e, j=j, b=b: e.activation(out=stage[s][:, j, :], in_=pA[b][:, 0:G], func=AF.Copy),
                      reads=[("pA", b)], writes=[("stage", s)])
            else:
                mm_fm(pB[b], pcol0, s, ("pB", b))
                pr.op("dve", lambda e, b=b, ti=ti: e.tensor_tensor(out=t1[b], in0=pA[b][:, 0:G], in1=tab[s][:, 2 * ti, :], op=ALU.mult),
                      reads=[("pA", b), ("tab", s)], writes=[("t1", b)])
                pr.op("dve", lambda e, b=b, ti=ti: e.tensor_tensor(out=t2[b], in0=pB[b][:, 0:G], in1=tab[s][:, 2 * ti + 1, :], op=ALU.mult),
                      reads=[("pB", b), ("tab", s)], writes=[("t2", b)])
                pr.op("pool", lambda e, b=b, j=j: e.tensor_tensor(out=stage[s][:, j, :], in0=t1[b], in1=t2[b], op=ALU.add),
                      reads=[("t1", b), ("t2", b)], writes=[("stage", s)])
        pr.op("sp", lambda e: e.dma_start(out=fmT.rearrange("c p s -> p c s")[:, :, g * G:(g + 1) * G], in_=stage[s]),
              reads=[("stage", s)], writes=[("fmT", g)], dsem=("fst", s))
        for t in range(2):
            for i, col0 in enumerate(tm_jobs):
                b = i % 2
                for k in range(8):
                    pr.op("pe", lambda e, k=k, t=t, col0=col0, b=b: e.matmul(pT[b][:, :], lhsT=hT[s][:, k, t * 128:(t + 1) * 128],
                                                                              rhs=w_sb[:, k, col0:col0 + 512],
                                                                              start=(k == 0), stop=(k == 7)),
                          reads=wkeys("w_in", col0, 512, 512) + [("hT", s)], writes=[("pT", b)])
                pr.op("act", lambda e, t=t, i=i, b=b: e.activation(out=tstage[s][:, t, i * 512:(i + 1) * 512], in_=pT[b][:, :], func=AF.Copy),
                      reads=[("pT", b)], writes=[("tstage", s)])
            if tm32_job is not None:
                col0, n = tm32_job
                for k in range(8):
                    pr.op("pe", lambda e, k=k, t=t: e.matmul(pT[0][:, 0:n], lhsT=hT[s][:, k, t * 128:(t + 1) * 128],
                                                              rhs=w_sb[:, k, col0:col0 + n], start=(k == 0), stop=(k == 7)),
                          reads=wkeys("w_in", col0, n, 512) + [("hT", s)], writes=[("pT", 0)])
                pr.op("act", lambda e, t=t: e.activation(out=t32[s][:, t, 0:n], in_=pT[0][:, 0:n], func=AF.Copy),
                      reads=[("pT", 0)], writes=[("t32", s)])
        if ntm:
            pr.op("sp", lambda e: e.dma_start(out=tm_out[g * G:(g + 1) * G, :].rearrange("(n p) d -> p n d", p=128), in_=tstage[s]),
                  reads=[("tstage", s)], writes=[("tm", g)], dsem=("tst", s))
        if tm32_job is not None:
            n = tm32_job[1]
            pr.op("sp", lambda e: e.dma_start(out=tm32_out[g * G:(g + 1) * G, :].rearrange("(n p) d -> p n d", p=128),
                                              in_=t32[s][:, :, 0:n]),
                  reads=[("t32", s)], writes=[("tm32", g)], dsem=("t32st", s))

    load(0)
    prep(0)
    for g in range(NG):
        if g + 1 < NG:
            load(g + 1)
        compute(g)
        if g + 1 < NG:
            prep(g + 1)


def outproj_phase(pr, c, S, oT, w_out, src, dst):
    pr.barrier()
    pr.reset_phase()
    G = 256
    NG = S // G
    w_sb = v3(pr.alloc(8 * D, BF16), 8)
    load_weight_bf16(pr, w_sb, w_out, 8, D, "w_out", "w_out")
    xs = [v3(pr.alloc(2 * D, F32), 2) for _ in range(2)]
    ot = [v3(pr.alloc(8 * G, BF16), 8) for _ in range(2)]
    pO = [c.ps[1], c.ps[2]]

    def load(g):
        s = g % 2
        pr.op("sp", lambda e: e.dma_start(out=xs[s], in_=src[g * G:(g + 1) * G, :].rearrange("(n p) d -> p n d", p=128)),
              reads=[("xd", g)], writes=[("x", s)], dsem=("xl", s))
        pr.op("sp", lambda e: e.dma_start(out=ot[s], in_=oT.rearrange("c p s -> p c s")[:, :, g * G:(g + 1) * G]),
              reads=["oT"], writes=[("ot", s)], dsem=("otl", s))

    load(0)
    for g in range(NG):
        s = g % 2
        if g + 1 < NG:
            load(g + 1)
        for t in range(2):
            for hf in range(2):
                for k in range(8):
                    pr.op("pe", lambda e, k=k, t=t, hf=hf: e.matmul(pO[hf][:, :], lhsT=ot[s][:, k, t * 128:(t + 1) * 128],
                                                                    rhs=w_sb[:, k, hf * 512:(hf + 1) * 512],
                                                                    start=(k == 0), stop=(k == 7)),
                          reads=["w_out", ("ot", s)], writes=[("pO", hf)])
                pr.op("dve", lambda e, t=t, hf=hf: e.tensor_tensor(out=xs[s][:, t, hf * 512:(hf + 1) * 512], in0=pO[hf][:, :],
                                                                   in1=xs[s][:, t, hf * 512:(hf + 1) * 512], op=ALU.add),
                      reads=[("pO", hf), ("x", s)], writes=[("x", s)])
        pr.op("sp", lambda e: e.dma_start(out=dst[g * G:(g + 1) * G, :].rearrange("(n p) d -> p n d", p=128), in_=xs[s]),
              reads=[("x", s)], writes=[("xd", g)], dsem=("xst", s))


def pipeline(fronts, backs, look):
    n = len(fronts)
    for i in range(n + look):
        if i < n:
            fronts[i]()
        if i >= look:
            backs[i - look]()


def col_maxnorm(pr, c, T, key, rows, S, sq, psb, out_max, tagk, psbkey="psb"):
    r0, r1 = rows
    first = True
    for g in range(S // 512):
        pr.op("pool", lambda e, g=g: e.tensor_tensor(out=sq[r0:r1, :], in0=T[r0:r1, g * 512:(g + 1) * 512],
                                                     in1=T[r0:r1, g * 512:(g + 1) * 512], op=ALU.mult),
              reads=[key], writes=["sq"])
        pr.op("pe", lambda e: e.matmul(psb[:, :], lhsT=c.ones_b[r0:r1, :], rhs=sq[r0:r1, :], start=True, stop=True),
              reads=["sq", "ones_b"], writes=[psbkey])
        if first:
            pr.op("dve", lambda e: e.tensor_reduce(out=out_max, in_=psb[:, :], axis=AX.X, op=ALU.max),
                  reads=[psbkey], writes=[tagk])
            first = False
        else:
            pr.op("dve", lambda e: e.tensor_reduce(out=c.tmpmax, in_=psb[:, :], axis=AX.X, op=ALU.max),
                  reads=[psbkey], writes=["tmpmax"])
            pr.op("dve", lambda e: e.tensor_tensor(out=out_max, in0=out_max, in1=c.tmpmax, op=ALU.max),
                  reads=["tmpmax", tagk], writes=[tagk])


def neg_bound(pr, c, mq, mk, out_negM, keys, outkey):
    pr.op("dve", lambda e: e.tensor_tensor(out=c.tmpmax, in0=mq, in1=mk, op=ALU.mult), reads=keys, writes=["tmpmax"])
    pr.op("pool", lambda e: e.tensor_tensor(out=c.tmpmax, in0=c.tmpmax, in1=c.phalf, op=ALU.pow), reads=["tmpmax", "phalf"], writes=["tmpmax"])
    pr.op("dve", lambda e: e.tensor_scalar(out=out_negM, in0=c.tmpmax, scalar1=-1.0, scalar2=None, op0=ALU.mult),
          reads=["tmpmax"], writes=[outkey])


def fm_epilogue(pr, c, o_ap, okey, N, gate_ap, gatekey, wcol, wkey, sqb, psb, rs, out_ap, outkey, const_scale=1.0, psbkey="psb"):
    pr.op("pool", lambda e: e.tensor_tensor(out=sqb[:, 0:N], in0=o_ap, in1=o_ap, op=ALU.mult), reads=[okey], writes=["sqb"])
    pr.op("pe", lambda e: e.matmul(psb[:, 0:N], lhsT=c.ones_b, rhs=sqb[:, 0:N], start=True, stop=True),
          reads=["sqb", "ones_b"], writes=[psbkey])
    pr.op("dve", lambda e: e.tensor_scalar(out=rs[:, 0:N], in0=psb[:, 0:N], scalar1=1.0 / 128.0, scalar2=EPS, op0=ALU.mult, op1=ALU.add),
          reads=[psbkey], writes=["rs"])
    pr.op("act", lambda e: e.activation(out=rs[:, 0:N], in_=rs[:, 0:N], func=AF.Ln), reads=["rs"], writes=["rs"])
    pr.op("act", lambda e: e.activation(out=rs[:, 0:N], in_=rs[:, 0:N], func=AF.Exp, scale=-0.5), reads=["rs"], writes=["rs"])
    if gate_ap is None:
        if wcol is None:
            pr.op("dve", lambda e: e.tensor_tensor(out=out_ap, in0=o_ap, in1=rs[:, 0:N], op=ALU.mult),
                  reads=[okey, "rs"], writes=[outkey])
        else:
            pr.op("dve", lambda e: e.scalar_tensor_tensor(out=out_ap, in0=o_ap, scalar=wcol, in1=rs[:, 0:N], op0=ALU.mult, op1=ALU.mult),
                  reads=[okey, "rs", wkey], writes=[outkey])
    else:
        if wcol is None:
            pr.op("dve", lambda e: e.tensor_tensor(out=rs[:, 0:N], in0=o_ap, in1=rs[:, 0:N], op=ALU.mult),
                  reads=[okey, "rs"], writes=["rs"])
        else:
            pr.op("dve", lambda e: e.scalar_tensor_tensor(out=rs[:, 0:N], in0=o_ap, scalar=wcol, in1=rs[:, 0:N], op0=ALU.mult, op1=ALU.mult),
                  reads=[okey, "rs", wkey], writes=["rs"])
        pr.op("dve", lambda e: e.tensor_tensor(out=out_ap, in0=rs[:, 0:N], in1=gate_ap, op=ALU.mult),
              reads=["rs", gatekey], writes=[outkey])


def l1_attn_phase(pr, c, S, fmT, tm, oT, lam_dram, dnorm_dram, lambda_init):
    pr.barrier()
    pr.reset_phase()
    NB = S // 128
    QT = pr.alloc(S, BF16)
    KT = pr.alloc(S, BF16)
    Vp = v3(pr.alloc(NB * 128, BF16), NB)
    Vp3 = [Vp, v3(pr.alloc(NB * 128, BF16), NB), v3(pr.alloc(NB * 128, BF16), NB)]
    num = pr.alloc(S, F32)
    den = pr.alloc(S, F32)
    pTs = [pr.alloc(512, BF16) for _ in range(4)]
    sq = pr.alloc(512, BF16)
    sqb = pr.alloc(512, BF16)
    rs = pr.alloc(512, F32)
    o1s = [pr.alloc(512, F32) for _ in range(2)]
    o2 = pr.alloc(512, F32)
    rs2 = pr.alloc(512, F32)
    ost = [pr.alloc(512, BF16) for _ in range(2)]
    mask2 = pr.alloc(256, BF16)
    maskd = v3(pr.alloc(4 * 512, BF16), 4)
    small = pr.alloc(16, F32)
    c.tmpmax = small[:, 0:1]
    c.phalf = small[:, 1:2]
    mq = small[:, 2:3]
    mk = small[:, 3:4]
    negM = small[:, 4:5]
    lamneg = small[:, 5:6]
    dnorm = small[:, 6:7]
    lamt = pr.alloc(256, F32)
    pr.op("pool", lambda e: e.memset(c.phalf, 0.5), writes=["phalf"])
    pr.op("pool", lambda e: e.memset(mask2, 1.0), writes=["mask2"])
    pr.op("pool", lambda e: e.affine_select(out=mask2[:, 0:128], in_=mask2[:, 0:128], pattern=[[-1, 128]], compare_op=ALU.is_ge,
                                            fill=0.0, base=0, channel_multiplier=1), reads=["mask2"], writes=["mask2"])
    pr.op("pool", lambda e: e.affine_select(out=mask2[:, 128:256], in_=mask2[:, 128:256], pattern=[[1, 128]], compare_op=ALU.is_ge,
                                            fill=0.0, base=0, channel_multiplier=-1), reads=["mask2"], writes=["mask2"])
    pr.op("pool", lambda e: e.memset(maskd, 1.0), writes=["maskd"])
    for dd in range(4):
        pr.op("pool", lambda e, dd=dd: e.affine_select(out=maskd[:, dd, :], in_=maskd[:, dd, :], pattern=[[1, 512]], compare_op=ALU.is_ge,
                                                       fill=0.0, base=-128 * dd, channel_multiplier=-1), reads=["maskd"], writes=["maskd"])
    pr.op("sp", lambda e: e.dma_start(out=lamt, in_=bcast_rows(lam_dram, 256)), writes=["lamt"], dsem="lamt")
    pr.op("sp", lambda e: e.dma_start(out=dnorm, in_=dnorm_dram.rearrange("(p o) -> p o", o=1)), writes=["dnorm"], dsem="dnorm")
    pr.op("dve", lambda e: e.tensor_tensor(out=lamt[:, 0:64], in0=lamt[:, 0:64], in1=lamt[:, 64:128], op=ALU.mult), reads=["lamt"], writes=["lamt"])
    pr.op("dve", lambda e: e.tensor_tensor(out=lamt[:, 128:192], in0=lamt[:, 128:192], in1=lamt[:, 192:256], op=ALU.mult), reads=["lamt"], writes=["lamt"])
    pr.op("dve", lambda e: e.tensor_reduce(out=small[:, 8:9], in_=lamt[:, 0:64], axis=AX.X, op=ALU.add), reads=["lamt"], writes=["lam_a"])
    pr.op("dve", lambda e: e.tensor_reduce(out=small[:, 9:10], in_=lamt[:, 128:192], axis=AX.X, op=ALU.add), reads=["lamt"], writes=["lam_b"])
    pr.op("act", lambda e: e.activation(out=small[:, 8:10], in_=small[:, 8:10], func=AF.Exp), reads=["lam_a", "lam_b"], writes=["lam_a", "lam_b"])
    pr.op("dve", lambda e: e.tensor_tensor(out=small[:, 10:11], in0=small[:, 9:10], in1=small[:, 8:9], op=ALU.subtract), reads=["lam_a", "lam_b"], writes=["lam_c"])
    pr.op("dve", lambda e: e.tensor_scalar(out=lamneg, in0=small[:, 10:11], scalar1=-float(lambda_init), scalar2=None, op0=ALU.add),
          reads=["lam_c"], writes=["lamneg"])
    pr.op("dve", lambda e: e.tensor_scalar(out=dnorm, in0=dnorm, scalar1=float(1.0 - lambda_init), scalar2=None, op0=ALU.mult),
          reads=["dnorm"], writes=["dnorm"])
    psc = [c.ps[0], c.ps[1], c.ps[2], c.ps[3]]
    pO = [c.ps[4], c.ps[5]]
    pL = [c.ps[6], c.ps[7]]
    psb = c.ps[3]
    PSB = ("sc", 3)
    oTv = oT.rearrange("c p s -> p c s")
    fmv = fmT.rearrange("c p s -> p c s")

    for h in range(4):
        pr.op("sp", lambda e, h=h: e.dma_start(out=QT, in_=fmv[:, h, :]), reads=["fmT"], writes=["QT"], dsem="ql")
        pr.op("sp", lambda e, h=h: e.dma_start(out=KT, in_=fmv[:, 4 + h, :]), reads=["fmT"], writes=["KT"], dsem="kl")
        col_maxnorm(pr, c, QT, "QT", (0, 128), S, sq, psb, mq, "mq", psbkey=PSB)
        col_maxnorm(pr, c, KT, "KT", (0, 128), S, sq, psb, mk, "mk", psbkey=PSB)
        neg_bound(pr, c, mq, mk, negM, ["mq", "mk"], "negM")
        fronts, backs = [], []
        cnt = 0
        gcnt = 0
        for bi, d in enumerate((1, 4, 16)):
            L = S // d
            nb = L // 128
            for r in range(d):
                for n0 in range(0, nb, 16):
                    n1 = min(nb, n0 + 16)
                    pr.op("sp", lambda e, r=r, d=d, nb=nb, h=h, n0=n0, n1=n1: e.dma_start(
                        out=Vp3[bi][:, r * nb + n0:r * nb + n1, :],
                        in_=tm.rearrange("(n m r) c -> r m n c", r=d, m=128)[r, :, n0:n1, h * 128:(h + 1) * 128]),
                          reads=["tm"], writes=[("Vp", bi)], dsem=("vl", bi))
            Qv = QT.rearrange("p (m r) -> p r m", r=d)
            Kv = KT.rearrange("p (m r) -> p r m", r=d)
            numv = num.rearrange("p (m r) -> p r m", r=d)
            denv = den.rearrange("p (m r) -> p r m", r=d)
            for r in range(d):
                for n0 in range(0, nb, 4):
                    nn = min(4, nb - n0)
                    gp = gcnt % 2
                    gcnt += 1
                    for i in range(nn):
                        n = n0 + i
                        sb_ = cnt % 3
                        cnt += 1

                        def front(n=n, r=r, sb_=sb_, Kv=Kv, Qv=Qv):
                            sc = psc[sb_]
                            pT = pTs[sb_]
                            lo = 0 if n > 0 else 128
                            if n > 0:
                                pr.op("pe", lambda e: e.matmul(sc[:, 0:128], lhsT=Kv[:, r, (n - 1) * 128:n * 128],
                                                               rhs=Qv[:, r, n * 128:(n + 1) * 128], start=True, stop=True),
                                      reads=["QT", "KT"], writes=[("sc", sb_)])
                            pr.op("pe", lambda e: e.matmul(sc[:, 128:256], lhsT=Kv[:, r, n * 128:(n + 1) * 128],
                                                           rhs=Qv[:, r, n * 128:(n + 1) * 128], start=True, stop=True),
                                  reads=["QT", "KT"], writes=[("sc", sb_)])
                            pr.op("act", lambda e: e.activation(out=pT[:, lo:256], in_=sc[:, lo:256], func=AF.Exp, bias=negM),
                                  reads=[("sc", sb_), "negM"], writes=[("pT", sb_)])
                            pr.op("dve", lambda e: e.tensor_tensor(out=pT[:, lo:256], in0=pT[:, lo:256], in1=mask2[:, lo:256], op=ALU.mult),
                                  reads=[("pT", sb_), "mask2"], writes=[("pT", sb_)])

                        def back(n=n, r=r, i=i, sb_=sb_, nb=nb, bi=bi, gp=gp, nn=nn, n0=n0, numv=numv, denv=denv):
                            pT = pTs[sb_]
                            Vb = Vp3[bi]
                            cs = slice(i * 128, (i + 1) * 128)
                            kO, kL = ("pO", gp), ("pL", gp)
                            if n > 0:
                                pr.op("pe", lambda e: e.matmul(pO[gp][:, cs], lhsT=Vb[:, r * nb + n - 1, :], rhs=pT[:, 0:128], start=True, stop=False),
                                      reads=[("Vp", bi), ("pT", sb_)], writes=[kO])
                            pr.op("pe", lambda e: e.matmul(pO[gp][:, cs], lhsT=Vb[:, r * nb + n, :], rhs=pT[:, 128:256], start=(n == 0), stop=True),
                                  reads=[("Vp", bi), ("pT", sb_)], writes=[kO])
                            if n > 0:
                                pr.op("pe", lambda e: e.matmul(pL[gp][:, cs], lhsT=c.ones_b, rhs=pT[:, 0:128], start=True, stop=False),
                                      reads=["ones_b", ("pT", sb_)], writes=[kL])
                            pr.op("pe", lambda e: e.matmul(pL[gp][:, cs], lhsT=c.ones_b, rhs=pT[:, 128:256], start=(n == 0), stop=True),
                                  reads=["ones_b", ("pT", sb_)], writes=[kL])
                            if i == nn - 1:
                                W = nn * 128
                                dstn = numv[:, r, n0 * 128:n0 * 128 + W]
                                dstd = denv[:, r, n0 * 128:n0 * 128 + W]
                                if bi == 0:
                                    pr.op("act", lambda e: e.activation(out=dstn, in_=pO[gp][:, 0:W], func=AF.Copy), reads=[kO], writes=["num"])
                                    pr.op("dve", lambda e: e.tensor_copy(out=dstd, in_=pL[gp][:, 0:W]), reads=[kL], writes=["den"])
                                else:
                                    pr.op("dve", lambda e: e.tensor_tensor(out=dstn, in0=pO[gp][:, 0:W], in1=dstn, op=ALU.add), reads=[kO, "num"], writes=["num"])
                                    pr.op("dve", lambda e: e.tensor_tensor(out=dstd, in0=pL[gp][:, 0:W], in1=dstd, op=ALU.add), reads=[kL, "den"], writes=["den"])

                        fronts.append(front)
                        backs.append(back)
        pipeline(fronts, backs, 2)
        for g in range(S // 512):
            s = g % 2
            cs = slice(g * 512, (g + 1) * 512)
            pr.op("dve", lambda e, cs=cs: e.reciprocal(out=den[:, cs], in_=den[:, cs]), reads=["den"], writes=["den"])
            pr.op("dve", lambda e, cs=cs, s=s: e.tensor_tensor(out=ost[s], in0=num[:, cs], in1=den[:, cs], op=ALU.mult),
                  reads=["num", "den"], writes=[("ost", s)])
            pr.op("sp", lambda e, cs=cs, s=s, h=h: e.dma_start(out=oTv[:, h, cs], in_=ost[s]), reads=[("ost", s)], writes=["oT"], dsem=("ostd", s))

    for h in range(4):
        pr.op("sp", lambda e, h=h: e.dma_start(out=QT, in_=fmv[:, 8 + h, :]), reads=["fmT"], writes=["QT"], dsem="ql")
        pr.op("sp", lambda e, h=h: e.dma_start(out=KT, in_=fmv[:, 12 + h, :]), reads=["fmT"], writes=["KT"], dsem="kl")
        for n0 in range(0, NB, 16):
            n1 = min(NB, n0 + 16)
            pr.op("sp", lambda e, h=h, n0=n0, n1=n1: e.dma_start(
                out=Vp[:, n0:n1, :], in_=tm.rearrange("(n p) c -> p n c", p=128)[:, n0:n1, 512 + h * 128:512 + (h + 1) * 128]),
                  reads=["tm"], writes=[("Vp", 0)], dsem="vl")
        negMs = [small[:, 11:12], small[:, 12:13]]
        for cp in range(2):
            col_maxnorm(pr, c, QT, "QT", (64 * cp, 64 * cp + 64), S, sq, psb, mq, "mq", psbkey=PSB)
            col_maxnorm(pr, c, KT, "KT", (64 * cp, 64 * cp + 64), S, sq, psb, mk, "mk", psbkey=PSB)
            neg_bound(pr, c, mq, mk, negMs[cp], ["mq", "mk"], ("negMd", cp))
        fronts, backs = [], []
        ucnt = 0
        for qg in range(S // 512):
            qs = slice(qg * 512, (qg + 1) * 512)
            nkb = 4 * (qg + 1)
            for kb in range(nkb):
                slot = ucnt % 2
                ucnt += 1

                def front(kb=kb, slot=slot, qs=qs, dd=kb - 4 * qg):
                    sbs = [2 * slot, 2 * slot + 1]
                    for cp in range(2):
                        rows = slice(64 * cp, 64 * cp + 64)
                        sc = psc[sbs[cp]]
                        wr = [("sc", sbs[0]), ("sc", sbs[1])] if cp == 0 else [("sc", sbs[1])]
                        pr.op("pe", lambda e: e.matmul(sc[:, :], lhsT=KT[rows, kb * 128:(kb + 1) * 128], rhs=QT[rows, qs], start=True, stop=True),
                              reads=["QT", "KT"], writes=wr)
                    for cp in range(2):
                        sc = psc[sbs[cp]]
                        pT = pTs[sbs[cp]]
                        pr.op("act", lambda e: e.activation(out=pT, in_=sc[:, :], func=AF.Exp, bias=negMs[cp]),
                              reads=[("sc", sbs[cp]), ("negMd", cp)], writes=[("pT", sbs[cp])])
                        if dd >= 0:
                            pr.op("pool", lambda e: e.tensor_tensor(out=pT, in0=pT, in1=maskd[:, dd, :], op=ALU.mult),
                                  reads=[("pT", sbs[cp]), "maskd"], writes=[("pT", sbs[cp])])

                def back(kb=kb, slot=slot, nkb=nkb, qg=qg, qs=qs, h=h):
                    sbs = [2 * slot, 2 * slot + 1]
                    for cp in range(2):
                        pT = pTs[sbs[cp]]
                        rd = [("pT", sbs[0]), ("pT", sbs[1])] if cp == 0 else [("pT", sbs[1])]
                        pr.op("pe", lambda e: e.matmul(pO[cp][:, :], lhsT=Vp[:, kb, :], rhs=pT, start=(kb == 0), stop=(kb == nkb - 1)),
                              reads=[("Vp", 0)] + rd, writes=[("pO", cp)])
                        pr.op("pe", lambda e: e.matmul(pL[cp][:, :], lhsT=c.ones_b, rhs=pT, start=(kb == 0), stop=(kb == nkb - 1)),
                              reads=["ones_b", ("pT", sbs[cp])], writes=[("pL", cp)])
                    if kb != nkb - 1:
                        return
                    oa = o1s[qg % 2]
                    ka = ("o1", qg % 2)
                    pr.op("dve", lambda e: e.reciprocal(out=rs2, in_=pL[0][:, :]), reads=[("pL", 0)], writes=["rs2"])
                    pr.op("dve", lambda e: e.tensor_tensor(out=oa, in0=pO[0][:, :], in1=rs2, op=ALU.mult), reads=[("pO", 0), "rs2"], writes=[ka])
                    pr.op("dve", lambda e: e.reciprocal(out=rs2, in_=pL[1][:, :]), reads=[("pL", 1)], writes=["rs2"])
                    pr.op("dve", lambda e: e.tensor_tensor(out=o2, in0=pO[1][:, :], in1=rs2, op=ALU.mult), reads=[("pO", 1), "rs2"], writes=["o2"])
                    pr.op("dve", lambda e: e.scalar_tensor_tensor(out=oa, in0=o2, scalar=lamneg, in1=oa, op0=ALU.mult, op1=ALU.add),
                          reads=[ka, "o2", "lamneg"], writes=[ka])
                    s = qg % 2
                    fm_epilogue(pr, c, oa, ka, 512, None, None, dnorm, "dnorm", sqb, psb, rs, ost[s], ("ost", s), psbkey=PSB)
                    pr.op("sp", lambda e: e.dma_start(out=oTv[:, 4 + h, qs], in_=ost[s]), reads=[("ost", s)], writes=["oT"], dsem=("ostd", s))

                fronts.append(front)
                backs.append(back)
        pipeline(fronts, backs, 1)


def l0_attn_phase(pr, c, S, fmT, tm, tm32, oT, convT, alog_d, dtb_d, gnorm_d, rmask_d, rqsc_d, rksc_d):
    pr.barrier()
    pr.reset_phase()
    NB = S // 128
    NG4 = NB // 4
    raw = pr.alloc(S, BF16)
    acc = pr.alloc(S, F32)
    qh = pr.alloc(S, BF16)
    kh = pr.alloc(S, BF16)
    vtok = v3(pr.alloc(S, BF16), NB)
    kdec = v3(pr.alloc(S, BF16), NB)
    zt = pr.alloc(S, BF16)
    ba = v3(pr.alloc(NB * 8, F32), NB)
    gcol = pr.alloc(NB, F32)
    negb = pr.alloc(NB, F32)
    egc = pr.alloc(NB, F32)
    edec = pr.alloc(NB, F32)
    egl = pr.alloc(NB, F32)
    tmpn = pr.alloc(NB, F32)
    cw = pr.alloc(4, F32)
    small = pr.alloc(16, F32)
    alog = small[:, 0:4]
    dtb = small[:, 4:8]
    gnorm = small[:, 8:9]
    rksc = small[:, 12:16]
    U4i = pr.alloc(512, F32)
    U4s = pr.alloc(512, F32)
    I4 = pr.alloc(512, F32)
    Lst = pr.alloc(128, F32)
    Gt = pr.alloc(512, F32)
    Dx = pr.alloc(512, F32)
    Dm = pr.alloc(512, F32)
    DmS = pr.alloc(512, F32)
    EGs = pr.alloc(512, F32)
    XY = [[pr.alloc(512, F32), pr.alloc(512, F32)] for _ in range(2)]
    Pm = pr.alloc(512, F32)
    PTb = pr.alloc(512, BF16)
    MTb = pr.alloc(512, BF16)
    qtl = pr.alloc(512, BF16)
    PTb2 = [PTb, pr.alloc(512, BF16)]
    MTb2 = [MTb, pr.alloc(512, BF16)]
    qtl2 = [qtl, pr.alloc(512, BF16)]
    Rm = pr.alloc(128, BF16)
    vnew = pr.alloc(128, BF16)
    sq = pr.alloc(512, BF16)
    sqb = pr.alloc(512, BF16)
    rs = pr.alloc(512, F32)
    S_f = pr.alloc(128, F32)
    S_b = pr.alloc(128, BF16)
    o_sb = pr.alloc(512, F32)
    gate = pr.alloc(512, F32)
    ost = [pr.alloc(512, BF16) for _ in range(2)]
    rmask = pr.alloc(512, F32)
    rqsc = pr.alloc(512, F32)
    ps = c.ps
    oTv = oT.rearrange("c p s -> p c s")
    fmv = fmT.rearrange("c p s -> p c s")
    K0, K1, K2, K3, K4, K5, K6 = "ps0", "ps1", "ps2", "ps3", "ps4", "ps5", "ps6"

    pr.op("pool", lambda e: e.memset(U4i, 1.0), writes=["U4i"])
    pr.op("pool", lambda e: e.memset(U4s, 1.0), writes=["U4s"])
    pr.op("pool", lambda e: e.memset(Lst, 1.0), writes=["Lst"])
    for i in range(4):
        cs = slice(i * 128, (i + 1) * 128)
        pr.op("pool", lambda e, cs=cs: e.affine_select(out=U4i[:, cs], in_=U4i[:, cs], pattern=[[1, 128]], compare_op=ALU.is_ge,
                                                       fill=0.0, base=0, channel_multiplier=-1), reads=["U4i"], writes=["U4i"])
        pr.op("pool", lambda e, cs=cs: e.affine_select(out=U4s[:, cs], in_=U4s[:, cs], pattern=[[1, 128]], compare_op=ALU.is_ge,
                                                       fill=0.0, base=-1, channel_multiplier=-1), reads=["U4s"], writes=["U4s"])
        pr.op("dve", lambda e, cs=cs: e.tensor_copy(out=I4[:, cs], in_=c.ident_f), reads=["ident_f"], writes=["I4"])
    pr.op("pool", lambda e: e.affine_select(out=Lst, in_=Lst, pattern=[[-1, 128]], compare_op=ALU.is_ge,
                                            fill=0.0, base=-1, channel_multiplier=1), reads=["Lst"], writes=["Lst"])
    pr.op("sp", lambda e: e.dma_start(out=alog, in_=bcast_rows(alog_d, 4)), writes=["alog"], dsem="c1")
    pr.op("sp", lambda e: e.dma_start(out=dtb, in_=bcast_rows(dtb_d, 4)), writes=["dtb"], dsem="c2")
    pr.op("sp", lambda e: e.dma_start(out=gnorm, in_=gnorm_d.rearrange("(p o) -> p o", o=1)), writes=["gnorm"], dsem="c3")
    pr.op("sp", lambda e: e.dma_start(out=rksc, in_=rksc_d[:, :]), writes=["rksc"], dsem="c4")
    pr.op("sp", lambda e: e.dma_start(out=ba, in_=tm32.rearrange("(n p) c -> p n c", p=128)), reads=["tm32"], writes=["ba"], dsem="c5")
    pr.op("act", lambda e: e.activation(out=alog, in_=alog, func=AF.Exp), reads=["alog"], writes=["alog"])
    pr.op("dve", lambda e: e.tensor_scalar(out=alog, in0=alog, scalar1=-1.0, scalar2=None, op0=ALU.mult), reads=["alog"], writes=["alog"])

    def conv(ch):
        pr.op("sp", lambda e: e.dma_start(out=raw, in_=fmv[:, ch, :]), reads=["fmT"], writes=["raw"], dsem="rawl")
        pr.op("sp", lambda e: e.dma_start(out=cw, in_=convT[ch * 128:(ch + 1) * 128, :]), writes=["cw"], dsem="cwl")
        pr.op("dve", lambda e: e.tensor_scalar(out=acc, in0=raw, scalar1=cw[:, 3:4], scalar2=None, op0=ALU.mult),
              reads=["raw", "cw"], writes=["acc"])
        for sh in (1, 2, 3):
            pr.op("dve", lambda e, sh=sh: e.scalar_tensor_tensor(out=acc[:, sh:S], in0=raw[:, 0:S - sh], scalar=cw[:, 3 - sh:4 - sh],
                                                                 in1=acc[:, sh:S], op0=ALU.mult, op1=ALU.add),
                  reads=["raw", "cw", "acc"], writes=["acc"])
        pr.op("act", lambda e: e.activation(out=acc, in_=acc, func=AF.Silu), reads=["acc"], writes=["acc"])

    def l2norm(outb, outkey, scale):
        for g in range(S // 512):
            cs = slice(g * 512, (g + 1) * 512)
            pr.op("pool", lambda e, cs=cs: e.tensor_tensor(out=sq, in0=acc[:, cs], in1=acc[:, cs], op=ALU.mult), reads=["acc"], writes=["sq"])
            pr.op("pe", lambda e: e.matmul(ps[3][:, :], lhsT=c.ones_b, rhs=sq, start=True, stop=True), reads=["sq", "ones_b"], writes=[K3])
            pr.op("dve", lambda e: e.tensor_scalar(out=rs, in0=ps[3][:, :], scalar1=EPS, scalar2=None, op0=ALU.add), reads=[K3], writes=["rs"])
            pr.op("act", lambda e: e.activation(out=rs, in_=rs, func=AF.Ln), reads=["rs"], writes=["rs"])
            pr.op("act", lambda e: e.activation(out=rs, in_=rs, func=AF.Exp, scale=-0.5), reads=["rs"], writes=["rs"])
            pr.op("dve", lambda e, cs=cs: e.scalar_tensor_tensor(out=outb[:, cs], in0=acc[:, cs], scalar=float(scale), in1=rs, op0=ALU.mult, op1=ALU.mult),
                  reads=["acc", "rs"], writes=[outkey])

    def epilogue(gq, chunk, wcol, wkey, src_ps=None, src_key=None):
        cs = slice(gq * 512, (gq + 1) * 512)
        s_ = gq % 2
        if src_ps is None:
            src_ps, src_key = ps[0], K0
        pr.op("act", lambda e: e.activation(out=o_sb, in_=src_ps[:, :], func=AF.Copy), reads=[src_key], writes=["o_sb"])
        pr.op("act", lambda e: e.activation(out=gate, in_=zt[:, cs], func=AF.Silu), reads=["zt"], writes=["gate"])
        fm_epilogue(pr, c, o_sb, "o_sb", 512, gate, "gate", wcol, wkey, sqb, ps[3], rs, ost[s_], ("ost", s_), psbkey=K3)
        pr.op("sp", lambda e: e.dma_start(out=oTv[:, chunk, cs], in_=ost[s_]), reads=[("ost", s_)], writes=["oT"], dsem=("ostd", s_))

    psT1 = ps[1][:, :].bitcast(BF16)

    for h in range(4):
        pr.op("act", lambda e, h=h: e.activation(out=tmpn, in_=ba[:, :, h], func=AF.Exp, scale=-1.0), reads=["ba"], writes=["tmpn"])
        pr.op("dve", lambda e: e.tensor_scalar(out=tmpn, in0=tmpn, scalar1=1.0, scalar2=None, op0=ALU.add), reads=["tmpn"], writes=["tmpn"])
        pr.op("dve", lambda e: e.reciprocal(out=tmpn, in_=tmpn), reads=["tmpn"], writes=["tmpn"])
        pr.op("dve", lambda e: e.tensor_scalar(out=negb, in0=tmpn, scalar1=-1.0, scalar2=None, op0=ALU.mult), reads=["tmpn"], writes=["negb"])
        pr.op("act", lambda e, h=h: e.activation(out=tmpn, in_=ba[:, :, 4 + h], func=AF.Exp, bias=dtb[:, h:h + 1]), reads=["ba", "dtb"], writes=["tmpn"])
        pr.op("dve", lambda e: e.tensor_scalar(out=tmpn, in0=tmpn, scalar1=1.0, scalar2=None, op0=ALU.add), reads=["tmpn"], writes=["tmpn"])
        pr.op("act", lambda e: e.activation(out=tmpn, in_=tmpn, func=AF.Ln), reads=["tmpn"], writes=["tmpn"])
        pr.op("dve", lambda e, h=h: e.tensor_scalar(out=gcol, in0=tmpn, scalar1=alog[:, h:h + 1], scalar2=None, op0=ALU.mult),
              reads=["tmpn", "alog"], writes=["gcol"])
        pr.op("pe", lambda e: e.matmul(ps[4][:, 0:NB], lhsT=U4i[:, 0:128], rhs=gcol, start=True, stop=True), reads=["U4i", "gcol"], writes=[K4])
        pr.op("pe", lambda e: e.matmul(ps[5][:, 0:NB], lhsT=c.ones_f, rhs=gcol, start=True, stop=True), reads=["ones_f", "gcol"], writes=[K5])
        pr.op("act", lambda e: e.activation(out=egc, in_=ps[4][:, 0:NB], func=AF.Exp), reads=[K4], writes=["egc"])
        pr.op("act", lambda e: e.activation(out=egl, in_=ps[5][:, 0:NB], func=AF.Exp), reads=[K5], writes=["egl"])
        pr.op("dve", lambda e: e.tensor_copy(out=tmpn, in_=ps[4][:, 0:NB]), reads=[K4], writes=["tmpn"])
        pr.op("dve", lambda e: e.tensor_tensor(out=tmpn, in0=ps[5][:, 0:NB], in1=tmpn, op=ALU.subtract), reads=[K5, "tmpn"], writes=["tmpn"])
        pr.op("act", lambda e: e.activation(out=edec, in_=tmpn, func=AF.Exp), reads=["tmpn"], writes=["edec"])
        conv(h)
        l2norm(qh, "qh", 128 ** -0.5)
        conv(4 + h)
        l2norm(kh, "kh", 1.0)
        for g4 in range(NG4):
            for i in range(4):
                n = 4 * g4 + i
                pr.op("pe", lambda e, n=n, i=i: e.transpose(out=psT1[:, i * 128:(i + 1) * 128], in_=kh[:, n * 128:(n + 1) * 128], identity=c.ident_b),
                      reads=["kh", "ident_b"], writes=[K1])
            for i in range(4):
                n = 4 * g4 + i
                pr.op("dve", lambda e, n=n, i=i: e.tensor_scalar(out=kdec[:, n, :], in0=psT1[:, i * 128:(i + 1) * 128], scalar1=edec[:, n:n + 1],
                                                                 scalar2=None, op0=ALU.mult), reads=[K1, "edec"], writes=["kdec"])
        conv(8 + h)
        for g4 in range(NG4):
            for i in range(4):
                n = 4 * g4 + i
                pr.op("pe", lambda e, n=n, i=i: e.transpose(out=ps[2][:, i * 128:(i + 1) * 128], in_=acc[:, n * 128:(n + 1) * 128], identity=c.ident_f),
                      reads=["acc", "ident_f"], writes=[K2])
            pr.op("act", lambda e, g4=g4: e.activation(out=vtok[:, 4 * g4:4 * g4 + 4, :], in_=v3(ps[2][:, :], 4), func=AF.Copy), reads=[K2], writes=["vtok"])
        pr.op("sp", lambda e, h=h: e.dma_start(out=zt, in_=fmv[:, 12 + h, :]), reads=["fmT"], writes=["zt"], dsem="ztl")
        pr.op("pool", lambda e: e.memset(S_f, 0.0), writes=["S_f"])
        pr.op("pool", lambda e: e.memset(S_b, 0.0), writes=["S_b"])
        def pre_gen(g4, b):
            gs = slice(g4 * 512, (g4 + 1) * 512)
            kP, kM, kQ = ("PTb", b), ("MTb", b), ("qtl", b)
            for i in range(4):
                n = 4 * g4 + i
                pr.op("dve", lambda e, n=n, i=i: e.tensor_scalar(out=Gt[:, i * 128:(i + 1) * 128], in0=U4i[:, 0:128], scalar1=gcol[:, n:n + 1],
                                                                 scalar2=None, op0=ALU.mult), reads=["U4i", "gcol"], writes=["Gt"])
            yield
            for i in range(4):
                cs = slice(i * 128, (i + 1) * 128)
                pr.op("pe", lambda e, cs=cs: e.matmul(ps[0][:, cs], lhsT=Lst, rhs=Gt[:, cs], start=True, stop=True), reads=["Lst", "Gt"], writes=[K0])
                pr.op("pe", lambda e, cs=cs: e.matmul(ps[1][:, cs], lhsT=c.ones_f, rhs=Gt[:, cs], start=True, stop=True), reads=["ones_f", "Gt"], writes=[K1])
            yield
            pr.op("act", lambda e: e.activation(out=Dx, in_=ps[0][:, :], func=AF.Exp), reads=[K0], writes=["Dx"])
            pr.op("act", lambda e: e.activation(out=EGs, in_=ps[1][:, :], func=AF.Exp), reads=[K1], writes=["EGs"])
            yield
            pr.op("dve", lambda e: e.tensor_tensor(out=Dm, in0=Dx, in1=U4i, op=ALU.mult), reads=["Dx", "U4i"], writes=["Dm"])
            pr.op("pool", lambda e: e.tensor_tensor(out=DmS, in0=Dx, in1=U4s, op=ALU.mult), reads=["Dx", "U4s"], writes=["DmS"])
            pr.op("dve", lambda e: e.tensor_tensor(out=qtl2[b], in0=qh[:, gs], in1=EGs, op=ALU.mult), reads=["qh", "EGs"], writes=[kQ])
            for i in range(4):
                n = 4 * g4 + i
                cs = slice(i * 128, (i + 1) * 128)
                ns = slice(n * 128, (n + 1) * 128)
                pr.op("pe", lambda e, cs=cs, ns=ns: e.matmul(ps[0][:, cs], lhsT=kh[:, ns], rhs=kh[:, ns], start=True, stop=True), reads=["kh"], writes=[K0])
                pr.op("pe", lambda e, cs=cs, ns=ns: e.matmul(ps[1][:, cs], lhsT=kh[:, ns], rhs=qh[:, ns], start=True, stop=True), reads=["kh", "qh"], writes=[K1])
            yield
            pr.op("dve", lambda e: e.tensor_tensor(out=MTb2[b], in0=ps[1][:, :], in1=Dm, op=ALU.mult), reads=[K1, "Dm"], writes=[kM])
            X, Y = XY[0]
            for i in range(4):
                n = 4 * g4 + i
                cs = slice(i * 128, (i + 1) * 128)
                pr.op("dve", lambda e, cs=cs, n=n: e.scalar_tensor_tensor(out=X[:, cs], in0=ps[0][:, cs], scalar=negb[:, n:n + 1], in1=DmS[:, cs],
                                                                          op0=ALU.mult, op1=ALU.mult), reads=[K0, "negb", "DmS"], writes=["X0"])
            yield
            for i in range(4):
                cs = slice(i * 128, (i + 1) * 128)
                pr.op("pe", lambda e, cs=cs: e.transpose(out=ps[5][:, cs], in_=X[:, cs], identity=c.ident_f), reads=["X0", "ident_f"], writes=[K5])
            pr.op("dve", lambda e: e.tensor_tensor(out=Pm, in0=X, in1=I4, op=ALU.add), reads=["X0", "I4"], writes=["Pm"])
            yield
            pr.op("act", lambda e: e.activation(out=Y, in_=ps[5][:, :], func=AF.Copy), reads=[K5], writes=["Y0"])
            yield
            cur = 0
            for m in range(6):
                Xc, Yc = XY[cur]
                Xn, Yn = XY[1 - cur]
                kc = ("X%d" % cur, "Y%d" % cur)
                kn_ = ("X%d" % (1 - cur), "Y%d" % (1 - cur))
                for i in range(4):
                    cs = slice(i * 128, (i + 1) * 128)
                    pr.op("pe", lambda e, cs=cs: e.matmul(ps[5][:, cs], lhsT=Xc[:, cs], rhs=Yc[:, cs], start=True, stop=True),
                          reads=[kc[0], kc[1]], writes=[K5])
                if m < 5:
                    for i in range(4):
                        cs = slice(i * 128, (i + 1) * 128)
                        pr.op("pe", lambda e, cs=cs: e.matmul(ps[4][:, cs], lhsT=Yc[:, cs], rhs=Xc[:, cs], start=True, stop=True),
                              reads=[kc[0], kc[1]], writes=[K4])
                yield
                pr.op("act", lambda e: e.activation(out=Yn, in_=ps[5][:, :], func=AF.Copy), reads=[K5], writes=[kn_[1]])
                if m < 5:
                    pr.op("dve", lambda e: e.tensor_copy(out=Xn, in_=ps[4][:, :]), reads=[K4], writes=[kn_[0]])
                yield
                for i in range(4):
                    cs = slice(i * 128, (i + 1) * 128)
                    pr.op("pe", lambda e, cs=cs: e.matmul(ps[6][:, cs], lhsT=Yn[:, cs], rhs=Pm[:, cs], start=True, stop=True),
                          reads=[kn_[1], "Pm"], writes=[K6])
                yield
                pr.op("dve", lambda e: e.tensor_tensor(out=Pm, in0=ps[6][:, :], in1=Pm, op=ALU.add), reads=[K6, "Pm"], writes=["Pm"])
                yield
                cur = 1 - cur
            pr.op("act", lambda e: e.activation(out=PTb2[b], in_=Pm, func=AF.Copy), reads=["Pm"], writes=[kP])
            yield

        def scan_gen(g4, b):
            kP, kM, kQ = ("PTb", b), ("MTb", b), ("qtl", b)
            for i in range(4):
                n = 4 * g4 + i
                cs = slice(i * 128, (i + 1) * 128)
                ns = slice(n * 128, (n + 1) * 128)
                pr.op("pe", lambda e: e.matmul(ps[7][:, 0:128], lhsT=kh[:, ns], rhs=S_b, start=True, stop=True), reads=["kh", "S_b"], writes=[("ps7", 0)])
                yield
                pr.op("dve", lambda e: e.scalar_tensor_tensor(out=Rm, in0=ps[7][:, 0:128], scalar=egc[:, n:n + 1], in1=vtok[:, n, :],
                                                              op0=ALU.mult, op1=ALU.subtract), reads=[("ps7", 0), "egc", "vtok"], writes=["Rm"])
                yield
                pr.op("pe", lambda e: e.matmul(ps[7][:, 128:256], lhsT=PTb2[b][:, cs], rhs=Rm, start=True, stop=True), reads=[kP, "Rm"], writes=[("ps7", 1)])
                yield
                pr.op("dve", lambda e: e.tensor_scalar(out=vnew, in0=ps[7][:, 128:256], scalar1=negb[:, n:n + 1], scalar2=None, op0=ALU.mult),
                      reads=[("ps7", 1), "negb"], writes=["vnew"])
                yield
                pr.op("pe", lambda e: e.matmul(ps[2][:, cs], lhsT=S_b, rhs=qtl2[b][:, cs], start=True, stop=False), reads=["S_b", kQ], writes=[K2])
                pr.op("pe", lambda e: e.matmul(ps[2][:, cs], lhsT=vnew, rhs=MTb2[b][:, cs], start=False, stop=True), reads=["vnew", kM], writes=[K2])
                pr.op("pe", lambda e: e.matmul(ps[7][:, 256:384], lhsT=kdec[:, n, :], rhs=vnew, start=True, stop=True), reads=["kdec", "vnew"], writes=[("ps7", 2)])
                yield
                pr.op("dve", lambda e: e.scalar_tensor_tensor(out=S_f, in0=S_f, scalar=egl[:, n:n + 1], in1=ps[7][:, 256:384],
                                                              op0=ALU.mult, op1=ALU.add), reads=["S_f", "egl", ("ps7", 2)], writes=["S_f"])
                pr.op("act", lambda e: e.activation(out=S_b, in_=S_f, func=AF.Copy), reads=["S_f"], writes=["S_b"])
                yield
            epilogue(g4, h, gnorm, "gnorm", src_ps=ps[2], src_key=K2)
            yield

        for g4 in range(NG4 + 1):
            gens = []
            if g4 < NG4:
                gens.append(pre_gen(g4, g4 % 2))
            if g4 >= 1:
                gens.append(scan_gen(g4 - 1, (g4 - 1) % 2))
            while gens:
                for gg in list(gens):
                    try:
                        next(gg)
                    except StopIteration:
                        gens.remove(gg)

    for h in range(4):
        rows = slice(64 * (h % 2), 64 * (h % 2) + 64)
        gam = 1.0 - 2.0 ** (-5.0 - h)
        pr.op("sp", lambda e, h=h: e.dma_start(out=qh, in_=fmv[:, 16 + h // 2, :]), reads=["fmT"], writes=["qh"], dsem="ql")
        pr.op("sp", lambda e, h=h: e.dma_start(out=kh, in_=fmv[:, 18 + h // 2, :]), reads=["fmT"], writes=["kh"], dsem="kl")
        pr.op("sp", lambda e, h=h: e.dma_start(out=zt, in_=fmv[:, 20 + h, :]), reads=["fmT"], writes=["zt"], dsem="ztl")
        pr.op("sp", lambda e, h=h: e.dma_start(out=rmask, in_=rmask_d[h, :, :]), writes=["rmask"], dsem="rml")
        pr.op("sp", lambda e, h=h: e.dma_start(out=rqsc, in_=rqsc_d[h, :, :]), writes=["rqsc"], dsem="rql")
        for n0 in range(0, NB, 16):
            n1 = min(NB, n0 + 16)
            pr.op("sp", lambda e, h=h, n0=n0, n1=n1: e.dma_start(
                out=vtok[:, n0:n1, :], in_=tm.rearrange("(n p) c -> p n c", p=128)[:, n0:n1, h * 128:(h + 1) * 128]),
                  reads=["tm"], writes=["vtok"], dsem="vl")
        for g4 in range(NG4):
            for i in range(4):
                n = 4 * g4 + i
                pr.op("pe", lambda e, n=n, i=i: e.transpose(out=psT1[:, i * 64:(i + 1) * 64], in_=kh[rows, n * 128:(n + 1) * 128],
                                                            identity=c.ident_b[rows, rows]), reads=["kh", "ident_b"], writes=[K1])
            pr.op("dve", lambda e, g4=g4, h=h: e.tensor_scalar(out=kdec[:, 4 * g4:4 * g4 + 4, 0:64], in0=v3(psT1[:, 0:256], 4), scalar1=rksc[:, h:h + 1],
                                                               scalar2=None, op0=ALU.mult), reads=[K1, "rksc"], writes=["kdec"])
        pr.op("pool", lambda e: e.memset(S_f, 0.0), writes=["S_f"])
        pr.op("pool", lambda e: e.memset(S_b, 0.0), writes=["S_b"])
        for g4 in range(NG4):
            gs = slice(g4 * 512, (g4 + 1) * 512)
            for i in range(4):
                n = 4 * g4 + i
                cs = slice(i * 128, (i + 1) * 128)
                ns = slice(n * 128, (n + 1) * 128)
                pr.op("pe", lambda e, cs=cs, ns=ns: e.matmul(ps[2][:, cs], lhsT=kh[rows, ns], rhs=qh[rows, ns], start=True, stop=True), reads=["kh", "qh"], writes=[K2])
            pr.op("dve", lambda e: e.tensor_tensor(out=PTb, in0=ps[2][:, :], in1=rmask, op=ALU.mult), reads=[K2, "rmask"], writes=[("PTb", 0)])
            pr.op("dve", lambda e, gs=gs: e.tensor_tensor(out=qtl[rows, :], in0=qh[rows, gs], in1=rqsc[rows, :], op=ALU.mult), reads=["qh", "rqsc"], writes=[("qtl", 0)])
            for i in range(4):
                n = 4 * g4 + i
                cs = slice(i * 128, (i + 1) * 128)
                pr.op("pe", lambda e, cs=cs: e.matmul(ps[0][:, cs], lhsT=S_b[rows, :], rhs=qtl[rows, cs], start=True, stop=False), reads=["S_b", ("qtl", 0)], writes=[K0])
                pr.op("pe", lambda e, cs=cs, n=n: e.matmul(ps[0][:, cs], lhsT=vtok[:, n, :], rhs=PTb[:, cs], start=False, stop=True), reads=["vtok", ("PTb", 0)], writes=[K0])
                pr.op("pe", lambda e, n=n: e.matmul(ps[7][rows, 0:128], lhsT=kdec[:, n, 0:64], rhs=vtok[:, n, :], start=True, stop=True), reads=["kdec", "vtok"], writes=[("ps7", 0)])
                pr.op("dve", lambda e: e.scalar_tensor_tensor(out=S_f[rows, :], in0=S_f[rows, :], scalar=float(gam ** 128), in1=ps[7][rows, 0:128],
                                                              op0=ALU.mult, op1=ALU.add), reads=["S_f", ("ps7", 0)], writes=["S_f"])
                pr.op("act", lambda e: e.activation(out=S_b[rows, :], in_=S_f[rows, :], func=AF.Copy), reads=["S_f"], writes=["S_b"])
            epilogue(g4, 4 + h, None, None)


ROPE_THETA = 10000.0


def rope_np(S, dim):
    inv = (ROPE_THETA ** (-np.arange(0, dim, 2, dtype=np.float32) / np.float32(dim))).astype(np.float32)
    ang = np.arange(S, dtype=np.float32)[:, None] * inv[None, :]
    return np.cos(ang).astype(np.float32), np.sin(ang).astype(np.float32)


def rope_table(S, dim, reps, scale):
    cos, sin = rope_np(S, dim)
    half = dim // 2
    cl = np.concatenate([cos.T, cos.T], axis=0)
    sl = np.concatenate([-sin.T, sin.T], axis=0)
    cl = np.tile(cl, (reps, 1)) * np.float32(scale)
    sl = np.tile(sl, (reps, 1)) * np.float32(scale)
    return np.stack([cl, sl], 0).astype(np.float32)


def perm_cols(col0, nheads, dim):
    idx = []
    half = dim // 2
    for h in range(nheads):
        for f in range(dim):
            idx.append(col0 + h * dim + (f + half) % dim)
    return np.array(idx)


def host_consts(S, weights):
    c = {"ident": np.eye(128, dtype=np.float32)}
    w1 = weights["l1_w_in"]
    c["l1_w_ext"] = np.ascontiguousarray(np.concatenate(
        [w1, w1[:, perm_cols(0, 4, 128)], w1[:, perm_cols(512, 4, 128)],
         w1[:, perm_cols(1536, 8, 64)], w1[:, perm_cols(2048, 8, 64)]], axis=1))
    c["l1_tabs"] = np.ascontiguousarray(np.stack([
        rope_table(S, 128, 1, 128 ** -0.5), rope_table(S, 128, 1, 1.0),
        rope_table(S, 64, 2, 64 ** -0.5), rope_table(S, 64, 2, 1.0)], 0))
    w0 = weights["l0_w_in"]
    c["l0_w_ext"] = np.ascontiguousarray(np.concatenate([w0, w0[:, perm_cols(2056, 4, 64)], w0[:, perm_cols(2312, 4, 64)]], axis=1))
    c["l0_tabs"] = np.ascontiguousarray(np.stack([rope_table(S, 64, 2, 64 ** -0.5), rope_table(S, 64, 2, 1.0)], 0))
    c["l0_convT"] = np.ascontiguousarray(weights["l0_conv_w"].T)
    idx = np.arange(128, dtype=np.float64)
    rm = np.zeros((4, 128, 512), np.float32)
    rq = np.zeros((4, 128, 512), np.float32)
    rk = np.zeros((128, 4), np.float32)
    for h in range(4):
        lg = np.log(1.0 - 2.0 ** (-5.0 - h))
        rel = idx[None, :] - idx[:, None]
        m = np.where(rel >= 0, np.exp(np.where(rel >= 0, rel, 0.0) * lg), 0.0)
        rm[h] = np.tile(m, (1, 4))
        rq[h] = np.tile(np.exp((idx + 1.0) * lg)[None, :], (128, 4))
        rk[:, h] = np.exp((127.0 - idx) * lg)
    c["l0_rmask"] = rm
    c["l0_rqsc"] = rq
    c["l0_rksc"] = rk
    c["l1_lam"] = np.ascontiguousarray(np.concatenate(
        [weights["l1_lambda_q1"], weights["l1_lambda_k1"], weights["l1_lambda_q2"], weights["l1_lambda_k2"]]))
    return c


W_NAMES = ["l0_ffn1_norm", "l0_ffn1_w_up", "l0_ffn1_w_down", "l0_mix_norm", "l0_w_in", "l0_conv_w", "l0_a_log",
           "l0_dt_bias", "l0_gdn_norm", "l0_w_out", "l0_ffn2_norm", "l0_ffn2_w_up", "l0_ffn2_w_down",
           "l1_ffn1_norm", "l1_ffn1_w_up", "l1_ffn1_w_down", "l1_mix_norm", "l1_w_in", "l1_lambda_q1", "l1_lambda_k1",
           "l1_lambda_q2", "l1_lambda_k2", "l1_diff_norm", "l1_w_out", "l1_ffn2_norm", "l1_ffn2_w_up", "l1_ffn2_w_down",
           "final_norm"]


def build_program(S, phases, shapes):
    nc = bass.Bass("TRN2", target_bir_lowering=False)
    stack = ExitStack()
    pr = Prog(nc, stack)
    c = Ctx()
    c.dram = {}
    for name, shp in shapes.items():
        c.dram[name] = nc.dram_tensor(name, list(shp), F32, kind="ExternalInput").ap()
    x_in = nc.dram_tensor("x", [S, D], F32, kind="ExternalInput").ap()
    out = nc.dram_tensor("out", [S, D], F32, kind="ExternalOutput").ap()
    xs = nc.dram_tensor("xstream", [S, D], F32, kind="Internal").ap()
    with stack:
        pr.init_sbuf()
        setup_common(pr, c)
        src = x_in
        w = c.dram
        for i, ph in enumerate(phases):
            last = (i == len(phases) - 1)
            if ph in ("l0_ffn1", "l0_ffn2", "l1_ffn1", "l1_ffn2"):
                fin = w["final_norm"] if ph == "l1_ffn2" else None
                if fin is None and last:
                    ffn_phase(pr, c, ph, src, out, w[ph + "_w_up"], w[ph + "_w_down"], w[ph + "_norm"], S)
                else:
                    ffn_phase(pr, c, ph, src, xs, w[ph + "_w_up"], w[ph + "_w_down"], w[ph + "_norm"], S,
                              fin=fin, out_final=out)
                src = xs
            elif ph == "l0_mix":
                fmT = nc.dram_tensor("fmT0", [24, 128, S], BF16, kind="Internal").ap()
                tm = nc.dram_tensor("tm0", [S, 512], BF16, kind="Internal").ap()
                tm32 = nc.dram_tensor("tm32_0", [S, 8], F32, kind="Internal").ap()
                oT = nc.dram_tensor("oT0", [8, 128, S], BF16, kind="Internal").ap()
                fm_jobs = ([(j * 128, None, None) for j in range(16)] + [(2056 + j * 128, 3592 + j * 128, 0) for j in range(2)]
                           + [(2312 + j * 128, 3848 + j * 128, 1) for j in range(2)] + [(3080 + j * 128, None, None) for j in range(4)])
                proj_phase(pr, c, S, src, w["l0_w_ext"], 4104, w["l0_mix_norm"], fm_jobs, [2568], fmT, tm, w["l0_tabs"],
                           tm32_job=(2048, 8), tm32_out=tm32)
                l0_attn_phase(pr, c, S, fmT, tm, tm32, oT, w["l0_convT"], w["l0_a_log"], w["l0_dt_bias"], w["l0_gdn_norm"],
                              w["l0_rmask"], w["l0_rqsc"], w["l0_rksc"])
                dstp = out if last else xs
                outproj_phase(pr, c, S, oT, w["l0_w_out"], src, dstp)
                src = xs
            elif ph == "l1_mix":
                fmT = nc.dram_tensor("fmT1", [16, 128, S], BF16, kind="Internal").ap()
                tm = nc.dram_tensor("tm1", [S, 1024], BF16, kind="Internal").ap()
                oT = nc.dram_tensor("oT1", [8, 128, S], BF16, kind="Internal").ap()
                fm_jobs = ([(h * 128, 3072 + h * 128, 0) for h in range(4)] + [(512 + h * 128, 3584 + h * 128, 1) for h in range(4)]
                           + [(1536 + h * 128, 4096 + h * 128, 2) for h in range(4)] + [(2048 + h * 128, 4608 + h * 128, 3) for h in range(4)])
                proj_phase(pr, c, S, src, w["l1_w_ext"], 5120, w["l1_mix_norm"], fm_jobs, [1024, 2560], fmT, tm, w["l1_tabs"])
                lambda_init = 0.8 - 0.6 * math.exp(-0.3 * 1)
                l1_attn_phase(pr, c, S, fmT, tm, oT, w["l1_lam"], w["l1_diff_norm"], lambda_init)
                dstp = out if last else xs
                outproj_phase(pr, c, S, oT, w["l1_w_out"], src, dstp)
                src = xs
            else:
                raise ValueError(ph)
        pr.barrier()
        pr.op("sp", None)
        pr.emit()
    return nc


ALL_PHASES = ["l0_ffn1", "l0_mix", "l0_ffn2", "l1_ffn1", "l1_mix", "l1_ffn2"]


def run(S, phases, x_list, weights):
    consts = host_consts(S, weights)
    shapes = {k: v.shape for k, v in weights.items()}
    shapes.update({k: v.shape for k, v in consts.items()})
    nc = build_program(S, phases, shapes)
    in_maps = []
    for xb in x_list:
        m = {"x": np.ascontiguousarray(xb)}
        m.update(weights)
        m.update(consts)
        in_maps.append(m)
    res = run_bass_kernel_spmd(nc, in_maps, core_ids=list(range(len(x_list))))
    return [r["out"] for r in res.results]


def kernel(**inputs):
    x = np.asarray(inputs["x"])
    B, S, _ = x.shape
    weights = {k: np.ascontiguousarray(np.asarray(inputs[k], dtype=np.float32)) for k in W_NAMES}
    outs = run(S, ALL_PHASES, [x[b] for b in range(B)], weights)
    return np.stack(outs, axis=0).astype(np.float32)
```

```python
from contextlib import ExitStack
import math
import numpy as np
import ml_dtypes
import concourse.bass as bass
import concourse.mybir as mybir
from concourse.bass_utils import run_bass_kernel_spmd

F32 = mybir.dt.float32
BF16 = mybir.dt.bfloat16
AF = mybir.ActivationFunctionType
ALU = mybir.AluOpType
AX = mybir.AxisListType

D = 1024
DFF = 2816
EPS = 1e-6
SBUF_BYTES = 196608 - 2048
EPOCH = 16000
ENGS = ("pe", "act", "dve", "pool", "sp")


class _Rec:
    def __init__(self):
        self.call = None

    def __getattr__(self, name):
        def f(*a, **k):
            self.call = (name, a, k)
            return self
        return f


class Prog:
    def __init__(self, nc, stack):
        self.nc = nc
        self.stack = stack
        self.streams = {e: [] for e in ENGS}
        self.cnt = {e: 0 for e in ENGS}
        self.esem = {e: None for e in ENGS}
        self.ebase = {e: 0 for e in ENGS}
        self.lastw = {}
        self.readers = {}
        self.known = {e: {} for e in ENGS}
        self.dsems = {}
        self.nsem = 0
        self.pending = {e: [] for e in ENGS}
        self.sb = None
        self.sb_off = 0
        self.sb_persist = 0
        self.latest = {}

    def newsem(self, name):
        self.nsem += 1
        return self.stack.enter_context(self.nc.semaphore("s%d_%s" % (self.nsem, name)))

    def init_sbuf(self):
        self.sb = self.stack.enter_context(self.nc.sbuf_tensor("sbuf_all", [128, SBUF_BYTES // 4], F32))

    def alloc(self, cols, dtype, parts=128):
        nbytes = cols * (4 if dtype == F32 else 2)
        nbytes = (nbytes + 63) // 64 * 64
        assert self.sb_off + nbytes <= SBUF_BYTES, ("SBUF overflow", self.sb_off, nbytes)
        a = self.sb[0:parts, self.sb_off // 4:(self.sb_off + nbytes) // 4]
        self.sb_off += nbytes
        if dtype != F32:
            a = a.bitcast(dtype)
        return a[:, 0:cols]

    def mark_persistent(self):
        self.sb_persist = self.sb_off

    def reset_phase(self):
        self.sb_off = self.sb_persist

    def _event(self, eng):
        if self.esem[eng] is None or self.cnt[eng] - self.ebase[eng] >= EPOCH:
            self.esem[eng] = self.newsem(eng)
            self.ebase[eng] = self.cnt[eng]
        self.cnt[eng] += 1
        return (self.esem[eng], self.cnt[eng] - self.ebase[eng], eng)

    def op(self, eng, fn, reads=(), writes=(), dsem=None):
        deps = list(self.pending[eng])
        self.pending[eng] = []
        for k in reads:
            ev = self.lastw.get(k)
            if ev is not None:
                deps.append(ev)
        for k in writes:
            ev = self.lastw.get(k)
            if ev is not None:
                deps.append(ev)
            deps.extend(self.readers.get(k, {}).values())
        waits = {}
        kn = self.known[eng]
        for (sem, val, peng) in deps:
            if peng == eng and eng == "pe":
                continue
            if kn.get(id(sem), 0) >= val:
                continue
            if waits.get(id(sem), (None, 0))[1] < val:
                waits[id(sem)] = (sem, val)
        for sid, (sem, val) in waits.items():
            kn[sid] = val
        if fn is None:
            self.streams[eng].append((list(waits.values()), None, None))
            return None
        rec = _Rec()
        fn(rec)
        fn = rec.call
        if dsem is None:
            ev = self._event(eng)
            inc = (ev[0], 1)
        else:
            if dsem not in self.dsems:
                self.dsems[dsem] = [self.newsem("d"), 0]
            d = self.dsems[dsem]
            d[1] += 16
            ev = (d[0], d[1], "dma")
            inc = (d[0], 16)
        self.streams[eng].append((list(waits.values()), fn, inc))
        self.latest[id(ev[0])] = ev
        for k in writes:
            self.lastw[k] = ev
            self.readers[k] = {}
        for k in reads:
            if k in writes:
                continue
            self.readers.setdefault(k, {})[id(ev[0])] = ev
        return ev

    def barrier(self):
        evs = list(self.latest.values())
        for e in ENGS:
            self.pending[e] = list(evs)

    def emit(self):
        with self.nc.Block() as block:
            decos = {"pe": block.tensor, "act": block.scalar, "dve": block.vector,
                     "pool": block.gpsimd, "sp": block.sync}
            for eng in ENGS:
                stream = self.streams[eng]

                def body(e, stream=stream):
                    for waits, fn, inc in stream:
                        for sem, val in waits:
                            e.wait_ge(sem, val)
                        if fn is not None:
                            getattr(e, fn[0])(*fn[1], **fn[2]).then_inc(inc[0], inc[1])

                decos[eng](body)


def v3(ap, a):
    return ap.rearrange("p (a b) -> p a b", a=a)


def bcast_rows(vec_ap, n, parts=128):
    return vec_ap.rearrange("(o n) -> o n", o=1).broadcast_to([parts, n])


class Ctx:
    pass


def setup_common(pr, c):
    nc = pr.nc
    c.ps = [pr.stack.enter_context(nc.psum_tensor("ps%d" % i, [128, 512], F32)) for i in range(8)]
    c.ident_f = pr.alloc(128, F32)
    c.ident_b = pr.alloc(128, BF16)
    c.mhalf = pr.alloc(1, F32)
    c.ones_b = pr.alloc(128, BF16)
    c.ones_f = pr.alloc(128, F32)
    pr.op("sp", lambda e: e.dma_start(out=c.ident_f, in_=c.dram["ident"][:, :]), writes=["ident_f"], dsem="const")
    pr.op("dve", lambda e: e.tensor_copy(out=c.ident_b, in_=c.ident_f), reads=["ident_f"], writes=["ident_b"])
    pr.op("pool", lambda e: e.memset(c.mhalf, -0.5), writes=["mhalf"])
    pr.op("pool", lambda e: e.memset(c.ones_b, 1.0), writes=["ones_b"])
    pr.op("pool", lambda e: e.memset(c.ones_f, 1.0), writes=["ones_f"])
    pr.mark_persistent()


def rms_rows(pr, c, x_ap, xkey, out_ap, outkey, wn, wnkey, junk, ss, tag):
    pr.op("dve", lambda e: e.scalar_tensor_tensor(out=junk, in0=x_ap, scalar=1.0, in1=x_ap,
                                                  op0=ALU.mult, op1=ALU.mult, accum_out=ss[:, 0:1]),
          reads=[xkey], writes=["junk" + tag, "ss" + tag])
    pr.op("dve", lambda e: e.tensor_scalar(out=ss[:, 1:2], in0=ss[:, 0:1], scalar1=1.0 / x_ap.shape[-1], scalar2=EPS,
                                           op0=ALU.mult, op1=ALU.add),
          reads=["ss" + tag], writes=["ms" + tag])
    pr.op("pool", lambda e: e.tensor_tensor(out=ss[:, 2:3], in0=ss[:, 1:2], in1=c.mhalf, op=ALU.pow),
          reads=["ms" + tag, "mhalf"], writes=["rstd" + tag])
    if wn is not None:
        pr.op("dve", lambda e: e.scalar_tensor_tensor(out=out_ap, in0=x_ap, scalar=ss[:, 2:3], in1=wn,
                                                      op0=ALU.mult, op1=ALU.mult),
              reads=[xkey, "rstd" + tag, wnkey], writes=[outkey])
    else:
        pr.op("dve", lambda e: e.tensor_scalar(out=out_ap, in0=x_ap, scalar1=ss[:, 2:3], scalar2=None,
                                               op0=ALU.mult),
              reads=[xkey, "rstd" + tag], writes=[outkey])


def load_weight_bf16(pr, dst3, src2, nk, ncols, key, dsem, colchunk=1024):
    for k in range(nk):
        c0 = 0
        while c0 < ncols:
            w = min(colchunk, ncols - c0)
            pr.op("pool", lambda e, k=k, c0=c0, w=w: e.dma_start(out=dst3[:, k, c0:c0 + w],
                                                                  in_=src2[k * 128:(k + 1) * 128, c0:c0 + w]),
                  writes=[key], dsem=dsem)
            c0 += w


def load_weight_chunked(pr, dst3, src2, nk, ncols, key, dsem, cw, order=None):
    ncc = (ncols + cw - 1) // cw
    for cc in (order if order is not None else range(ncc)):
        c0 = cc * cw
        w = min(cw, ncols - c0)
        for k in range(nk):
            pr.op("pool", lambda e, k=k, c0=c0, w=w: e.dma_start(out=dst3[:, k, c0:c0 + w],
                                                                  in_=src2[k * 128:(k + 1) * 128, c0:c0 + w]),
                  writes=[(key, cc)], dsem=(dsem, cc))


def wkeys(key, c0, w, cw):
    return [(key, cc) for cc in range(c0 // cw, (c0 + w - 1) // cw + 1)]


def ffn_phase(pr, c, tag, src, dst, wup, wdn, nrm, S, fin=None, out_final=None):
    pr.barrier()
    pr.reset_phase()
    G = 256
    NG = S // G
    NF = DFF // 128
    wup_sb = v3(pr.alloc(8 * 2 * DFF, BF16), 8)
    wdn_sb = v3(pr.alloc(NF * D, BF16), NF)
    xs = [v3(pr.alloc(2 * D, F32), 2) for _ in range(2)]
    hb = [pr.alloc(D, BF16) for _ in range(2)]
    hT = [v3(pr.alloc(8 * G, BF16), 8) for _ in range(2)]
    actT = v3(pr.alloc(NF * G, BF16), NF)
    sg = [pr.alloc(G, F32) for _ in range(2)]
    junk = pr.alloc(D, BF16)
    wn = pr.alloc(D, F32)
    ss = pr.alloc(4, F32)
    if fin is not None:
        wfin = pr.alloc(D, F32)
        pr.op("sp", lambda e: e.dma_start(out=wfin, in_=bcast_rows(fin, D)), writes=["wfin"], dsem="wn2")
    pr.op("sp", lambda e: e.dma_start(out=wn, in_=bcast_rows(nrm, D)), writes=["wn"], dsem="wn")
    load_weight_chunked(pr, wup_sb, wup, 8, 2 * DFF, "wup", "wup", 704, order=[0, 4, 1, 5, 2, 6, 3, 7])
    load_weight_bf16(pr, wdn_sb, wdn, NF, D, "wdn", "wdn")
    pst = c.ps[0][:, :].bitcast(BF16)
    psg = [c.ps[1], c.ps[2]]
    psu = [c.ps[3], c.ps[4]]
    psd = [c.ps[5], c.ps[6]]

    def load(g):
        s = g % 2
        pr.op("sp", lambda e: e.dma_start(out=xs[s], in_=src[g * G:(g + 1) * G, :].rearrange("(n p) d -> p n d", p=128)),
              reads=[("xd", g)], writes=[("x", s, 0), ("x", s, 1)], dsem=("xl", s))

    def prep_dve(g):
        s = g % 2
        for t in range(2):
            rms_rows(pr, c, xs[s][:, t, :], ("x", s, t), hb[t], ("hb", t), wn, "wn", junk, ss, "f")

    def prep_pe(g):
        s = g % 2
        for t in range(2):
            for k in range(8):
                pr.op("pe", lambda e, t=t, k=k: e.transpose(out=pst[:, k * 128:(k + 1) * 128],
                                                            in_=hb[t][:, k * 128:(k + 1) * 128], identity=c.ident_b),
                      reads=[("hb", t), "ident_b"], writes=["pst"])
            pr.op("act", lambda e, t=t, s=s: e.activation(out=hT[s][:, :, t * 128:(t + 1) * 128], in_=v3(pst, 8), func=AF.Copy),
                  reads=["pst"], writes=[("hT", s)])

    def up(g):
        s = g % 2
        for f in range(NF):
            b = f % 2
            for k in range(8):
                pr.op("pe", lambda e, f=f, k=k, b=b: e.matmul(psg[b][:, 0:G], lhsT=wup_sb[:, k, f * 128:(f + 1) * 128],
                                                              rhs=hT[s][:, k, :], start=(k == 0), stop=(k == 7)),
                      reads=wkeys("wup", f * 128, 128, 704) + [("hT", s)], writes=[("psg", b)])
            for k in range(8):
                pr.op("pe", lambda e, f=f, k=k, b=b: e.matmul(psu[b][:, 0:G], lhsT=wup_sb[:, k, DFF + f * 128:DFF + (f + 1) * 128],
                                                              rhs=hT[s][:, k, :], start=(k == 0), stop=(k == 7)),
                      reads=wkeys("wup", DFF + f * 128, 128, 704) + [("hT", s)], writes=[("psu", b)])
            pr.op("act", lambda e, b=b: e.activation(out=sg[b], in_=psg[b][:, 0:G], func=AF.Silu),
                  reads=[("psg", b)], writes=[("sg", b)])
            pr.op("dve", lambda e, f=f, b=b: e.tensor_tensor(out=actT[:, f, :], in0=sg[b], in1=psu[b][:, 0:G], op=ALU.mult),
                  reads=[("sg", b), ("psu", b)], writes=["actT"])

    def down(g):
        s = g % 2
        for t in range(2):
            for hf in range(2):
                b = hf
                for f in range(NF):
                    pr.op("pe", lambda e, f=f, t=t, hf=hf, b=b: e.matmul(psd[b][:, :], lhsT=actT[:, f, t * 128:(t + 1) * 128],
                                                                          rhs=wdn_sb[:, f, hf * 512:(hf + 1) * 512],
                                                                          start=(f == 0), stop=(f == NF - 1)),
                          reads=["actT", "wdn"], writes=[("psd", b)])
                pr.op("dve", lambda e, t=t, hf=hf, b=b, s=s: e.scalar_tensor_tensor(
                    out=xs[s][:, t, hf * 512:(hf + 1) * 512], in0=psd[b][:, :], scalar=0.5,
                    in1=xs[s][:, t, hf * 512:(hf + 1) * 512], op0=ALU.mult, op1=ALU.add),
                      reads=[("psd", b), ("x", s, t)], writes=[("x", s, t)])
            if fin is not None:
                rms_rows(pr, c, xs[s][:, t, :], ("x", s, t), xs[s][:, t, :], ("x", s, t), wfin, "wfin", junk, ss, "f")
        tgt = dst if fin is None else out_final
        pr.op("sp", lambda e: e.dma_start(out=tgt[g * G:(g + 1) * G, :].rearrange("(n p) d -> p n d", p=128), in_=xs[s]),
              reads=[("x", s, 0), ("x", s, 1)], writes=[("xd", g)], dsem=("xst", s))

    load(0)
    prep_dve(0)
    prep_pe(0)
    for g in range(NG):
        if g + 1 < NG:
            load(g + 1)
            prep_dve(g + 1)
        up(g)
        if g + 1 < NG:
            prep_pe(g + 1)
        down(g)


def proj_phase(pr, c, S, src, w_dram, ncols, nrm, fm_jobs, tm_jobs, fmT, tm_out, tabs, tm32_job=None, tm32_out=None):
    pr.barrier()
    pr.reset_phase()
    G = 256
    NG = S // G
    nch = len(fm_jobs)
    ntab = 0 if tabs is None else tabs.shape[0]
    w_sb = v3(pr.alloc(8 * ncols, BF16), 8)
    xs = [v3(pr.alloc(2 * D, F32), 2) for _ in range(2)]
    hb = [pr.alloc(D, BF16) for _ in range(2)]
    hT = [v3(pr.alloc(8 * G, BF16), 8) for _ in range(2)]
    junk = pr.alloc(D, BF16)
    wn = pr.alloc(D, F32)
    ss = pr.alloc(4, F32)
    stage = [v3(pr.alloc(nch * G, BF16), nch) for _ in range(2)]
    ntm = len(tm_jobs)
    tstage = [v3(pr.alloc(2 * max(ntm, 1) * 512, BF16), 2) for _ in range(2)]
    t32 = [v3(pr.alloc(2 * 8, F32), 2) for _ in range(2)]
    tab = [v3(pr.alloc(max(ntab, 1) * 2 * G, F32), max(ntab, 1) * 2) for _ in range(2)]
    t1 = [pr.alloc(G, F32) for _ in range(2)]
    t2 = [pr.alloc(G, F32) for _ in range(2)]
    pr.op("sp", lambda e: e.dma_start(out=wn, in_=bcast_rows(nrm, D)), writes=["wn"], dsem="wn")
    load_weight_chunked(pr, w_sb, w_dram, 8, ncols, "w_in", "w_in", 512)
    pst = c.ps[0][:, :].bitcast(BF16)
    pA = [c.ps[1], c.ps[2]]
    pB = [c.ps[3], c.ps[4]]
    pT = [c.ps[5], c.ps[6]]

    def load(g):
        s = g % 2
        pr.op("sp", lambda e: e.dma_start(out=xs[s], in_=src[g * G:(g + 1) * G, :].rearrange("(n p) d -> p n d", p=128)),
              reads=[("xd", g)], writes=[("x", s, 0), ("x", s, 1)], dsem=("xl", s))
        if ntab:
            pr.op("sp", lambda e: e.dma_start(out=tab[s], in_=tabs.rearrange("t two p s -> p (t two) s")[:, :, g * G:(g + 1) * G]),
                  writes=[("tab", s)], dsem=("tabl", s))

    def prep(g):
        s = g % 2
        for t in range(2):
            rms_rows(pr, c, xs[s][:, t, :], ("x", s, t), hb[t], ("hb", t), wn, "wn", junk, ss, "f")
        for t in range(2):
            for k in range(8):
                pr.op("pe", lambda e, t=t, k=k: e.transpose(out=pst[:, k * 128:(k + 1) * 128],
                                                            in_=hb[t][:, k * 128:(k + 1) * 128], identity=c.ident_b),
                      reads=[("hb", t), "ident_b"], writes=["pst"])
            pr.op("act", lambda e, t=t, s=s: e.activation(out=hT[s][:, :, t * 128:(t + 1) * 128], in_=v3(pst, 8), func=AF.Copy),
                  reads=["pst"], writes=[("hT", s)])

    def mm_fm(ps, col0, s, key):
        for k in range(8):
            pr.op("pe", lambda e, k=k: e.matmul(ps[:, 0:G], lhsT=w_sb[:, k, col0:col0 + 128], rhs=hT[s][:, k, :],
                                                start=(k == 0), stop=(k == 7)),
                  reads=wkeys("w_in", col0, 128, 512) + [("hT", s)], writes=[key])

    def compute(g):
        s = g % 2
        for j, (col0, pcol0, ti) in enumerate(fm_jobs):
            b = j % 2
            mm_fm(pA[b], col0, s, ("pA", b))
            if pcol0 is None:
                pr.op("act", lambda e, j=j, b=b: e.activation(out=stage[s][:, j, :], in_=pA[b][:, 0:G], func=AF.Copy),
                      reads=[("pA", b)], writes=[("stage", s)])
            else:
                mm_fm(pB[b], pcol0, s, ("pB", b))
                pr.op("dve", lambda e, b=b, ti=ti: e.tensor_tensor(out=t1[b], in0=pA[b][:, 0:G], in1=tab[s][:, 2 * ti, :], op=ALU.mult),
                      reads=[("pA", b), ("tab", s)], writes=[("t1", b)])
                pr.op("dve", lambda e, b=b, ti=ti: e.tensor_tensor(out=t2[b], in0=pB[b][:, 0:G], in1=tab[s][:, 2 * ti + 1, :], op=ALU.mult),
                      reads=[("pB", b), ("tab", s)], writes=[("t2", b)])
                pr.op("pool", lambda e, b=b, j=j: e.tensor_tensor(out=stage[s][:, j, :], in0=t1[b], in1=t2[b], op=ALU.add),
                      reads=[("t1", b), ("t2", b)], writes=[("stage", s)])
        pr.op("sp", lambda e: e.dma_start(out=fmT.rearrange("c p s -> p c s")[:, :, g * G:(g + 1) * G], in_=stage[s]),
              reads=[("stage", s)], writes=[("fmT", g)], dsem=("fst", s))
        for t in range(2):
            for i, col0 in enumerate(tm_jobs):
                b = i % 2
                for k in range(8):
                    pr.op("pe", lambda e, k=k, t=t, col0=col0, b=b: e.matmul(pT[b][:, :], lhsT=hT[s][:, k, t * 128:(t + 1) * 128],
                                                                              rhs=w_sb[:, k, col0:col0 + 512],
                                                                              start=(k == 0), stop=(k == 7)),
                          reads=wkeys("w_in", col0, 512, 512) + [("hT", s)], writes=[("pT", b)])
                pr.op("act", lambda e, t=t, i=i, b=b: e.activation(out=tstage[s][:, t, i * 512:(i + 1) * 512], in_=pT[b][:, :], func=AF.Copy),
                      reads=[("pT", b)], writes=[("tstage", s)])
            if tm32_job is not None:
                col0, n = tm32_job
                for k in range(8):
                    pr.op("pe", lambda e, k=k, t=t: e.matmul(pT[0][:, 0:n], lhsT=hT[s][:, k, t * 128:(t + 1) * 128],
                                                              rhs=w_sb[:, k, col0:col0 + n], start=(k == 0), stop=(k == 7)),
                          reads=wkeys("w_in", col0, n, 512) + [("hT", s)], writes=[("pT", 0)])
                pr.op("act", lambda e, t=t: e.activation(out=t32[s][:, t, 0:n], in_=pT[0][:, 0:n], func=AF.Copy),
                      reads=[("pT", 0)], writes=[("t32", s)])
        if ntm:
            pr.op("sp", lambda e: e.dma_start(out=tm_out[g * G:(g + 1) * G, :].rearrange("(n p) d -> p n d", p=128), in_=tstage[s]),
                  reads=[("tstage", s)], writes=[("tm", g)], dsem=("tst", s))
        if tm32_job is not None:
            n = tm32_job[1]
            pr.op("sp", lambda e: e.dma_start(out=tm32_out[g * G:(g + 1) * G, :].rearrange("(n p) d -> p n d", p=128),
                                              in_=t32[s][:, :, 0:n]),
                  reads=[("t32", s)], writes=[("tm32", g)], dsem=("t32st", s))

    load(0)
    prep(0)
    for g in range(NG):
        if g + 1 < NG:
            load(g + 1)
        compute(g)
        if g + 1 < NG:
            prep(g + 1)


def outproj_phase(pr, c, S, oT, w_out, src, dst):
    pr.barrier()
    pr.reset_phase()
    G = 256
    NG = S // G
    w_sb = v3(pr.alloc(8 * D, BF16), 8)
    load_weight_bf16(pr, w_sb, w_out, 8, D, "w_out", "w_out")
    xs = [v3(pr.alloc(2 * D, F32), 2) for _ in range(2)]
    ot = [v3(pr.alloc(8 * G, BF16), 8) for _ in range(2)]
    pO = [c.ps[1], c.ps[2]]

    def load(g):
        s = g % 2
        pr.op("sp", lambda e: e.dma_start(out=xs[s], in_=src[g * G:(g + 1) * G, :].rearrange("(n p) d -> p n d", p=128)),
              reads=[("xd", g)], writes=[("x", s)], dsem=("xl", s))
        pr.op("sp", lambda e: e.dma_start(out=ot[s], in_=oT.rearrange("c p s -> p c s")[:, :, g * G:(g + 1) * G]),
              reads=["oT"], writes=[("ot", s)], dsem=("otl", s))

    load(0)
    for g in range(NG):
        s = g % 2
        if g + 1 < NG:
            load(g + 1)
        for t in range(2):
            for hf in range(2):
                for k in range(8):
                    pr.op("pe", lambda e, k=k, t=t, hf=hf: e.matmul(pO[hf][:, :], lhsT=ot[s][:, k, t * 128:(t + 1) * 128],
                                                                    rhs=w_sb[:, k, hf * 512:(hf + 1) * 512],
                                                                    start=(k == 0), stop=(k == 7)),
                          reads=["w_out", ("ot", s)], writes=[("pO", hf)])
                pr.op("dve", lambda e, t=t, hf=hf: e.tensor_tensor(out=xs[s][:, t, hf * 512:(hf + 1) * 512], in0=pO[hf][:, :],
                                                                   in1=xs[s][:, t, hf * 512:(hf + 1) * 512], op=ALU.add),
                      reads=[("pO", hf), ("x", s)], writes=[("x", s)])
        pr.op("sp", lambda e: e.dma_start(out=dst[g * G:(g + 1) * G, :].rearrange("(n p) d -> p n d", p=128), in_=xs[s]),
              reads=[("x", s)], writes=[("xd", g)], dsem=("xst", s))


def pipeline(fronts, backs, look):
    n = len(fronts)
    for i in range(n + look):
        if i < n:
            fronts[i]()
        if i >= look:
            backs[i - look]()


def col_maxnorm(pr, c, T, key, rows, S, sq, psb, out_max, tagk, psbkey="psb"):
    r0, r1 = rows
    first = True
    for g in range(S // 512):
        pr.op("pool", lambda e, g=g: e.tensor_tensor(out=sq[r0:r1, :], in0=T[r0:r1, g * 512:(g + 1) * 512],
                                                     in1=T[r0:r1, g * 512:(g + 1) * 512], op=ALU.mult),
              reads=[key], writes=["sq"])
        pr.op("pe", lambda e: e.matmul(psb[:, :], lhsT=c.ones_b[r0:r1, :], rhs=sq[r0:r1, :], start=True, stop=True),
              reads=["sq", "ones_b"], writes=[psbkey])
        if first:
            pr.op("dve", lambda e: e.tensor_reduce(out=out_max, in_=psb[:, :], axis=AX.X, op=ALU.max),
                  reads=[psbkey], writes=[tagk])
            first = False
        else:
            pr.op("dve", lambda e: e.tensor_reduce(out=c.tmpmax, in_=psb[:, :], axis=AX.X, op=ALU.max),
                  reads=[psbkey], writes=["tmpmax"])
            pr.op("dve", lambda e: e.tensor_tensor(out=out_max, in0=out_max, in1=c.tmpmax, op=ALU.max),
                  reads=["tmpmax", tagk], writes=[tagk])


def neg_bound(pr, c, mq, mk, out_negM, keys, outkey):
    pr.op("dve", lambda e: e.tensor_tensor(out=c.tmpmax, in0=mq, in1=mk, op=ALU.mult), reads=keys, writes=["tmpmax"])
    pr.op("pool", lambda e: e.tensor_tensor(out=c.tmpmax, in0=c.tmpmax, in1=c.phalf, op=ALU.pow), reads=["tmpmax", "phalf"], writes=["tmpmax"])
    pr.op("dve", lambda e: e.tensor_scalar(out=out_negM, in0=c.tmpmax, scalar1=-1.0, scalar2=None, op0=ALU.mult),
          reads=["tmpmax"], writes=[outkey])


def fm_epilogue(pr, c, o_ap, okey, N, gate_ap, gatekey, wcol, wkey, sqb, psb, rs, out_ap, outkey, const_scale=1.0, psbkey="psb"):
    pr.op("pool", lambda e: e.tensor_tensor(out=sqb[:, 0:N], in0=o_ap, in1=o_ap, op=ALU.mult), reads=[okey], writes=["sqb"])
    pr.op("pe", lambda e: e.matmul(psb[:, 0:N], lhsT=c.ones_b, rhs=sqb[:, 0:N], start=True, stop=True),
          reads=["sqb", "ones_b"], writes=[psbkey])
    pr.op("dve", lambda e: e.tensor_scalar(out=rs[:, 0:N], in0=psb[:, 0:N], scalar1=1.0 / 128.0, scalar2=EPS, op0=ALU.mult, op1=ALU.add),
          reads=[psbkey], writes=["rs"])
    pr.op("act", lambda e: e.activation(out=rs[:, 0:N], in_=rs[:, 0:N], func=AF.Ln), reads=["rs"], writes=["rs"])
    pr.op("act", lambda e: e.activation(out=rs[:, 0:N], in_=rs[:, 0:N], func=AF.Exp, scale=-0.5), reads=["rs"], writes=["rs"])
    if gate_ap is None:
        if wcol is None:
            pr.op("dve", lambda e: e.tensor_tensor(out=out_ap, in0=o_ap, in1=rs[:, 0:N], op=ALU.mult),
                  reads=[okey, "rs"], writes=[outkey])
        else:
            pr.op("dve", lambda e: e.scalar_tensor_tensor(out=out_ap, in0=o_ap, scalar=wcol, in1=rs[:, 0:N], op0=ALU.mult, op1=ALU.mult),
                  reads=[okey, "rs", wkey], writes=[outkey])
    else:
        if wcol is None:
            pr.op("dve", lambda e: e.tensor_tensor(out=rs[:, 0:N], in0=o_ap, in1=rs[:, 0:N], op=ALU.mult),
                  reads=[okey, "rs"], writes=["rs"])
        else:
            pr.op("dve", lambda e: e.scalar_tensor_tensor(out=rs[:, 0:N], in0=o_ap, scalar=wcol, in1=rs[:, 0:N], op0=ALU.mult, op1=ALU.mult),
                  reads=[okey, "rs", wkey], writes=["rs"])
        pr.op("dve", lambda e: e.tensor_tensor(out=out_ap, in0=rs[:, 0:N], in1=gate_ap, op=ALU.mult),
              reads=["rs", gatekey], writes=[outkey])


def l1_attn_phase(pr, c, S, fmT, tm, oT, lam_dram, dnorm_dram, lambda_init):
    pr.barrier()
    pr.reset_phase()
    NB = S // 128
    QT = pr.alloc(S, BF16)
    KT = pr.alloc(S, BF16)
    Vp = v3(pr.alloc(NB * 128, BF16), NB)
    Vp3 = [Vp, v3(pr.alloc(NB * 128, BF16), NB), v3(pr.alloc(NB * 128, BF16), NB)]
    num = pr.alloc(S, F32)
    den = pr.alloc(S, F32)
    pTs = [pr.alloc(512, BF16) for _ in range(4)]
    sq = pr.alloc(512, BF16)
    sqb = pr.alloc(512, BF16)
    rs = pr.alloc(512, F32)
    o1s = [pr.alloc(512, F32) for _ in range(2)]
    o2 = pr.alloc(512, F32)
    rs2 = pr.alloc(512, F32)
    ost = [pr.alloc(512, BF16) for _ in range(2)]
    mask2 = pr.alloc(256, BF16)
    maskd = v3(pr.alloc(4 * 512, BF16), 4)
    small = pr.alloc(16, F32)
    c.tmpmax = small[:, 0:1]
    c.phalf = small[:, 1:2]
    mq = small[:, 2:3]
    mk = small[:, 3:4]
    negM = small[:, 4:5]
    lamneg = small[:, 5:6]
    dnorm = small[:, 6:7]
    lamt = pr.alloc(256, F32)
    pr.op("pool", lambda e: e.memset(c.phalf, 0.5), writes=["phalf"])
    pr.op("pool", lambda e: e.memset(mask2, 1.0), writes=["mask2"])
    pr.op("pool", lambda e: e.affine_select(out=mask2[:, 0:128], in_=mask2[:, 0:128], pattern=[[-1, 128]], compare_op=ALU.is_ge,
                                            fill=0.0, base=0, channel_multiplier=1), reads=["mask2"], writes=["mask2"])
    pr.op("pool", lambda e: e.affine_select(out=mask2[:, 128:256], in_=mask2[:, 128:256], pattern=[[1, 128]], compare_op=ALU.is_ge,
                                            fill=0.0, base=0, channel_multiplier=-1), reads=["mask2"], writes=["mask2"])
    pr.op("pool", lambda e: e.memset(maskd, 1.0), writes=["maskd"])
    for dd in range(4):
        pr.op("pool", lambda e, dd=dd: e.affine_select(out=maskd[:, dd, :], in_=maskd[:, dd, :], pattern=[[1, 512]], compare_op=ALU.is_ge,
                                                       fill=0.0, base=-128 * dd, channel_multiplier=-1), reads=["maskd"], writes=["maskd"])
    pr.op("sp", lambda e: e.dma_start(out=lamt, in_=bcast_rows(lam_dram, 256)), writes=["lamt"], dsem="lamt")
    pr.op("sp", lambda e: e.dma_start(out=dnorm, in_=dnorm_dram.rearrange("(p o) -> p o", o=1)), writes=["dnorm"], dsem="dnorm")
    pr.op("dve", lambda e: e.tensor_tensor(out=lamt[:, 0:64], in0=lamt[:, 0:64], in1=lamt[:, 64:128], op=ALU.mult), reads=["lamt"], writes=["lamt"])
    pr.op("dve", lambda e: e.tensor_tensor(out=lamt[:, 128:192], in0=lamt[:, 128:192], in1=lamt[:, 192:256], op=ALU.mult), reads=["lamt"], writes=["lamt"])
    pr.op("dve", lambda e: e.tensor_reduce(out=small[:, 8:9], in_=lamt[:, 0:64], axis=AX.X, op=ALU.add), reads=["lamt"], writes=["lam_a"])
    pr.op("dve", lambda e: e.tensor_reduce(out=small[:, 9:10], in_=lamt[:, 128:192], axis=AX.X, op=ALU.add), reads=["lamt"], writes=["lam_b"])
    pr.op("act", lambda e: e.activation(out=small[:, 8:10], in_=small[:, 8:10], func=AF.Exp), reads=["lam_a", "lam_b"], writes=["lam_a", "lam_b"])
    pr.op("dve", lambda e: e.tensor_tensor(out=small[:, 10:11], in0=small[:, 9:10], in1=small[:, 8:9], op=ALU.subtract), reads=["lam_a", "lam_b"], writes=["lam_c"])
    pr.op("dve", lambda e: e.tensor_scalar(out=lamneg, in0=small[:, 10:11], scalar1=-float(lambda_init), scalar2=None, op0=ALU.add),
          reads=["lam_c"], writes=["lamneg"])
    pr.op("dve", lambda e: e.tensor_scalar(out=dnorm, in0=dnorm, scalar1=float(1.0 - lambda_init), scalar2=None, op0=ALU.mult),
          reads=["dnorm"], writes=["dnorm"])
    psc = [c.ps[0], c.ps[1], c.ps[2], c.ps[3]]
    pO = [c.ps[4], c.ps[5]]
    pL = [c.ps[6], c.ps[7]]
    psb = c.ps[3]
    PSB = ("sc", 3)
    oTv = oT.rearrange("c p s -> p c s")
    fmv = fmT.rearrange("c p s -> p c s")

    for h in range(4):
        pr.op("sp", lambda e, h=h: e.dma_start(out=QT, in_=fmv[:, h, :]), reads=["fmT"], writes=["QT"], dsem="ql")
        pr.op("sp", lambda e, h=h: e.dma_start(out=KT, in_=fmv[:, 4 + h, :]), reads=["fmT"], writes=["KT"], dsem="kl")
        col_maxnorm(pr, c, QT, "QT", (0, 128), S, sq, psb, mq, "mq", psbkey=PSB)
        col_maxnorm(pr, c, KT, "KT", (0, 128), S, sq, psb, mk, "mk", psbkey=PSB)
        neg_bound(pr, c, mq, mk, negM, ["mq", "mk"], "negM")
        fronts, backs = [], []
        cnt = 0
        gcnt = 0
        for bi, d in enumerate((1, 4, 16)):
            L = S // d
            nb = L // 128
            for r in range(d):
                for n0 in range(0, nb, 16):
                    n1 = min(nb, n0 + 16)
                    pr.op("sp", lambda e, r=r, d=d, nb=nb, h=h, n0=n0, n1=n1: e.dma_start(
                        out=Vp3[bi][:, r * nb + n0:r * nb + n1, :],
                        in_=tm.rearrange("(n m r) c -> r m n c", r=d, m=128)[r, :, n0:n1, h * 128:(h + 1) * 128]),
                          reads=["tm"], writes=[("Vp", bi)], dsem=("vl", bi))
            Qv = QT.rearrange("p (m r) -> p r m", r=d)
            Kv = KT.rearrange("p (m r) -> p r m", r=d)
            numv = num.rearrange("p (m r) -> p r m", r=d)
            denv = den.rearrange("p (m r) -> p r m", r=d)
            for r in range(d):
                for n0 in range(0, nb, 4):
                    nn = min(4, nb - n0)
                    gp = gcnt % 2
                    gcnt += 1
                    for i in range(nn):
                        n = n0 + i
                        sb_ = cnt % 3
                        cnt += 1

                        def front(n=n, r=r, sb_=sb_, Kv=Kv, Qv=Qv):
                            sc = psc[sb_]
                            pT = pTs[sb_]
                            lo = 0 if n > 0 else 128
                            if n > 0:
                                pr.op("pe", lambda e: e.matmul(sc[:, 0:128], lhsT=Kv[:, r, (n - 1) * 128:n * 128],
                                                               rhs=Qv[:, r, n * 128:(n + 1) * 128], start=True, stop=True),
                                      reads=["QT", "KT"], writes=[("sc", sb_)])
                            pr.op("pe", lambda e: e.matmul(sc[:, 128:256], lhsT=Kv[:, r, n * 128:(n + 1) * 128],
                                                           rhs=Qv[:, r, n * 128:(n + 1) * 128], start=True, stop=True),
                                  reads=["QT", "KT"], writes=[("sc", sb_)])
                            pr.op("act", lambda e: e.activation(out=pT[:, lo:256], in_=sc[:, lo:256], func=AF.Exp, bias=negM),
                                  reads=[("sc", sb_), "negM"], writes=[("pT", sb_)])
                            pr.op("dve", lambda e: e.tensor_tensor(out=pT[:, lo:256], in0=pT[:, lo:256], in1=mask2[:, lo:256], op=ALU.mult),
                                  reads=[("pT", sb_), "mask2"], writes=[("pT", sb_)])

                        def back(n=n, r=r, i=i, sb_=sb_, nb=nb, bi=bi, gp=gp, nn=nn, n0=n0, numv=numv, denv=denv):
                            pT = pTs[sb_]
                            Vb = Vp3[bi]
                            cs = slice(i * 128, (i + 1) * 128)
                            kO, kL = ("pO", gp), ("pL", gp)
                            if n > 0:
                                pr.op("pe", lambda e: e.matmul(pO[gp][:, cs], lhsT=Vb[:, r * nb + n - 1, :], rhs=pT[:, 0:128], start=True, stop=False),
                                      reads=[("Vp", bi), ("pT", sb_)], writes=[kO])
                            pr.op("pe", lambda e: e.matmul(pO[gp][:, cs], lhsT=Vb[:, r * nb + n, :], rhs=pT[:, 128:256], start=(n == 0), stop=True),
                                  reads=[("Vp", bi), ("pT", sb_)], writes=[kO])
                            if n > 0:
                                pr.op("pe", lambda e: e.matmul(pL[gp][:, cs], lhsT=c.ones_b, rhs=pT[:, 0:128], start=True, stop=False),
                                      reads=["ones_b", ("pT", sb_)], writes=[kL])
                            pr.op("pe", lambda e: e.matmul(pL[gp][:, cs], lhsT=c.ones_b, rhs=pT[:, 128:256], start=(n == 0), stop=True),
                                  reads=["ones_b", ("pT", sb_)], writes=[kL])
                            if i == nn - 1:
                                W = nn * 128
                                dstn = numv[:, r, n0 * 128:n0 * 128 + W]
                                dstd = denv[:, r, n0 * 128:n0 * 128 + W]
                                if bi == 0:
                                    pr.op("act", lambda e: e.activation(out=dstn, in_=pO[gp][:, 0:W], func=AF.Copy), reads=[kO], writes=["num"])
                                    pr.op("dve", lambda e: e.tensor_copy(out=dstd, in_=pL[gp][:, 0:W]), reads=[kL], writes=["den"])
                                else:
                                    pr.op("dve", lambda e: e.tensor_tensor(out=dstn, in0=pO[gp][:, 0:W], in1=dstn, op=ALU.add), reads=[kO, "num"], writes=["num"])
                                    pr.op("dve", lambda e: e.tensor_tensor(out=dstd, in0=pL[gp][:, 0:W], in1=dstd, op=ALU.add), reads=[kL, "den"], writes=["den"])

                        fronts.append(front)
                        backs.append(back)
        pipeline(fronts, backs, 2)
        for g in range(S // 512):
            s = g % 2
            cs = slice(g * 512, (g + 1) * 512)
            pr.op("dve", lambda e, cs=cs: e.reciprocal(out=den[:, cs], in_=den[:, cs]), reads=["den"], writes=["den"])
            pr.op("dve", lambda e, cs=cs, s=s: e.tensor_tensor(out=ost[s], in0=num[:, cs], in1=den[:, cs], op=ALU.mult),
                  reads=["num", "den"], writes=[("ost", s)])
            pr.op("sp", lambda e, cs=cs, s=s, h=h: e.dma_start(out=oTv[:, h, cs], in_=ost[s]), reads=[("ost", s)], writes=["oT"], dsem=("ostd", s))

    for h in range(4):
        pr.op("sp", lambda e, h=h: e.dma_start(out=QT, in_=fmv[:, 8 + h, :]), reads=["fmT"], writes=["QT"], dsem="ql")
        pr.op("sp", lambda e, h=h: e.dma_start(out=KT, in_=fmv[:, 12 + h, :]), reads=["fmT"], writes=["KT"], dsem="kl")
        for n0 in range(0, NB, 16):
            n1 = min(NB, n0 + 16)
            pr.op("sp", lambda e, h=h, n0=n0, n1=n1: e.dma_start(
                out=Vp[:, n0:n1, :], in_=tm.rearrange("(n p) c -> p n c", p=128)[:, n0:n1, 512 + h * 128:512 + (h + 1) * 128]),
                  reads=["tm"], writes=[("Vp", 0)], dsem="vl")
        negMs = [small[:, 11:12], small[:, 12:13]]
        for cp in range(2):
            col_maxnorm(pr, c, QT, "QT", (64 * cp, 64 * cp + 64), S, sq, psb, mq, "mq", psbkey=PSB)
            col_maxnorm(pr, c, KT, "KT", (64 * cp, 64 * cp + 64), S, sq, psb, mk, "mk", psbkey=PSB)
            neg_bound(pr, c, mq, mk, negMs[cp], ["mq", "mk"], ("negMd", cp))
        fronts, backs = [], []
        ucnt = 0
        for qg in range(S // 512):
            qs = slice(qg * 512, (qg + 1) * 512)
            nkb = 4 * (qg + 1)
            for kb in range(nkb):
                slot = ucnt % 2
                ucnt += 1

                def front(kb=kb, slot=slot, qs=qs, dd=kb - 4 * qg):
                    sbs = [2 * slot, 2 * slot + 1]
                    for cp in range(2):
                        rows = slice(64 * cp, 64 * cp + 64)
                        sc = psc[sbs[cp]]
                        wr = [("sc", sbs[0]), ("sc", sbs[1])] if cp == 0 else [("sc", sbs[1])]
                        pr.op("pe", lambda e: e.matmul(sc[:, :], lhsT=KT[rows, kb * 128:(kb + 1) * 128], rhs=QT[rows, qs], start=True, stop=True),
                              reads=["QT", "KT"], writes=wr)
                    for cp in range(2):
                        sc = psc[sbs[cp]]
                        pT = pTs[sbs[cp]]
                        pr.op("act", lambda e: e.activation(out=pT, in_=sc[:, :], func=AF.Exp, bias=negMs[cp]),
                              reads=[("sc", sbs[cp]), ("negMd", cp)], writes=[("pT", sbs[cp])])
                        if dd >= 0:
                            pr.op("dve", lambda e: e.tensor_tensor(out=pT, in0=pT, in1=maskd[:, dd, :], op=ALU.mult),
                                  reads=[("pT", sbs[cp]), "maskd"], writes=[("pT", sbs[cp])])

                def back(kb=kb, slot=slot, nkb=nkb, qg=qg, qs=qs, h=h):
                    sbs = [2 * slot, 2 * slot + 1]
                    for cp in range(2):
                        pT = pTs[sbs[cp]]
                        rd = [("pT", sbs[0]), ("pT", sbs[1])] if cp == 0 else [("pT", sbs[1])]
                        pr.op("pe", lambda e: e.matmul(pO[cp][:, :], lhsT=Vp[:, kb, :], rhs=pT, start=(kb == 0), stop=(kb == nkb - 1)),
                              reads=[("Vp", 0)] + rd, writes=[("pO", cp)])
                        pr.op("pe", lambda e: e.matmul(pL[cp][:, :], lhsT=c.ones_b, rhs=pT, start=(kb == 0), stop=(kb == nkb - 1)),
                              reads=["ones_b", ("pT", sbs[cp])], writes=[("pL", cp)])
                    if kb != nkb - 1:
                        return
                    oa = o1s[qg % 2]
                    ka = ("o1", qg % 2)
                    pr.op("dve", lambda e: e.reciprocal(out=rs2, in_=pL[0][:, :]), reads=[("pL", 0)], writes=["rs2"])
                    pr.op("dve", lambda e: e.tensor_tensor(out=oa, in0=pO[0][:, :], in1=rs2, op=ALU.mult), reads=[("pO", 0), "rs2"], writes=[ka])
                    pr.op("dve", lambda e: e.reciprocal(out=rs2, in_=pL[1][:, :]), reads=[("pL", 1)], writes=["rs2"])
                    pr.op("dve", lambda e: e.tensor_tensor(out=o2, in0=pO[1][:, :], in1=rs2, op=ALU.mult), reads=[("pO", 1), "rs2"], writes=["o2"])
                    pr.op("dve", lambda e: e.scalar_tensor_tensor(out=oa, in0=o2, scalar=lamneg, in1=oa, op0=ALU.mult, op1=ALU.add),
                          reads=[ka, "o2", "lamneg"], writes=[ka])
                    s = qg % 2
                    fm_epilogue(pr, c, oa, ka, 512, None, None, dnorm, "dnorm", sqb, psb, rs, ost[s], ("ost", s), psbkey=PSB)
                    pr.op("sp", lambda e: e.dma_start(out=oTv[:, 4 + h, qs], in_=ost[s]), reads=[("ost", s)], writes=["oT"], dsem=("ostd", s))

                fronts.append(front)
                backs.append(back)
        pipeline(fronts, backs, 1)


def l0_attn_phase(pr, c, S, fmT, tm, tm32, oT, convT, alog_d, dtb_d, gnorm_d, rmask_d, rqsc_d, rksc_d):
    pr.barrier()
    pr.reset_phase()
    NB = S // 128
    NG4 = NB // 4
    raw = pr.alloc(S, BF16)
    acc = pr.alloc(S, F32)
    qh = pr.alloc(S, BF16)
    kh = pr.alloc(S, BF16)
    vtok = v3(pr.alloc(S, BF16), NB)
    kdec = v3(pr.alloc(S, BF16), NB)
    zt = pr.alloc(S, BF16)
    ba = v3(pr.alloc(NB * 8, F32), NB)
    gcol = pr.alloc(NB, F32)
    negb = pr.alloc(NB, F32)
    egc = pr.alloc(NB, F32)
    edec = pr.alloc(NB, F32)
    egl = pr.alloc(NB, F32)
    tmpn = pr.alloc(NB, F32)
    cw = pr.alloc(4, F32)
    small = pr.alloc(16, F32)
    alog = small[:, 0:4]
    dtb = small[:, 4:8]
    gnorm = small[:, 8:9]
    rksc = small[:, 12:16]
    U4i = pr.alloc(512, F32)
    U4s = pr.alloc(512, F32)
    I4 = pr.alloc(512, F32)
    Lst = pr.alloc(128, F32)
    Gt = pr.alloc(512, F32)
    Dx = pr.alloc(512, F32)
    Dm = pr.alloc(512, F32)
    DmS = pr.alloc(512, F32)
    EGs = pr.alloc(512, F32)
    XY = [[pr.alloc(512, F32), pr.alloc(512, F32)] for _ in range(2)]
    Pm = pr.alloc(512, F32)
    PTb = pr.alloc(512, BF16)
    MTb = pr.alloc(512, BF16)
    qtl = pr.alloc(512, BF16)
    PTb2 = [PTb, pr.alloc(512, BF16)]
    MTb2 = [MTb, pr.alloc(512, BF16)]
    qtl2 = [qtl, pr.alloc(512, BF16)]
    Rm = pr.alloc(128, BF16)
    vnew = pr.alloc(128, BF16)
    sq = pr.alloc(512, BF16)
    sqb = pr.alloc(512, BF16)
    rs = pr.alloc(512, F32)
    S_f = pr.alloc(128, F32)
    S_b = pr.alloc(128, BF16)
    o_sb = pr.alloc(512, F32)
    gate = pr.alloc(512, F32)
    ost = [pr.alloc(512, BF16) for _ in range(2)]
    rmask = pr.alloc(512, F32)
    rqsc = pr.alloc(512, F32)
    ps = c.ps
    oTv = oT.rearrange("c p s -> p c s")
    fmv = fmT.rearrange("c p s -> p c s")
    K0, K1, K2, K3, K4, K5, K6 = "ps0", "ps1", "ps2", "ps3", "ps4", "ps5", "ps6"

    pr.op("pool", lambda e: e.memset(U4i, 1.0), writes=["U4i"])
    pr.op("pool", lambda e: e.memset(U4s, 1.0), writes=["U4s"])
    pr.op("pool", lambda e: e.memset(Lst, 1.0), writes=["Lst"])
    for i in range(4):
        cs = slice(i * 128, (i + 1) * 128)
        pr.op("pool", lambda e, cs=cs: e.affine_select(out=U4i[:, cs], in_=U4i[:, cs], pattern=[[1, 128]], compare_op=ALU.is_ge,
                                                       fill=0.0, base=0, channel_multiplier=-1), reads=["U4i"], writes=["U4i"])
        pr.op("pool", lambda e, cs=cs: e.affine_select(out=U4s[:, cs], in_=U4s[:, cs], pattern=[[1, 128]], compare_op=ALU.is_ge,
                                                       fill=0.0, base=-1, channel_multiplier=-1), reads=["U4s"], writes=["U4s"])
        pr.op("dve", lambda e, cs=cs: e.tensor_copy(out=I4[:, cs], in_=c.ident_f), reads=["ident_f"], writes=["I4"])
    pr.op("pool", lambda e: e.affine_select(out=Lst, in_=Lst, pattern=[[-1, 128]], compare_op=ALU.is_ge,
                                            fill=0.0, base=-1, channel_multiplier=1), reads=["Lst"], writes=["Lst"])
    pr.op("sp", lambda e: e.dma_start(out=alog, in_=bcast_rows(alog_d, 4)), writes=["alog"], dsem="c1")
    pr.op("sp", lambda e: e.dma_start(out=dtb, in_=bcast_rows(dtb_d, 4)), writes=["dtb"], dsem="c2")
    pr.op("sp", lambda e: e.dma_start(out=gnorm, in_=gnorm_d.rearrange("(p o) -> p o", o=1)), writes=["gnorm"], dsem="c3")
    pr.op("sp", lambda e: e.dma_start(out=rksc, in_=rksc_d[:, :]), writes=["rksc"], dsem="c4")
    pr.op("sp", lambda e: e.dma_start(out=ba, in_=tm32.rearrange("(n p) c -> p n c", p=128)), reads=["tm32"], writes=["ba"], dsem="c5")
    pr.op("act", lambda e: e.activation(out=alog, in_=alog, func=AF.Exp), reads=["alog"], writes=["alog"])
    pr.op("dve", lambda e: e.tensor_scalar(out=alog, in0=alog, scalar1=-1.0, scalar2=None, op0=ALU.mult), reads=["alog"], writes=["alog"])

    def conv(ch):
        pr.op("sp", lambda e: e.dma_start(out=raw, in_=fmv[:, ch, :]), reads=["fmT"], writes=["raw"], dsem="rawl")
        pr.op("sp", lambda e: e.dma_start(out=cw, in_=convT[ch * 128:(ch + 1) * 128, :]), writes=["cw"], dsem="cwl")
        pr.op("dve", lambda e: e.tensor_scalar(out=acc, in0=raw, scalar1=cw[:, 3:4], scalar2=None, op0=ALU.mult),
              reads=["raw", "cw"], writes=["acc"])
        for sh in (1, 2, 3):
            pr.op("dve", lambda e, sh=sh: e.scalar_tensor_tensor(out=acc[:, sh:S], in0=raw[:, 0:S - sh], scalar=cw[:, 3 - sh:4 - sh],
                                                                 in1=acc[:, sh:S], op0=ALU.mult, op1=ALU.add),
                  reads=["raw", "cw", "acc"], writes=["acc"])
        pr.op("act", lambda e: e.activation(out=acc, in_=acc, func=AF.Silu), reads=["acc"], writes=["acc"])

    def l2norm(outb, outkey, scale):
        for g in range(S // 512):
            cs = slice(g * 512, (g + 1) * 512)
            pr.op("pool", lambda e, cs=cs: e.tensor_tensor(out=sq, in0=acc[:, cs], in1=acc[:, cs], op=ALU.mult), reads=["acc"], writes=["sq"])
            pr.op("pe", lambda e: e.matmul(ps[3][:, :], lhsT=c.ones_b, rhs=sq, start=True, stop=True), reads=["sq", "ones_b"], writes=[K3])
            pr.op("dve", lambda e: e.tensor_scalar(out=rs, in0=ps[3][:, :], scalar1=EPS, scalar2=None, op0=ALU.add), reads=[K3], writes=["rs"])
            pr.op("act", lambda e: e.activation(out=rs, in_=rs, func=AF.Ln), reads=["rs"], writes=["rs"])
            pr.op("act", lambda e: e.activation(out=rs, in_=rs, func=AF.Exp, scale=-0.5), reads=["rs"], writes=["rs"])
            pr.op("dve", lambda e, cs=cs: e.scalar_tensor_tensor(out=outb[:, cs], in0=acc[:, cs], scalar=float(scale), in1=rs, op0=ALU.mult, op1=ALU.mult),
                  reads=["acc", "rs"], writes=[outkey])

    def epilogue(gq, chunk, wcol, wkey, src_ps=None, src_key=None):
        cs = slice(gq * 512, (gq + 1) * 512)
        s_ = gq % 2
        if src_ps is None:
            src_ps, src_key = ps[0], K0
        pr.op("act", lambda e: e.activation(out=o_sb, in_=src_ps[:, :], func=AF.Copy), reads=[src_key], writes=["o_sb"])
        pr.op("act", lambda e: e.activation(out=gate, in_=zt[:, cs], func=AF.Silu), reads=["zt"], writes=["gate"])
        fm_epilogue(pr, c, o_sb, "o_sb", 512, gate, "gate", wcol, wkey, sqb, ps[3], rs, ost[s_], ("ost", s_), psbkey=K3)
        pr.op("sp", lambda e: e.dma_start(out=oTv[:, chunk, cs], in_=ost[s_]), reads=[("ost", s_)], writes=["oT"], dsem=("ostd", s_))

    psT1 = ps[1][:, :].bitcast(BF16)

    for h in range(4):
        pr.op("act", lambda e, h=h: e.activation(out=tmpn, in_=ba[:, :, h], func=AF.Exp, scale=-1.0), reads=["ba"], writes=["tmpn"])
        pr.op("dve", lambda e: e.tensor_scalar(out=tmpn, in0=tmpn, scalar1=1.0, scalar2=None, op0=ALU.add), reads=["tmpn"], writes=["tmpn"])
        pr.op("dve", lambda e: e.reciprocal(out=tmpn, in_=tmpn), reads=["tmpn"], writes=["tmpn"])
        pr.op("dve", lambda e: e.tensor_scalar(out=negb, in0=tmpn, scalar1=-1.0, scalar2=None, op0=ALU.mult), reads=["tmpn"], writes=["negb"])
        pr.op("act", lambda e, h=h: e.activation(out=tmpn, in_=ba[:, :, 4 + h], func=AF.Exp, bias=dtb[:, h:h + 1]), reads=["ba", "dtb"], writes=["tmpn"])
        pr.op("dve", lambda e: e.tensor_scalar(out=tmpn, in0=tmpn, scalar1=1.0, scalar2=None, op0=ALU.add), reads=["tmpn"], writes=["tmpn"])
        pr.op("act", lambda e: e.activation(out=tmpn, in_=tmpn, func=AF.Ln), reads=["tmpn"], writes=["tmpn"])
        pr.op("dve", lambda e, h=h: e.tensor_scalar(out=gcol, in0=tmpn, scalar1=alog[:, h:h + 1], scalar2=None, op0=ALU.mult),
              reads=["tmpn", "alog"], writes=["gcol"])
        pr.op("pe", lambda e: e.matmul(ps[4][:, 0:NB], lhsT=U4i[:, 0:128], rhs=gcol, start=True, stop=True), reads=["U4i", "gcol"], writes=[K4])
        pr.op("pe", lambda e: e.matmul(ps[5][:, 0:NB], lhsT=c.ones_f, rhs=gcol, start=True, stop=True), reads=["ones_f", "gcol"], writes=[K5])
        pr.op("act", lambda e: e.activation(out=egc, in_=ps[4][:, 0:NB], func=AF.Exp), reads=[K4], writes=["egc"])
        pr.op("act", lambda e: e.activation(out=egl, in_=ps[5][:, 0:NB], func=AF.Exp), reads=[K5], writes=["egl"])
        pr.op("dve", lambda e: e.tensor_copy(out=tmpn, in_=ps[4][:, 0:NB]), reads=[K4], writes=["tmpn"])
        pr.op("dve", lambda e: e.tensor_tensor(out=tmpn, in0=ps[5][:, 0:NB], in1=tmpn, op=ALU.subtract), reads=[K5, "tmpn"], writes=["tmpn"])
        pr.op("act", lambda e: e.activation(out=edec, in_=tmpn, func=AF.Exp), reads=["tmpn"], writes=["edec"])
        conv(h)
        l2norm(qh, "qh", 128 ** -0.5)
        conv(4 + h)
        l2norm(kh, "kh", 1.0)
        for g4 in range(NG4):
            for i in range(4):
                n = 4 * g4 + i
                pr.op("pe", lambda e, n=n, i=i: e.transpose(out=psT1[:, i * 128:(i + 1) * 128], in_=kh[:, n * 128:(n + 1) * 128], identity=c.ident_b),
                      reads=["kh", "ident_b"], writes=[K1])
            for i in range(4):
                n = 4 * g4 + i
                pr.op("dve", lambda e, n=n, i=i: e.tensor_scalar(out=kdec[:, n, :], in0=psT1[:, i * 128:(i + 1) * 128], scalar1=edec[:, n:n + 1],
                                                                 scalar2=None, op0=ALU.mult), reads=[K1, "edec"], writes=["kdec"])
        conv(8 + h)
        for g4 in range(NG4):
            for i in range(4):
                n = 4 * g4 + i
                pr.op("pe", lambda e, n=n, i=i: e.transpose(out=ps[2][:, i * 128:(i + 1) * 128], in_=acc[:, n * 128:(n + 1) * 128], identity=c.ident_f),
                      reads=["acc", "ident_f"], writes=[K2])
            pr.op("act", lambda e, g4=g4: e.activation(out=vtok[:, 4 * g4:4 * g4 + 4, :], in_=v3(ps[2][:, :], 4), func=AF.Copy), reads=[K2], writes=["vtok"])
        pr.op("sp", lambda e, h=h: e.dma_start(out=zt, in_=fmv[:, 12 + h, :]), reads=["fmT"], writes=["zt"], dsem="ztl")
        pr.op("pool", lambda e: e.memset(S_f, 0.0), writes=["S_f"])
        pr.op("pool", lambda e: e.memset(S_b, 0.0), writes=["S_b"])
        def pre_gen(g4, b):
            gs = slice(g4 * 512, (g4 + 1) * 512)
            kP, kM, kQ = ("PTb", b), ("MTb", b), ("qtl", b)
            for i in range(4):
                n = 4 * g4 + i
                pr.op("dve", lambda e, n=n, i=i: e.tensor_scalar(out=Gt[:, i * 128:(i + 1) * 128], in0=U4i[:, 0:128], scalar1=gcol[:, n:n + 1],
                                                                 scalar2=None, op0=ALU.mult), reads=["U4i", "gcol"], writes=["Gt"])
            yield
            for i in range(4):
                cs = slice(i * 128, (i + 1) * 128)
                pr.op("pe", lambda e, cs=cs: e.matmul(ps[0][:, cs], lhsT=Lst, rhs=Gt[:, cs], start=True, stop=True), reads=["Lst", "Gt"], writes=[K0])
                pr.op("pe", lambda e, cs=cs: e.matmul(ps[1][:, cs], lhsT=c.ones_f, rhs=Gt[:, cs], start=True, stop=True), reads=["ones_f", "Gt"], writes=[K1])
            yield
            pr.op("act", lambda e: e.activation(out=Dx, in_=ps[0][:, :], func=AF.Exp), reads=[K0], writes=["Dx"])
            pr.op("act", lambda e: e.activation(out=EGs, in_=ps[1][:, :], func=AF.Exp), reads=[K1], writes=["EGs"])
            yield
            pr.op("dve", lambda e: e.tensor_tensor(out=Dm, in0=Dx, in1=U4i, op=ALU.mult), reads=["Dx", "U4i"], writes=["Dm"])
            pr.op("pool", lambda e: e.tensor_tensor(out=DmS, in0=Dx, in1=U4s, op=ALU.mult), reads=["Dx", "U4s"], writes=["DmS"])
            pr.op("dve", lambda e: e.tensor_tensor(out=qtl2[b], in0=qh[:, gs], in1=EGs, op=ALU.mult), reads=["qh", "EGs"], writes=[kQ])
            for i in range(4):
                n = 4 * g4 + i
                cs = slice(i * 128, (i + 1) * 128)
                ns = slice(n * 128, (n + 1) * 128)
                pr.op("pe", lambda e, cs=cs, ns=ns: e.matmul(ps[0][:, cs], lhsT=kh[:, ns], rhs=kh[:, ns], start=True, stop=True), reads=["kh"], writes=[K0])
                pr.op("pe", lambda e, cs=cs, ns=ns: e.matmul(ps[1][:, cs], lhsT=kh[:, ns], rhs=qh[:, ns], start=True, stop=True), reads=["kh", "qh"], writes=[K1])
            yield
            pr.op("dve", lambda e: e.tensor_tensor(out=MTb2[b], in0=ps[1][:, :], in1=Dm, op=ALU.mult), reads=[K1, "Dm"], writes=[kM])
            X, Y = XY[0]
            for i in range(4):
                n = 4 * g4 + i
                cs = slice(i * 128, (i + 1) * 128)
                pr.op("dve", lambda e, cs=cs, n=n: e.scalar_tensor_tensor(out=X[:, cs], in0=ps[0][:, cs], scalar=negb[:, n:n + 1], in1=DmS[:, cs],
                                                                          op0=ALU.mult, op1=ALU.mult), reads=[K0, "negb", "DmS"], writes=["X0"])
            yield
            for i in range(4):
                cs = slice(i * 128, (i + 1) * 128)
                pr.op("pe", lambda e, cs=cs: e.transpose(out=ps[5][:, cs], in_=X[:, cs], identity=c.ident_f), reads=["X0", "ident_f"], writes=[K5])
            pr.op("dve", lambda e: e.tensor_tensor(out=Pm, in0=X, in1=I4, op=ALU.add), reads=["X0", "I4"], writes=["Pm"])
            yield
            pr.op("act", lambda e: e.activation(out=Y, in_=ps[5][:, :], func=AF.Copy), reads=[K5], writes=["Y0"])
            yield
            cur = 0
            for m in range(6):
                Xc, Yc = XY[cur]
                Xn, Yn = XY[1 - cur]
                kc = ("X%d" % cur, "Y%d" % cur)
                kn_ = ("X%d" % (1 - cur), "Y%d" % (1 - cur))
                for i in range(4):
                    cs = slice(i * 128, (i + 1) * 128)
                    pr.op("pe", lambda e, cs=cs: e.matmul(ps[5][:, cs], lhsT=Xc[:, cs], rhs=Yc[:, cs], start=True, stop=True),
                          reads=[kc[0], kc[1]], writes=[K5])
                if m < 5:
                    for i in range(4):
                        cs = slice(i * 128, (i + 1) * 128)
                        pr.op("pe", lambda e, cs=cs: e.matmul(ps[4][:, cs], lhsT=Yc[:, cs], rhs=Xc[:, cs], start=True, stop=True),
                              reads=[kc[0], kc[1]], writes=[K4])
                yield
                pr.op("act", lambda e: e.activation(out=Yn, in_=ps[5][:, :], func=AF.Copy), reads=[K5], writes=[kn_[1]])
                if m < 5:
                    pr.op("dve", lambda e: e.tensor_copy(out=Xn, in_=ps[4][:, :]), reads=[K4], writes=[kn_[0]])
                yield
                for i in range(4):
                    cs = slice(i * 128, (i + 1) * 128)
                    pr.op("pe", lambda e, cs=cs: e.matmul(ps[6][:, cs], lhsT=Yn[:, cs], rhs=Pm[:, cs], start=True, stop=True),
                          reads=[kn_[1], "Pm"], writes=[K6])
                yield
                pr.op("dve", lambda e: e.tensor_tensor(out=Pm, in0=ps[6][:, :], in1=Pm, op=ALU.add), reads=[K6, "Pm"], writes=["Pm"])
                yield
                cur = 1 - cur
            pr.op("act", lambda e: e.activation(out=PTb2[b], in_=Pm, func=AF.Copy), reads=["Pm"], writes=[kP])
            yield

        def scan_gen(g4, b):
            kP, kM, kQ = ("PTb", b), ("MTb", b), ("qtl", b)
            for i in range(4):
                n = 4 * g4 + i
                cs = slice(i * 128, (i + 1) * 128)
                ns = slice(n * 128, (n + 1) * 128)
                pr.op("pe", lambda e: e.matmul(ps[7][:, 0:128], lhsT=kh[:, ns], rhs=S_b, start=True, stop=True), reads=["kh", "S_b"], writes=[("ps7", 0)])
                yield
                pr.op("dve", lambda e: e.scalar_tensor_tensor(out=Rm, in0=ps[7][:, 0:128], scalar=egc[:, n:n + 1], in1=vtok[:, n, :],
                                                              op0=ALU.mult, op1=ALU.subtract), reads=[("ps7", 0), "egc", "vtok"], writes=["Rm"])
                yield
                pr.op("pe", lambda e: e.matmul(ps[7][:, 128:256], lhsT=PTb2[b][:, cs], rhs=Rm, start=True, stop=True), reads=[kP, "Rm"], writes=[("ps7", 1)])
                yield
                pr.op("dve", lambda e: e.tensor_scalar(out=vnew, in0=ps[7][:, 128:256], scalar1=negb[:, n:n + 1], scalar2=None, op0=ALU.mult),
                      reads=[("ps7", 1), "negb"], writes=["vnew"])
                yield
                pr.op("pe", lambda e: e.matmul(ps[2][:, cs], lhsT=S_b, rhs=qtl2[b][:, cs], start=True, stop=False), reads=["S_b", kQ], writes=[K2])
                pr.op("pe", lambda e: e.matmul(ps[2][:, cs], lhsT=vnew, rhs=MTb2[b][:, cs], start=False, stop=True), reads=["vnew", kM], writes=[K2])
                pr.op("pe", lambda e: e.matmul(ps[7][:, 256:384], lhsT=kdec[:, n, :], rhs=vnew, start=True, stop=True), reads=["kdec", "vnew"], writes=[("ps7", 2)])
                yield
                pr.op("dve", lambda e: e.scalar_tensor_tensor(out=S_f, in0=S_f, scalar=egl[:, n:n + 1], in1=ps[7][:, 256:384],
                                                              op0=ALU.mult, op1=ALU.add), reads=["S_f", "egl", ("ps7", 2)], writes=["S_f"])
                pr.op("act", lambda e: e.activation(out=S_b, in_=S_f, func=AF.Copy), reads=["S_f"], writes=["S_b"])
                yield
            epilogue(g4, h, gnorm, "gnorm", src_ps=ps[2], src_key=K2)
            yield

        for g4 in range(NG4 + 1):
            gens = []
            if g4 < NG4:
                gens.append(pre_gen(g4, g4 % 2))
            if g4 >= 1:
                gens.append(scan_gen(g4 - 1, (g4 - 1) % 2))
            while gens:
                for gg in list(gens):
                    try:
                        next(gg)
                    except StopIteration:
                        gens.remove(gg)

    for h in range(4):
        rows = slice(64 * (h % 2), 64 * (h % 2) + 64)
        gam = 1.0 - 2.0 ** (-5.0 - h)
        pr.op("sp", lambda e, h=h: e.dma_start(out=qh, in_=fmv[:, 16 + h // 2, :]), reads=["fmT"], writes=["qh"], dsem="ql")
        pr.op("sp", lambda e, h=h: e.dma_start(out=kh, in_=fmv[:, 18 + h // 2, :]), reads=["fmT"], writes=["kh"], dsem="kl")
        pr.op("sp", lambda e, h=h: e.dma_start(out=zt, in_=fmv[:, 20 + h, :]), reads=["fmT"], writes=["zt"], dsem="ztl")
        pr.op("sp", lambda e, h=h: e.dma_start(out=rmask, in_=rmask_d[h, :, :]), writes=["rmask"], dsem="rml")
        pr.op("sp", lambda e, h=h: e.dma_start(out=rqsc, in_=rqsc_d[h, :, :]), writes=["rqsc"], dsem="rql")
        for n0 in range(0, NB, 16):
            n1 = min(NB, n0 + 16)
            pr.op("sp", lambda e, h=h, n0=n0, n1=n1: e.dma_start(
                out=vtok[:, n0:n1, :], in_=tm.rearrange("(n p) c -> p n c", p=128)[:, n0:n1, h * 128:(h + 1) * 128]),
                  reads=["tm"], writes=["vtok"], dsem="vl")
        for g4 in range(NG4):
            for i in range(4):
                n = 4 * g4 + i
                pr.op("pe", lambda e, n=n, i=i: e.transpose(out=psT1[:, i * 64:(i + 1) * 64], in_=kh[rows, n * 128:(n + 1) * 128],
                                                            identity=c.ident_b[rows, rows]), reads=["kh", "ident_b"], writes=[K1])
            pr.op("dve", lambda e, g4=g4, h=h: e.tensor_scalar(out=kdec[:, 4 * g4:4 * g4 + 4, 0:64], in0=v3(psT1[:, 0:256], 4), scalar1=rksc[:, h:h + 1],
                                                               scalar2=None, op0=ALU.mult), reads=[K1, "rksc"], writes=["kdec"])
        pr.op("pool", lambda e: e.memset(S_f, 0.0), writes=["S_f"])
        pr.op("pool", lambda e: e.memset(S_b, 0.0), writes=["S_b"])
        for g4 in range(NG4):
            gs = slice(g4 * 512, (g4 + 1) * 512)
            for i in range(4):
                n = 4 * g4 + i
                cs = slice(i * 128, (i + 1) * 128)
                ns = slice(n * 128, (n + 1) * 128)
                pr.op("pe", lambda e, cs=cs, ns=ns: e.matmul(ps[2][:, cs], lhsT=kh[rows, ns], rhs=qh[rows, ns], start=True, stop=True), reads=["kh", "qh"], writes=[K2])
            pr.op("dve", lambda e: e.tensor_tensor(out=PTb, in0=ps[2][:, :], in1=rmask, op=ALU.mult), reads=[K2, "rmask"], writes=[("PTb", 0)])
            pr.op("dve", lambda e, gs=gs: e.tensor_tensor(out=qtl[rows, :], in0=qh[rows, gs], in1=rqsc[rows, :], op=ALU.mult), reads=["qh", "rqsc"], writes=[("qtl", 0)])
            for i in range(4):
                n = 4 * g4 + i
                cs = slice(i * 128, (i + 1) * 128)
                pr.op("pe", lambda e, cs=cs: e.matmul(ps[0][:, cs], lhsT=S_b[rows, :], rhs=qtl[rows, cs], start=True, stop=False), reads=["S_b", ("qtl", 0)], writes=[K0])
                pr.op("pe", lambda e, cs=cs, n=n: e.matmul(ps[0][:, cs], lhsT=vtok[:, n, :], rhs=PTb[:, cs], start=False, stop=True), reads=["vtok", ("PTb", 0)], writes=[K0])
                pr.op("pe", lambda e, n=n: e.matmul(ps[7][rows, 0:128], lhsT=kdec[:, n, 0:64], rhs=vtok[:, n, :], start=True, stop=True), reads=["kdec", "vtok"], writes=[("ps7", 0)])
                pr.op("dve", lambda e: e.scalar_tensor_tensor(out=S_f[rows, :], in0=S_f[rows, :], scalar=float(gam ** 128), in1=ps[7][rows, 0:128],
                                                              op0=ALU.mult, op1=ALU.add), reads=["S_f", ("ps7", 0)], writes=["S_f"])
                pr.op("act", lambda e: e.activation(out=S_b[rows, :], in_=S_f[rows, :], func=AF.Copy), reads=["S_f"], writes=["S_b"])
            epilogue(g4, 4 + h, None, None)


ROPE_THETA = 10000.0


def rope_np(S, dim):
    inv = (ROPE_THETA ** (-np.arange(0, dim, 2, dtype=np.float32) / np.float32(dim))).astype(np.float32)
    ang = np.arange(S, dtype=np.float32)[:, None] * inv[None, :]
    return np.cos(ang).astype(np.float32), np.sin(ang).astype(np.float32)


def rope_table(S, dim, reps, scale):
    cos, sin = rope_np(S, dim)
    half = dim // 2
    cl = np.concatenate([cos.T, cos.T], axis=0)
    sl = np.concatenate([-sin.T, sin.T], axis=0)
    cl = np.tile(cl, (reps, 1)) * np.float32(scale)
    sl = np.tile(sl, (reps, 1)) * np.float32(scale)
    return np.stack([cl, sl], 0).astype(np.float32)


def perm_cols(col0, nheads, dim):
    idx = []
    half = dim // 2
    for h in range(nheads):
        for f in range(dim):
            idx.append(col0 + h * dim + (f + half) % dim)
    return np.array(idx)


def host_consts(S, weights):
    c = {"ident": np.eye(128, dtype=np.float32)}
    w1 = weights["l1_w_in"]
    c["l1_w_ext"] = np.ascontiguousarray(np.concatenate(
        [w1, w1[:, perm_cols(0, 4, 128)], w1[:, perm_cols(512, 4, 128)],
         w1[:, perm_cols(1536, 8, 64)], w1[:, perm_cols(2048, 8, 64)]], axis=1))
    c["l1_tabs"] = np.ascontiguousarray(np.stack([
        rope_table(S, 128, 1, 128 ** -0.5), rope_table(S, 128, 1, 1.0),
        rope_table(S, 64, 2, 64 ** -0.5), rope_table(S, 64, 2, 1.0)], 0))
    w0 = weights["l0_w_in"]
    c["l0_w_ext"] = np.ascontiguousarray(np.concatenate([w0, w0[:, perm_cols(2056, 4, 64)], w0[:, perm_cols(2312, 4, 64)]], axis=1))
    c["l0_tabs"] = np.ascontiguousarray(np.stack([rope_table(S, 64, 2, 64 ** -0.5), rope_table(S, 64, 2, 1.0)], 0))
    c["l0_convT"] = np.ascontiguousarray(weights["l0_conv_w"].T)
    idx = np.arange(128, dtype=np.float64)
    rm = np.zeros((4, 128, 512), np.float32)
    rq = np.zeros((4, 128, 512), np.float32)
    rk = np.zeros((128, 4), np.float32)
    for h in range(4):
        lg = np.log(1.0 - 2.0 ** (-5.0 - h))
        rel = idx[None, :] - idx[:, None]
        m = np.where(rel >= 0, np.exp(np.where(rel >= 0, rel, 0.0) * lg), 0.0)
        rm[h] = np.tile(m, (1, 4))
        rq[h] = np.tile(np.exp((idx + 1.0) * lg)[None, :], (128, 4))
        rk[:, h] = np.exp((127.0 - idx) * lg)
    c["l0_rmask"] = rm
    c["l0_rqsc"] = rq
    c["l0_rksc"] = rk
    c["l1_lam"] = np.ascontiguousarray(np.concatenate(
        [weights["l1_lambda_q1"], weights["l1_lambda_k1"], weights["l1_lambda_q2"], weights["l1_lambda_k2"]]))
    return c


W_NAMES = ["l0_ffn1_norm", "l0_ffn1_w_up", "l0_ffn1_w_down", "l0_mix_norm", "l0_w_in", "l0_conv_w", "l0_a_log",
           "l0_dt_bias", "l0_gdn_norm", "l0_w_out", "l0_ffn2_norm", "l0_ffn2_w_up", "l0_ffn2_w_down",
           "l1_ffn1_norm", "l1_ffn1_w_up", "l1_ffn1_w_down", "l1_mix_norm", "l1_w_in", "l1_lambda_q1", "l1_lambda_k1",
           "l1_lambda_q2", "l1_lambda_k2", "l1_diff_norm", "l1_w_out", "l1_ffn2_norm", "l1_ffn2_w_up", "l1_ffn2_w_down",
           "final_norm"]


def build_program(S, phases, shapes):
    nc = bass.Bass("TRN2", target_bir_lowering=False)
    stack = ExitStack()
    pr = Prog(nc, stack)
    c = Ctx()
    c.dram = {}
    for name, shp in shapes.items():
        c.dram[name] = nc.dram_tensor(name, list(shp), F32, kind="ExternalInput").ap()
    x_in = nc.dram_tensor("x", [S, D], F32, kind="ExternalInput").ap()
    out = nc.dram_tensor("out", [S, D], F32, kind="ExternalOutput").ap()
    xs = nc.dram_tensor("xstream", [S, D], F32, kind="Internal").ap()
    with stack:
        pr.init_sbuf()
        setup_common(pr, c)
        src = x_in
        w = c.dram
        for i, ph in enumerate(phases):
            last = (i == len(phases) - 1)
            if ph in ("l0_ffn1", "l0_ffn2", "l1_ffn1", "l1_ffn2"):
                fin = w["final_norm"] if ph == "l1_ffn2" else None
                if fin is None and last:
                    ffn_phase(pr, c, ph, src, out, w[ph + "_w_up"], w[ph + "_w_down"], w[ph + "_norm"], S)
                else:
                    ffn_phase(pr, c, ph, src, xs, w[ph + "_w_up"], w[ph + "_w_down"], w[ph + "_norm"], S,
                              fin=fin, out_final=out)
                src = xs
            elif ph == "l0_mix":
                fmT = nc.dram_tensor("fmT0", [24, 128, S], BF16, kind="Internal").ap()
                tm = nc.dram_tensor("tm0", [S, 512], BF16, kind="Internal").ap()
                tm32 = nc.dram_tensor("tm32_0", [S, 8], F32, kind="Internal").ap()
                oT = nc.dram_tensor("oT0", [8, 128, S], BF16, kind="Internal").ap()
                fm_jobs = ([(j * 128, None, None) for j in range(16)] + [(2056 + j * 128, 3592 + j * 128, 0) for j in range(2)]
                           + [(2312 + j * 128, 3848 + j * 128, 1) for j in range(2)] + [(3080 + j * 128, None, None) for j in range(4)])
                proj_phase(pr, c, S, src, w["l0_w_ext"], 4104, w["l0_mix_norm"], fm_jobs, [2568], fmT, tm, w["l0_tabs"],
                           tm32_job=(2048, 8), tm32_out=tm32)
                l0_attn_phase(pr, c, S, fmT, tm, tm32, oT, w["l0_convT"], w["l0_a_log"], w["l0_dt_bias"], w["l0_gdn_norm"],
                              w["l0_rmask"], w["l0_rqsc"], w["l0_rksc"])
                dstp = out if last else xs
                outproj_phase(pr, c, S, oT, w["l0_w_out"], src, dstp)
                src = xs
            elif ph == "l1_mix":
                fmT = nc.dram_tensor("fmT1", [16, 128, S], BF16, kind="Internal").ap()
                tm = nc.dram_tensor("tm1", [S, 1024], BF16, kind="Internal").ap()
                oT = nc.dram_tensor("oT1", [8, 128, S], BF16, kind="Internal").ap()
                fm_jobs = ([(h * 128, 3072 + h * 128, 0) for h in range(4)] + [(512 + h * 128, 3584 + h * 128, 1) for h in range(4)]
                           + [(1536 + h * 128, 4096 + h * 128, 2) for h in range(4)] + [(2048 + h * 128, 4608 + h * 128, 3) for h in range(4)])
                proj_phase(pr, c, S, src, w["l1_w_ext"], 5120, w["l1_mix_norm"], fm_jobs, [1024, 2560], fmT, tm, w["l1_tabs"])
                lambda_init = 0.8 - 0.6 * math.exp(-0.3 * 1)
                l1_attn_phase(pr, c, S, fmT, tm, oT, w["l1_lam"], w["l1_diff_norm"], lambda_init)
                dstp = out if last else xs
                outproj_phase(pr, c, S, oT, w["l1_w_out"], src, dstp)
                src = xs
            else:
                raise ValueError(ph)
        pr.barrier()
        pr.op("sp", None)
        pr.emit()
    return nc


ALL_PHASES = ["l0_ffn1", "l0_mix", "l0_ffn2", "l1_ffn1", "l1_mix", "l1_ffn2"]


def run(S, phases, x_list, weights):
    consts = host_consts(S, weights)
    shapes = {k: v.shape for k, v in weights.items()}
    shapes.update({k: v.shape for k, v in consts.items()})
    nc = build_program(S, phases, shapes)
    in_maps = []
    for xb in x_list:
        m = {"x": np.ascontiguousarray(xb)}
        m.update(weights)
        m.update(consts)
        in_maps.append(m)
    res = run_bass_kernel_spmd(nc, in_maps, core_ids=list(range(len(x_list))))
    return [r["out"] for r in res.results]


def kernel(**inputs):
    x = np.asarray(inputs["x"])
    B, S, _ = x.shape
    weights = {k: np.ascontiguousarray(np.asarray(inputs[k], dtype=np.float32)) for k in W_NAMES}
    outs = run(S, ALL_PHASES, [x[b] for b in range(B)], weights)
    return np.stack(outs, axis=0).astype(np.float32)
```
